# Optimizing a Trainium2 kernel written in Bass

```python
import jax, jax.numpy as jnp
from jax import lax
import numpy as np

D_MODEL = 2048
BATCH = 1
SEQ = 8192
DEPTH = 1

CHUNK = 128
A_GROUPS = 8
A_GROUP_DIM = 128
A_WIDTH = A_GROUPS * A_GROUP_DIM
SB_HEADS = 8
SB_HEAD_DIM = 128
SB_WIDTH = SB_HEADS * SB_HEAD_DIM
Q_BLOCK = 128
N_GROUPS = 4
EXPERTS_PER_GROUP = 8
N_EXPERTS = N_GROUPS * EXPERTS_PER_GROUP
TOP_K_IN_GROUP = 2
D_EXPERT = D_MODEL // 2
MOE_BLOCK = 128
EPS = 1e-6
PROJ_WIDTH = 2 * A_WIDTH + 3 * SB_WIDTH + 2 * D_MODEL
SPLITS = [A_WIDTH, 2 * A_WIDTH, 2 * A_WIDTH + SB_WIDTH, 2 * A_WIDTH + 2 * SB_WIDTH,
          2 * A_WIDTH + 3 * SB_WIDTH, 2 * A_WIDTH + 3 * SB_WIDTH + D_MODEL]

kernel_name = "hybrid_gmlp_stickbreak_hmoe"


def rms_norm(x, g):
    xf = x.astype(jnp.float32)
    y = xf * lax.rsqrt(jnp.mean(xf * xf, axis=-1, keepdims=True) + EPS)
    return (y * g.astype(jnp.float32)).astype(x.dtype)


def layer_norm(x, g, b):
    xf = x.astype(jnp.float32)
    mu = jnp.mean(xf, axis=-1, keepdims=True)
    xc = xf - mu
    y = xc * lax.rsqrt(jnp.mean(xc * xc, axis=-1, keepdims=True) + EPS)
    return (y * g.astype(jnp.float32) + b.astype(jnp.float32)).astype(x.dtype)


def chunked_spatial_gating(u, v, ln_g, ln_b, w_s, b_s):
    B, S, _ = v.shape
    v = layer_norm(v, ln_g, ln_b)
    vc = v.reshape(B, S // CHUNK, CHUNK, A_GROUPS, A_GROUP_DIM)
    causal = jnp.tril(jnp.ones((CHUNK, CHUNK), dtype=bool))
    ws = jnp.where(causal[None], w_s, 0)
    mixed = jnp.einsum('gts,bcsgd->bctgd', ws, vc) + b_s.T[:, :, None]
    return u * mixed.reshape(B, S, A_WIDTH)


def stick_breaking_attention(q, k, v):
    B, S, _ = q.shape
    nb = S // Q_BLOCK
    def heads(t):
        return t.reshape(B, S, SB_HEADS, SB_HEAD_DIM).transpose(0, 2, 1, 3)
    q, k, v = heads(q), heads(k), heads(v)
    v32 = v.astype(jnp.float32)
    q_blocks = q.reshape(B, SB_HEADS, nb, Q_BLOCK, SB_HEAD_DIM).transpose(2, 0, 1, 3, 4)
    kpos = jnp.arange(S)
    scale = SB_HEAD_DIM ** -0.5

    def block(args):
        qi, i = args
        z = jnp.einsum('bhqd,bhkd->bhqk', qi, k).astype(jnp.float32) * scale
        qpos = i * Q_BLOCK + jnp.arange(Q_BLOCK)
        mask = kpos[None, :] < qpos[:, None]
        log_rem = jnp.where(mask, jax.nn.log_sigmoid(-z), 0.0)
        suffix = lax.cumsum(log_rem, axis=3, reverse=True) - log_rem
        weights = jnp.where(mask, jnp.exp(jax.nn.log_sigmoid(z) + suffix), 0.0)
        return jnp.einsum('bhqk,bhkd->bhqd', weights, v32).astype(v.dtype)

    o = lax.map(block, (q_blocks, jnp.arange(nb)))
    o = o.transpose(1, 0, 3, 2, 4).reshape(B, S, SB_WIDTH)
    return o


def hierarchical_moe(h, w_rg, b_rg, w_re, b_re, w_gate, w_up, w_down):
    B, S, D = h.shape
    T = B * S
    xt = h.reshape(T, D)
    lg = jnp.einsum('td,dg->tg', xt, w_rg).astype(jnp.float32) + b_rg.astype(jnp.float32)
    pg = jax.nn.softmax(lg, axis=-1)
    grp = jnp.argmax(lg, axis=-1)
    pg_sel = jnp.take_along_axis(pg, grp[:, None], axis=1)[:, 0]
    le = (jnp.einsum('td,de->te', xt, w_re).astype(jnp.float32)
          + b_re.astype(jnp.float32)).reshape(T, N_GROUPS, EXPERTS_PER_GROUP)
    le_sel = jnp.take_along_axis(le, grp[:, None, None], axis=1)[:, 0]
    top_v, top_i = lax.top_k(le_sel, TOP_K_IN_GROUP)
    pe = jax.nn.softmax(top_v, axis=-1)
    gate_w = pg_sel[:, None] * pe
    expert = grp[:, None] * EXPERTS_PER_GROUP + top_i

    N = T * TOP_K_IN_GROUP
    e_flat = expert.reshape(N).astype(jnp.int32)
    w_flat = gate_w.reshape(N)
    tok_flat = jnp.repeat(jnp.arange(T, dtype=jnp.int32), TOP_K_IN_GROUP)
    counts = jnp.bincount(e_flat, length=N_EXPERTS)
    starts = jnp.cumsum(counts) - counts
    padded = ((counts + MOE_BLOCK - 1) // MOE_BLOCK) * MOE_BLOCK
    pends = jnp.cumsum(padded)
    pstarts = pends - padded
    order = jnp.argsort(e_flat, stable=True)
    se = e_flat[order]
    dest = pstarts[se] + (jnp.arange(N) - starts[se])
    n_rows = ((N + MOE_BLOCK - 1) // MOE_BLOCK + N_EXPERTS) * MOE_BLOCK
    row_tok = jnp.zeros((n_rows,), jnp.int32).at[dest].set(tok_flat[order])
    row_w = jnp.zeros((n_rows,), jnp.float32).at[dest].set(w_flat[order])
    nblk = n_rows // MOE_BLOCK
    block_start = jnp.arange(nblk) * MOE_BLOCK
    block_expert = jnp.minimum(jnp.sum(block_start[:, None] >= pends[None, :], axis=1),
                               N_EXPERTS - 1)

    def expert_block(args):
        tok, wrow, e = args
        xb = xt[tok]
        hdn = jax.nn.silu(xb @ w_gate[e]) * (xb @ w_up[e])
        return (hdn @ w_down[e]) * wrow[:, None].astype(xb.dtype)

    y = lax.map(expert_block, (row_tok.reshape(nblk, MOE_BLOCK),
                               row_w.reshape(nblk, MOE_BLOCK), block_expert))
    out = jnp.zeros((T, D), xt.dtype).at[row_tok].add(y.reshape(n_rows, D))
    return out.reshape(B, S, D)


def setup_inputs(seed: int = 0) -> dict:
    key = jax.random.key(seed)
    ks = jax.random.split(key, 20)
    def nrm(k, shape, scale):
        return jax.random.normal(k, shape, jnp.float32) * scale
    return {
        "x": nrm(ks[0], (BATCH, SEQ, D_MODEL), 1.0),
        "g_mix": 1.0 + nrm(ks[1], (DEPTH, D_MODEL), 0.01),
        "w_in": nrm(ks[2], (DEPTH, D_MODEL, PROJ_WIDTH), D_MODEL ** -0.5),
        "ln_v_g": 1.0 + nrm(ks[3], (DEPTH, A_WIDTH), 0.01),
        "ln_v_b": nrm(ks[4], (DEPTH, A_WIDTH), 0.01),
        "w_spatial": jnp.tril(nrm(ks[5], (DEPTH, A_GROUPS, CHUNK, CHUNK), 0.5 * CHUNK ** -0.5)),
        "b_spatial": 1.0 + nrm(ks[6], (DEPTH, A_GROUPS, CHUNK), 0.01),
        "w_branch_a": nrm(ks[7], (DEPTH, A_WIDTH, D_MODEL), A_WIDTH ** -0.5),
        "w_branch_b": nrm(ks[8], (DEPTH, SB_WIDTH, D_MODEL), SB_WIDTH ** -0.5),
        "w_out": nrm(ks[9], (DEPTH, D_MODEL, D_MODEL), D_MODEL ** -0.5),
        "g_ffn": 1.0 + nrm(ks[10], (DEPTH, D_MODEL), 0.01),
        "w_router_group": nrm(ks[11], (DEPTH, D_MODEL, N_GROUPS), D_MODEL ** -0.5),
        "b_router_group": nrm(ks[12], (DEPTH, N_GROUPS), 0.01),
        "w_router_expert": nrm(ks[13], (DEPTH, D_MODEL, N_EXPERTS), D_MODEL ** -0.5),
        "b_router_expert": nrm(ks[14], (DEPTH, N_EXPERTS), 0.01),
        "w_gate": nrm(ks[15], (DEPTH, N_EXPERTS, D_MODEL, D_EXPERT), D_MODEL ** -0.5),
        "w_up": nrm(ks[16], (DEPTH, N_EXPERTS, D_MODEL, D_EXPERT), D_MODEL ** -0.5),
        "w_down": nrm(ks[17], (DEPTH, N_EXPERTS, D_EXPERT, D_MODEL), D_EXPERT ** -0.5),
        "g_final": 1.0 + nrm(ks[18], (D_MODEL,), 0.01),
    }


def reference(x, g_mix, w_in, ln_v_g, ln_v_b, w_spatial, b_spatial, w_branch_a, w_branch_b,
              w_out, g_ffn, w_router_group, b_router_group, w_router_expert, b_router_expert,
              w_gate, w_up, w_down, g_final):
    for i in range(DEPTH):
        h = rms_norm(x, g_mix[i])
        proj = jnp.einsum('bsd,dp->bsp', h, w_in[i])
        u_a, v_a, q, k, v, gate_a, gate_b = jnp.split(proj, SPLITS, axis=-1)
        y_a = chunked_spatial_gating(jax.nn.gelu(u_a), jax.nn.gelu(v_a),
                                     ln_v_g[i], ln_v_b[i], w_spatial[i], b_spatial[i])
        y_b = stick_breaking_attention(q, k, v)
        merged = (jax.nn.sigmoid(gate_a) * jnp.einsum('bsc,cd->bsd', y_a, w_branch_a[i])
                  + jax.nn.sigmoid(gate_b) * jnp.einsum('bsc,cd->bsd', y_b, w_branch_b[i]))
        x = x + jnp.einsum('bsd,de->bse', merged, w_out[i])
        h2 = rms_norm(x, g_ffn[i])
        x = x + hierarchical_moe(h2, w_router_group[i], b_router_group[i],
                                 w_router_expert[i], b_router_expert[i],
                                 w_gate[i], w_up[i], w_down[i])
    return rms_norm(x, g_final)
```

```python
import contextlib
import numpy as np
import concourse.bass as bass
import concourse.mybir as mybir
from concourse.bass_utils import run_bass_kernel_spmd

F32 = mybir.dt.float32
BF16 = mybir.dt.bfloat16
AF = mybir.ActivationFunctionType
ALU = mybir.AluOpType
AX = mybir.AxisListType

D = 2048
NDC = 16
EPS = 1e-6
NEG = -30000.0


class Buf:
    __slots__ = ("name", "w", "r", "dsem", "dcnt")

    def __init__(self, name):
        self.name = name
        self.w = None
        self.r = []
        self.dsem = None
        self.dcnt = 0


class FW:
    def __init__(self, nc, stack):
        self.nc = nc
        self.stack = stack
        self.eng = {"pe": nc.tensor, "act": nc.scalar, "dve": nc.vector, "pool": nc.gpsimd, "sp": nc.sync}
        self.sems = {}
        self.cnt = {}
        self.cur = {}
        self.waited = {k: {} for k in self.eng}
        self.nsem = 0
        self.dbufs = []
        self.nins = 0
        for k in ("pe", "act", "dve", "pool"):
            self._new_sem(k)

    def _mk(self, name):
        h = self.stack.enter_context(self.nc.semaphore(name))
        self.nsem += 1
        return h

    def _new_sem(self, k):
        name = f"s_{k}_{self.nsem}"
        self.sems[name] = self._mk(name)
        self.cnt[name] = 0
        self.cur[k] = name

    def _wait(self, k, tok):
        if tok is None:
            return
        s, v = tok
        if self.waited[k].get(s, 0) >= v:
            return
        self.eng[k].wait_ge(self.sems[s], v)
        self.waited[k][s] = v

    def _deps(self, k, reads, writes, extra):
        for b in reads:
            self._wait(k, b.w)
        for b in writes:
            self._wait(k, b.w)
            for t in b.r:
                self._wait(k, t)
        for t in extra:
            self._wait(k, t)

    @staticmethod
    def _commit(tok, reads, writes):
        for b in reads:
            b.r.append(tok)
        for b in writes:
            b.w = tok
            b.r = []

    def op(self, k, fns, reads=(), writes=(), extra=()):
        if callable(fns):
            fns = [fns]
        self._deps(k, reads, writes, extra)
        e = self.eng[k]
        ins = None
        for f in fns:
            ins = f(e)
            self.nins += 1
        s = self.cur[k]
        ins.then_inc(self.sems[s], 1)
        self.cnt[s] += 1
        tok = (s, self.cnt[s])
        self._commit(tok, reads, writes)
        return tok

    def dma(self, q, pairs, dst, reads=(), extra=()):
        self._deps(q, reads, [dst], extra)
        e = self.eng[q]
        if dst.dsem is None:
            dst.dsem = f"d_{dst.name}_{self.nsem}"
            self.sems[dst.dsem] = self._mk(dst.dsem)
            self.dbufs.append(dst)
        for (o, i) in pairs:
            e.dma_start(out=o, in_=i).then_inc(self.sems[dst.dsem], 16)
            dst.dcnt += 16
            self.nins += 1
        tok = (dst.dsem, dst.dcnt)
        self._commit(tok, reads, [dst])
        return tok

    def barrier(self, engines=("pe", "act", "dve", "pool", "sp")):
        toks = [(s, self.cnt[s]) for s in self.cnt if self.cnt[s] > 0]
        toks += [(b.dsem, b.dcnt) for b in self.dbufs if b.dcnt > 0]
        for k in engines:
            for t in toks:
                self._wait(k, t)


def build_nc(NT, DE, n_moe_experts=32, debug=False):
    S = NT * 512
    NB = S // 128
    NFC = DE // 128
    SCALE = 128 ** -0.5
    nc = bass.Bass("TRN2", target_bir_lowering=False)

    def din(name, shape):
        return nc.dram_tensor(name, shape, F32, kind="ExternalInput").ap()

    x_all = din("x_all", [S, D])
    x_own = din("x_own", [1024, D])
    qpos_d = din("qpos", [1, 1024])
    g_mix = din("g_mix", [1, D])
    w_in = din("w_in", [D, 9216])
    ln_g = din("ln_v_g", [1, 1024])
    ln_b = din("ln_v_b", [1, 1024])
    w_sp = din("w_spatial", [8, 128, 128])
    b_sp = din("b_spatial", [1, 1024])
    w_ba = din("w_branch_a", [1024, D])
    w_bb = din("w_branch_b", [1024, D])
    w_out = din("w_out", [D, D])
    g_ffn = din("g_ffn", [1, D])
    w_rg = din("w_router_group", [D, 4])
    b_rg = din("b_router_group", [1, 4])
    w_re = din("w_router_expert", [D, 32])
    b_re = din("b_router_expert", [1, 32])
    w_gate = din("w_gate", [32 * D, DE])
    w_up = din("w_up", [32 * D, DE])
    w_down = din("w_down", [32 * DE, D])
    g_fin = din("g_final", [1, D])
    out_d = nc.dram_tensor("out", [1024, D], F32, kind="ExternalOutput").ap()
    hTs = nc.dram_tensor("hTs", [NT * 128, NDC * 512], BF16).ap()
    hTo = nc.dram_tensor("hTo", [2 * 128, NDC * 512], BF16).ap()
    mTs = nc.dram_tensor("mTs", [128, NDC * 1024], BF16).ap()
    if debug:
        dbg_ya = nc.dram_tensor("dbg_ya", [128, 8 * 1024], BF16, kind="ExternalOutput").ap()
        dbg_yb = nc.dram_tensor("dbg_yb", [128, 8 * 1024], BF16, kind="ExternalOutput").ap()
        dbg_x1 = nc.dram_tensor("dbg_x1", [128, 8 * D], F32, kind="ExternalOutput").ap()
        dbg_lg = nc.dram_tensor("dbg_lg", [128, 8 * 36], F32, kind="ExternalOutput").ap()
        dbg_g = nc.dram_tensor("dbg_g", [128, 8 * 32], F32, kind="ExternalOutput").ap()
        dbg_pos = nc.dram_tensor("dbg_pos", [128, 8 * 32], F32, kind="ExternalOutput").ap()
        dbg_x2 = nc.dram_tensor("dbg_x2", [128, 8 * D], F32, kind="ExternalOutput").ap()
    Bdbg = Buf("dbg")

    with contextlib.ExitStack() as top:
        fw = FW(nc, top)

        def SB(st, name, shape, dt):
            return st.enter_context(nc.sbuf_tensor("sb_" + name, shape, dt))

        def PS(st, name, shape, dt):
            return st.enter_context(nc.psum_tensor("ps_" + name, shape, dt))

        ident = SB(top, "ident", [128, 128], BF16); Bident = Buf("ident")
        identf = SB(top, "identf", [128, 128], F32); Bidentf = Buf("identf")
        negU = SB(top, "negU", [128, 128], BF16); BnegU = Buf("negU")
        negOnes = SB(top, "negOnes", [128, 128], BF16); BnegOnes = Buf("negOnes")
        ones = SB(top, "ones", [128, 128], BF16); Bones = Buf("ones")
        onesf = SB(top, "onesf", [128, 128], F32); Bonesf = Buf("onesf")
        stri = SB(top, "stri", [128, 128], BF16); Bstri = Buf("stri")
        kpos = SB(top, "kpos", [128, NB], F32); Bkpos = Buf("kpos")
        iota_s = SB(top, "iota_s", [128, 128], F32); Biota = Buf("iota")
        stA = contextlib.ExitStack()
        qpos = SB(stA, "qpos", [128, 1024], F32); Bqpos = Buf("qpos")
        y_aT = SB(stA, "y_aT", [128, 8, 1024], BF16); By_a = Buf("y_aT")
        y_bT = SB(stA, "y_bT", [128, 8, 1024], BF16); By_b = Buf("y_bT")
        BmTs = Buf("mTs")

        fw.op("pool", lambda e: e.memset(ones[:], 1.0), writes=[Bones])
        fw.op("pool", lambda e: e.memset(onesf[:], 1.0), writes=[Bonesf])
        fw.op("pool", lambda e: e.memset(negOnes[:], -1.0), writes=[BnegOnes])
        fw.op("pool", lambda e: e.affine_select(out=ident[:], in_=ones[:], pattern=[[-1, 128]],
                                                compare_op=ALU.is_equal, fill=0.0, base=0, channel_multiplier=1),
              reads=[Bones], writes=[Bident])
        fw.op("pool", lambda e: e.affine_select(out=identf[:], in_=onesf[:], pattern=[[-1, 128]],
                                                compare_op=ALU.is_equal, fill=0.0, base=0, channel_multiplier=1),
              reads=[Bonesf], writes=[Bidentf])
        fw.op("pool", lambda e: e.affine_select(out=negU[:], in_=negOnes[:], pattern=[[-1, 128]],
                                                compare_op=ALU.is_ge, fill=0.0, base=0, channel_multiplier=1),
              reads=[BnegOnes], writes=[BnegU])
        fw.op("pool", lambda e: e.affine_select(out=stri[:], in_=ones[:], pattern=[[1, 128]],
                                                compare_op=ALU.is_gt, fill=0.0, base=0, channel_multiplier=-1),
              reads=[Bones], writes=[Bstri])
        fw.op("pool", lambda e: e.iota(kpos[:], pattern=[[128, NB]], base=0, channel_multiplier=1,
                                       allow_small_or_imprecise_dtypes=True), writes=[Bkpos])
        fw.op("pool", lambda e: e.iota(iota_s[:], pattern=[[1, 128]], base=0, channel_multiplier=0,
                                       allow_small_or_imprecise_dtypes=True), writes=[Biota])
        fw.dma("sp", [(qpos[:], qpos_d[0:1, :].partition_broadcast(128))], Bqpos)

        BhTs = Buf("hTs")
        BhTo = Buf("hTo")
        stW = contextlib.ExitStack()
        Wu = SB(stW, "Wu", [128, NDC, 1024], BF16); BWu = Buf("Wu")
        Wv = SB(stW, "Wv", [128, NDC, 1024], BF16); BWv = Buf("Wv")
        w_in_v = w_in.rearrange("(c p) f -> p c f", p=128)
        for half in range(2):
            fw.dma("pool", [(Wu[:, half * 8:(half + 1) * 8, :], w_in_v[:, half * 8:(half + 1) * 8, 0:1024])], BWu)
            fw.dma("pool", [(Wv[:, half * 8:(half + 1) * 8, :], w_in_v[:, half * 8:(half + 1) * 8, 1024:2048])], BWv)
        with contextlib.ExitStack() as st:
            gm = SB(st, "gm", [128, D], F32); Bgm = Buf("gm")
            fw.dma("sp", [(gm[:], g_mix[0:1, :].partition_broadcast(128))], Bgm)
            xt = [SB(st, f"xt{i}", [128, D], F32) for i in range(4)]
            Bxt = [Buf(f"xt{i}") for i in range(4)]
            junk = SB(st, "junk", [128, D], BF16); Bjunk = Buf("junk")
            stt = [SB(st, f"stt{i}", [128, 4], F32) for i in range(4)]
            Bstt = [Buf(f"stt{i}") for i in range(4)]
            hb = [SB(st, f"hb{i}", [128, D], BF16) for i in range(3)]
            Bhb = [Buf(f"hb{i}") for i in range(3)]
            hT4 = [SB(st, f"hT4{i}", [128, NDC, 512], BF16) for i in range(2)]
            BhT4 = [Buf(f"hT4{i}") for i in range(2)]
            pt = [PS(st, f"pt{i}", [128, D], BF16) for i in range(2)]
            Bpt = [Buf(f"pt{i}") for i in range(2)]
            blocks = [(tile, blk) for tile in range(NT + 2) for blk in range(4)]

            def p1_s1(bi):
                tile, blk = blocks[bi]
                own = tile >= NT
                src = x_own if own else x_all
                t_loc = tile - NT if own else tile
                r0 = (t_loc * 4 + blk) * 128
                X = xt[bi % 4]; BX = Bxt[bi % 4]
                sT = stt[bi % 4]; BsT = Bstt[bi % 4]
                H = hb[bi % 3]; BH = Bhb[bi % 3]
                fw.dma("sp", [(X[:], src[r0:r0 + 128, :])], BX)
                fw.op("act", lambda e: e.activation(out=junk[:], in_=X[:], func=AF.Square, accum_out=sT[:, 0:1]),
                      reads=[BX], writes=[Bjunk, BsT])
                fw.op("act", lambda e: e.activation(out=sT[:, 1:2], in_=sT[:, 0:1], func=AF.Ln, scale=1.0 / D, bias=EPS),
                      reads=[BsT], writes=[BsT])
                fw.op("act", lambda e: e.activation(out=sT[:, 2:3], in_=sT[:, 1:2], func=AF.Exp, scale=-0.5),
                      reads=[BsT], writes=[BsT])
                fw.op("dve", lambda e: e.scalar_tensor_tensor(out=H[:], in0=X[:], scalar=sT[:, 2:3], in1=gm[:],
                                                              op0=ALU.mult, op1=ALU.mult),
                      reads=[BX, BsT, Bgm], writes=[BH])

            def p1_s2(bi):
                tile, blk = blocks[bi]
                own = tile >= NT
                t_loc = tile - NT if own else tile
                h4 = hT4[tile % 2]; Bh4 = BhT4[tile % 2]
                H = hb[bi % 3]; BH = Bhb[bi % 3]
                P = pt[bi % 2]; BP = Bpt[bi % 2]
                fw.op("pe", [(lambda e, dc=dc: e.transpose(out=P[:, dc * 128:(dc + 1) * 128],
                                                            in_=H[:, dc * 128:(dc + 1) * 128], identity=ident[:]))
                             for dc in range(NDC)], reads=[BH, Bident], writes=[BP])
                if bi % 2 == 0:
                    fw.op("act", lambda e: e.copy(out=h4[:, :, blk * 128:(blk + 1) * 128],
                                                  in_=P[:].rearrange("p (c t) -> p c t", c=NDC)),
                          reads=[BP], writes=[Bh4])
                else:
                    fw.op("dve", lambda e: e.tensor_copy(out=h4[:, :, blk * 128:(blk + 1) * 128],
                                                         in_=P[:].rearrange("p (c t) -> p c t", c=NDC)),
                          reads=[BP], writes=[Bh4])
                if blk == 3:
                    dstd = hTo if own else hTs
                    fw.dma("pool", [(dstd[t_loc * 128:(t_loc + 1) * 128, :], h4[:].rearrange("p c t -> p (c t)"))],
                           BhTo if own else BhTs, reads=[Bh4])

            p1_s1(0)
            for bi in range(len(blocks)):
                if bi + 1 < len(blocks):
                    p1_s1(bi + 1)
                p1_s2(bi)
            fw.barrier()

        with contextlib.ExitStack() as st:
            lg_t = SB(st, "lg_t", [128, 1024], F32); Blg = Buf("lng")
            lb_t = SB(st, "lb_t", [128, 1024], F32); Blb = Buf("lnb")
            fw.dma("sp", [(lg_t[:], ln_g[0:1, :].partition_broadcast(128))], Blg)
            fw.dma("sp", [(lb_t[:], ln_b[0:1, :].partition_broadcast(128))], Blb)
            bsr_f = SB(st, "bsr_f", [1, 1024], F32); Bbsrf = Buf("bsrf")
            bsr = SB(st, "bsr", [1, 1024], BF16); Bbsr = Buf("bsr")
            fw.dma("sp", [(bsr_f[:], b_sp[0:1, :])], Bbsrf)
            fw.op("dve", lambda e: e.tensor_copy(out=bsr[:], in_=bsr_f[:]), reads=[Bbsrf], writes=[Bbsr])
            wsl = SB(st, "wsl", [128, 8, 128], F32); Bwsl = Buf("wsl")
            fw.dma("sp", [(wsl[:], w_sp.rearrange("g t s -> t g s"))], Bwsl)
            wsT = SB(st, "wsT", [128, 8, 128], BF16); BwsT = Buf("wsT")
            wsTf = SB(st, "wsTf", [128, 8, 128], F32); BwsTf = Buf("wsTf")
            pw = PS(st, "pw", [128, 1024], F32); Bpw = Buf("pw")
            fw.op("pe", [(lambda e, g=g: e.transpose(out=pw[:, g * 128:(g + 1) * 128], in_=wsl[:, g, :], identity=identf[:]))
                         for g in range(8)], reads=[Bwsl, Bidentf], writes=[Bpw])
            fw.op("dve", lambda e: e.tensor_copy(out=wsTf[:].rearrange("p g t -> p (g t)"), in_=pw[:]), reads=[Bpw], writes=[BwsTf])
            for g in range(8):
                fw.op("pool", lambda e: e.affine_select(out=wsT[:, g, :], in_=wsTf[:, g, :], pattern=[[1, 128]],
                                                        compare_op=ALU.is_ge, fill=0.0, base=0, channel_multiplier=-1),
                      reads=[BwsTf], writes=[BwsT])
            hto = SB(st, "hto", [128, NDC, 512], BF16); Bhto = Buf("hto")
            guT = SB(st, "guT", [128, 8, 512], BF16); BguT = Buf("guT")
            pu = [PS(st, f"pu{i}", [128, 512], F32) for i in range(2)]
            Bpu = [Buf(f"pu{i}") for i in range(2)]
            pv = PS(st, "pv", [128, 1024], F32); Bpv = Buf("pv")
            pm = PS(st, "pm", [128, 1024], F32); Bpm = Buf("pm")
            t1 = SB(st, "t1", [128, 1024], F32); Bt1 = Buf("t1")
            t2 = SB(st, "t2", [128, 1024], F32); Bt2 = Buf("t2")
            gv = SB(st, "gv", [128, 1024], F32); Bgv = Buf("gv")
            vn = SB(st, "vn", [128, 1024], BF16); Bvn = Buf("vn")
            u1 = SB(st, "u1", [128, 512], F32); Bu1 = Buf("u1")
            u2 = SB(st, "u2", [128, 512], F32); Bu2 = Buf("u2")
            sv = SB(st, "sv", [128, 8], F32); Bsv = Buf("sv")
            GC = 1.5957691216057308

            def gelu_ops(dst_ap, src_ap, a_ap, b_ap, reads, Ba, Bb, Bdst):
                fw.op("act", lambda e: e.copy(out=b_ap, in_=src_ap), reads=reads, writes=[Bb])
                fw.op("dve", lambda e: e.tensor_tensor(out=a_ap, in0=b_ap, in1=b_ap, op=ALU.mult), reads=[Bb], writes=[Ba])
                fw.op("dve", lambda e: e.tensor_scalar(out=a_ap, in0=a_ap, scalar1=0.044715, scalar2=1.0, op0=ALU.mult, op1=ALU.add),
                      reads=[Ba], writes=[Ba])
                fw.op("dve", lambda e: e.tensor_tensor(out=a_ap, in0=a_ap, in1=b_ap, op=ALU.mult), reads=[Ba, Bb], writes=[Ba])
                fw.op("act", lambda e: e.activation(out=a_ap, in_=a_ap, func=AF.Sigmoid, scale=GC), reads=[Ba], writes=[Ba])
                fw.op("dve", lambda e: e.tensor_tensor(out=dst_ap, in0=a_ap, in1=b_ap, op=ALU.mult), reads=[Ba, Bb], writes=[Bdst])

            u1r = [u1, SB(st, "u1b", [128, 512], F32)]; Bu1r = [Bu1, Buf("u1b")]
            u2r = [u2, SB(st, "u2b", [128, 512], F32)]; Bu2r = [Bu2, Buf("u2b")]
            t1r = [t1, SB(st, "t1b", [128, 1024], F32)]; Bt1r = [Bt1, Buf("t1b")]
            t2r = [t2, SB(st, "t2b", [128, 1024], F32)]; Bt2r = [Bt2, Buf("t2b")]
            pvr = [pv, pw]; Bpvr = [Bpv, Bpw]

            def gelu_s1(a_ap, b_ap, src_ap, Ba, Bb, Bsrc):
                fw.op("act", lambda e: e.copy(out=b_ap, in_=src_ap), reads=[Bsrc], writes=[Bb])
                fw.op("dve", lambda e: e.tensor_tensor(out=a_ap, in0=b_ap, in1=b_ap, op=ALU.mult), reads=[Bb], writes=[Ba])
                fw.op("dve", lambda e: e.tensor_scalar(out=a_ap, in0=a_ap, scalar1=0.044715, scalar2=1.0, op0=ALU.mult, op1=ALU.add),
                      reads=[Ba], writes=[Ba])
                fw.op("dve", lambda e: e.tensor_tensor(out=a_ap, in0=a_ap, in1=b_ap, op=ALU.mult), reads=[Ba, Bb], writes=[Ba])

            def gelu_s2(dst_ap, a_ap, b_ap, Ba, Bb, Bdst):
                fw.op("act", lambda e: e.activation(out=a_ap, in_=a_ap, func=AF.Sigmoid, scale=GC), reads=[Ba], writes=[Ba])
                fw.op("dve", lambda e: e.tensor_tensor(out=dst_ap, in0=a_ap, in1=b_ap, op=ALU.mult), reads=[Ba, Bb], writes=[Bdst])

            ucnt = {"u": 0, "v": 0}
            for tl in range(2):
                fw.dma("sp", [(hto[:].rearrange("p c t -> p (c t)"), hTo[tl * 128:(tl + 1) * 128, :])], Bhto, reads=[BhTo])

                def u_s1(cc):
                    k = (ucnt["u"] + cc) % 2
                    P = pu[k]; BP = Bpu[k]
                    fw.op("pe", [(lambda e, dc=dc: e.matmul(P[:], lhsT=Wu[:, dc, cc * 128:(cc + 1) * 128], rhs=hto[:, dc, :],
                                                            start=(dc == 0), stop=(dc == NDC - 1))) for dc in range(NDC)],
                          reads=[BWu, Bhto], writes=[BP])
                    gelu_s1(u1r[k][:], u2r[k][:], P[:], Bu1r[k], Bu2r[k], BP)

                def u_s2(cc):
                    k = (ucnt["u"] + cc) % 2
                    gelu_s2(guT[:, cc, :], u1r[k][:], u2r[k][:], Bu1r[k], Bu2r[k], BguT)

                u_s1(0)
                for cc in range(8):
                    if cc + 1 < 8:
                        u_s1(cc + 1)
                    u_s2(cc)
                ucnt["u"] += 8

                def v_s1(blk):
                    k = (ucnt["v"] + blk) % 2
                    PV = pvr[k]; BPV = Bpvr[k]
                    fw.op("pe", [(lambda e, dc=dc, hf=hf: e.matmul(PV[:, hf * 512:(hf + 1) * 512], lhsT=hto[:, dc, blk * 128:(blk + 1) * 128],
                                                                  rhs=Wv[:, dc, hf * 512:(hf + 1) * 512],
                                                                  start=(dc == 0), stop=(dc == NDC - 1)))
                                 for hf in range(2) for dc in range(NDC)], reads=[BWv, Bhto], writes=[BPV])
                    gelu_s1(t1r[k][:], t2r[k][:], PV[:], Bt1r[k], Bt2r[k], BPV)

                def v_s2(blk):
                    k = (ucnt["v"] + blk) % 2
                    tb = tl * 4 + blk
                    T1 = t1r[k]; T2 = t2r[k]; BT1 = Bt1r[k]; BT2 = Bt2r[k]
                    gelu_s2(gv[:], T1[:], T2[:], BT1, BT2, Bgv)
                    fw.op("act", lambda e: e.activation(out=T2[:], in_=gv[:], func=AF.Copy, accum_out=sv[:, 0:1]), reads=[Bgv], writes=[BT2, Bsv])
                    fw.op("act", lambda e: e.activation(out=T2[:], in_=gv[:], func=AF.Square, accum_out=sv[:, 1:2]), reads=[Bgv], writes=[BT2, Bsv])
                    fw.op("dve", lambda e: e.tensor_scalar(out=sv[:, 2:3], in0=sv[:, 0:1], scalar1=1.0 / 1024, scalar2=None, op0=ALU.mult), reads=[Bsv], writes=[Bsv])
                    fw.op("dve", lambda e: e.tensor_tensor(out=sv[:, 3:4], in0=sv[:, 2:3], in1=sv[:, 2:3], op=ALU.mult), reads=[Bsv], writes=[Bsv])
                    fw.op("dve", lambda e: e.scalar_tensor_tensor(out=sv[:, 4:5], in0=sv[:, 1:2], scalar=1.0 / 1024, in1=sv[:, 3:4],
                                                                  op0=ALU.mult, op1=ALU.subtract), reads=[Bsv], writes=[Bsv])
                    fw.op("act", lambda e: e.activation(out=sv[:, 5:6], in_=sv[:, 4:5], func=AF.Ln, bias=EPS), reads=[Bsv], writes=[Bsv])
                    fw.op("act", lambda e: e.activation(out=sv[:, 6:7], in_=sv[:, 5:6], func=AF.Exp, scale=-0.5), reads=[Bsv], writes=[Bsv])
                    fw.op("dve", lambda e: e.tensor_scalar(out=T1[:], in0=gv[:], scalar1=sv[:, 2:3], scalar2=sv[:, 6:7],
                                                           op0=ALU.subtract, op1=ALU.mult), reads=[Bgv, Bsv], writes=[BT1])
                    fw.op("dve", lambda e: e.tensor_tensor(out=T1[:], in0=T1[:], in1=lg_t[:], op=ALU.mult), reads=[Blg], writes=[BT1])
                    fw.op("dve", lambda e: e.tensor_tensor(out=vn[:], in0=T1[:], in1=lb_t[:], op=ALU.add), reads=[BT1, Blb], writes=[Bvn])
                    fns = []
                    for g in range(8):
                        fns.append(lambda e, g=g: e.matmul(pm[:, g * 128:(g + 1) * 128], lhsT=vn[:, g * 128:(g + 1) * 128],
                                                           rhs=wsT[:, g, :], start=True, stop=False))
                        fns.append(lambda e, g=g: e.matmul(pm[:, g * 128:(g + 1) * 128], lhsT=ones[0:1, :],
                                                           rhs=bsr[0:1, g * 128:(g + 1) * 128], start=False, stop=True))
                    fw.op("pe", fns, reads=[Bvn, BwsT, Bones, Bbsr], writes=[Bpm])
                    fw.op("dve", lambda e: e.tensor_tensor(out=y_aT[:, :, tb * 128:(tb + 1) * 128],
                                                           in0=pm[:].rearrange("p (g t) -> p g t", g=8),
                                                           in1=guT[:, :, blk * 128:(blk + 1) * 128], op=ALU.mult),
                          reads=[Bpm, BguT], writes=[By_a])

                v_s1(0)
                for blk in range(4):
                    if blk + 1 < 4:
                        v_s1(blk + 1)
                    v_s2(blk)
                ucnt["v"] += 4
            fw.barrier()
        stW.close()

        with contextlib.ExitStack() as st:
            KT = [SB(st, f"KT{i}", [128, S], BF16) for i in range(2)]; BKT = [Buf(f"KT{i}") for i in range(2)]
            V = [SB(st, f"V{i}", [128, NB, 128], BF16) for i in range(2)]; BV = [Buf(f"V{i}") for i in range(2)]
            QT = [SB(st, f"QT{i}", [128, 1024], BF16) for i in range(2)]; BQT = [Buf(f"QT{i}") for i in range(2)]
            Wq = [SB(st, f"Wq{i}", [128, NDC, 128], BF16) for i in range(2)]; BWq = [Buf(f"Wq{i}") for i in range(2)]
            Wk = [SB(st, f"Wk{i}", [128, NDC, 128], BF16) for i in range(2)]; BWk = [Buf(f"Wk{i}") for i in range(2)]
            Wvh = [SB(st, f"Wvh{i}", [128, NDC, 128], BF16) for i in range(2)]; BWvh = [Buf(f"Wvh{i}") for i in range(2)]
            htl = [SB(st, f"htl{i}", [128, NDC, 512], BF16) for i in range(2)]
            Bhtl = [Buf(f"htl{i}") for i in range(2)]
            Et = [SB(st, f"Et{i}", [128, 512], F32) for i in range(4)]; BEt = [Buf(f"Et{i}") for i in range(4)]
            Xt = [SB(st, f"Xt{i}", [128, 512], F32) for i in range(2)]; BXt = [Buf(f"Xt{i}") for i in range(2)]
            Lt = [SB(st, f"Lt{i}", [128, 512], BF16) for i in range(4)]; BLt = [Buf(f"Lt{i}") for i in range(4)]
            Rt = [SB(st, f"Rt{i}", [128, 512], BF16) for i in range(5)]; BRt = [Buf(f"Rt{i}") for i in range(5)]
            Wt = [SB(st, f"Wt{i}", [128, 512], BF16) for i in range(3)]; BWt = [Buf(f"Wt{i}") for i in range(3)]
            Mt = [SB(st, f"Mt{i}", [128, 512], BF16) for i in range(4)]; BMt = [Buf(f"Mt{i}") for i in range(4)]
            pz = [PS(st, f"pz{i}", [128, 512], F32) for i in range(2)]; Bpz = [Buf(f"pz{i}") for i in range(2)]
            pS = [PS(st, f"pS{i}", [128, 512], F32) for i in range(2)]; BpS = [Buf(f"pS{i}") for i in range(2)]
            pacc = PS(st, "pacc", [128, 512], F32); Bpacc = Buf("pacc")
            pk = [PS(st, f"pk{i}", [128, 512], F32) for i in range(2)]; Bpk = [Buf(f"pk{i}") for i in range(2)]
            ppv = PS(st, "ppv", [128, 512], F32); Bppv = Buf("ppv")
            w_in_v = w_in.rearrange("(c p) f -> p c f", p=128)
            cnt = {"ht": 0, "pk": 0}

            def load_w(hd):
                h2_ = hd % 2
                for (Wt_, BWt_, off) in ((Wq[h2_], BWq[h2_], 2048), (Wk[h2_], BWk[h2_], 3072), (Wvh[h2_], BWvh[h2_], 4096)):
                    fw.dma("pool", [(Wt_[:, q * 4:(q + 1) * 4, :], w_in_v[:, q * 4:(q + 1) * 4, off + hd * 128:off + (hd + 1) * 128])
                                    for q in range(4)], BWt_)

            def proj_tile_pieces(hd, tile):
                h2_ = hd % 2
                stt_ = {}

                def p0():
                    HT = htl[cnt["ht"] % 2]; BHT = Bhtl[cnt["ht"] % 2]; cnt["ht"] += 1
                    P = pk[cnt["pk"] % 2]; BP = Bpk[cnt["pk"] % 2]; cnt["pk"] += 1
                    stt_.update(HT=HT, BHT=BHT, P=P, BP=BP)
                    fw.dma("sp", [(HT[:].rearrange("p c t -> p (c t)"), hTs[tile * 128:(tile + 1) * 128, :])], BHT, reads=[BhTs])
                    fw.op("pe", [(lambda e, dc=dc: e.matmul(P[:], lhsT=Wk[h2_][:, dc, :], rhs=HT[:, dc, :],
                                                            start=(dc == 0), stop=False)) for dc in range(NDC // 2)],
                          reads=[BWk[h2_], BHT], writes=[BP])

                def p1():
                    HT, BHT, P, BP = stt_["HT"], stt_["BHT"], stt_["P"], stt_["BP"]
                    fw.op("pe", [(lambda e, dc=dc: e.matmul(P[:], lhsT=Wk[h2_][:, dc, :], rhs=HT[:, dc, :],
                                                            start=False, stop=(dc == NDC - 1))) for dc in range(NDC // 2, NDC)],
                          reads=[BWk[h2_], BHT], writes=[BP])
                    fw.op("act", lambda e: e.copy(out=KT[h2_][:, tile * 512:(tile + 1) * 512], in_=P[:]), reads=[BP], writes=[BKT[h2_]])

                def pv_(b):
                    def f():
                        HT, BHT = stt_["HT"], stt_["BHT"]
                        fw.op("pe", [(lambda e, dc=dc: e.matmul(ppv[:, b * 128:(b + 1) * 128], lhsT=HT[:, dc, b * 128:(b + 1) * 128],
                                                                rhs=Wvh[h2_][:, dc, :], start=(dc == 0), stop=(dc == NDC - 1)))
                                     for dc in range(NDC)], reads=[BWvh[h2_], BHT], writes=[Bppv])
                        if b == 3:
                            fw.op("dve", lambda e: e.tensor_copy(out=V[h2_][:, tile * 4:(tile + 1) * 4, :].rearrange("p b d -> p (b d)"), in_=ppv[:]),
                                  reads=[Bppv], writes=[BV[h2_]])
                    return f
                return [p0, p1, pv_(0), pv_(1), pv_(2), pv_(3)]

            def proj_q_pieces(hd, tl):
                h2_ = hd % 2
                stt_ = {}

                def p0():
                    HT = htl[cnt["ht"] % 2]; BHT = Bhtl[cnt["ht"] % 2]; cnt["ht"] += 1
                    P = pk[cnt["pk"] % 2]; BP = Bpk[cnt["pk"] % 2]; cnt["pk"] += 1
                    stt_.update(HT=HT, BHT=BHT, P=P, BP=BP)
                    fw.dma("sp", [(HT[:].rearrange("p c t -> p (c t)"), hTo[tl * 128:(tl + 1) * 128, :])], BHT, reads=[BhTo])
                    fw.op("pe", [(lambda e, dc=dc: e.matmul(P[:], lhsT=Wq[h2_][:, dc, :], rhs=HT[:, dc, :],
                                                            start=(dc == 0), stop=False)) for dc in range(NDC // 2)],
                          reads=[BWq[h2_], BHT], writes=[BP])

                def p1():
                    HT, BHT, P, BP = stt_["HT"], stt_["BHT"], stt_["P"], stt_["BP"]
                    fw.op("pe", [(lambda e, dc=dc: e.matmul(P[:], lhsT=Wq[h2_][:, dc, :], rhs=HT[:, dc, :],
                                                            start=False, stop=(dc == NDC - 1))) for dc in range(NDC // 2, NDC)],
                          reads=[BWq[h2_], BHT], writes=[BP])
                    fw.op("act", lambda e: e.mul(out=QT[h2_][:, tl * 512:(tl + 1) * 512], in_=P[:], mul=SCALE), reads=[BP], writes=[BQT[h2_]])
                return [p0, p1]

            def proj_jobs(hd):
                jobs = []
                for t in range(NT):
                    jobs += proj_tile_pieces(hd, t)
                for t in range(2):
                    jobs += proj_q_pieces(hd, t)
                return jobs

            units = []
            for tl in range(2):
                nkb = NB // 2 if tl == 0 else NB
                for un in range(nkb):
                    l = nkb - 1 - un
                    units.append((tl, l, (tl == 0) or (l >= NB // 2), un == 0, un == nkb - 1))
            NU = len(units)
            gbase = 0
            load_w(0)
            for j in proj_jobs(0):
                j()
            for hd in range(8):
                h2_ = hd % 2
                jobs = []
                if hd + 1 < 8:
                    load_w(hd + 1)
                    jobs = proj_jobs(hd + 1)
                nj = len(jobs)
                jdone = 0

                def sgM(i):
                    tl, l, masked, first, last = units[i]
                    gi = gbase + i
                    qs = slice(tl * 512, (tl + 1) * 512)
                    M = Mt[gi % 4]; BM = BMt[gi % 4]
                    if masked:
                        fw.op("dve", lambda e: e.tensor_scalar(out=M[:], in0=qpos[:, qs], scalar1=kpos[:, l:l + 1], scalar2=NEG,
                                                               op0=ALU.is_le, op1=ALU.mult), reads=[Bqpos, Bkpos], writes=[BM])

                def sgA(i):
                    tl, l, masked, first, last = units[i]
                    gi = gbase + i
                    qs = slice(tl * 512, (tl + 1) * 512); ks = slice(l * 128, (l + 1) * 128)
                    Z = pz[gi % 2]; BZ = Bpz[gi % 2]; E = Et[gi % 4]; BE = BEt[gi % 4]; L = Lt[gi % 4]; BL = BLt[gi % 4]
                    M = Mt[gi % 4]; BM = BMt[gi % 4]; Rp = Rt[gi % 5]; BRp = BRt[gi % 5]; Rn = Rt[(gi + 1) % 5]; BRn = BRt[(gi + 1) % 5]
                    if first:
                        fw.op("pool", lambda e: e.memset(Rp[:], 0.0), writes=[BRp])
                    fz = [lambda e: e.matmul(Z[:], lhsT=KT[h2_][:, ks], rhs=QT[h2_][:, qs], start=True, stop=not masked)]
                    if masked:
                        fz.append(lambda e: e.matmul(Z[:], lhsT=ident[:], rhs=M[:], start=False, stop=True))
                    fw.op("pe", fz, reads=[BKT[h2_], BQT[h2_], Bident] + ([BM] if masked else []), writes=[BZ])
                    fw.op("act", lambda e: e.activation(out=E[:], in_=Z[:], func=AF.Exp), reads=[BZ], writes=[BE])
                    fw.op("act", lambda e: e.activation(out=L[:], in_=E[:], func=AF.Ln, bias=1.0), reads=[BE], writes=[BL])
                    if not last:
                        fw.op("pool", lambda e: e.tensor_tensor(out=Rn[:], in0=Rp[:], in1=L[:], op=ALU.add), reads=[BRp, BL], writes=[BRn])

                def sgB(i):
                    tl, l, masked, first, last = units[i]
                    gi = gbase + i
                    E = Et[gi % 4]; BE = BEt[gi % 4]; L = Lt[gi % 4]; BL = BLt[gi % 4]; Rp = Rt[gi % 5]; BRp = BRt[gi % 5]
                    Sp = pS[gi % 2]; BSp = BpS[gi % 2]; X = Xt[gi % 2]; BX = BXt[gi % 2]; Wm = Wt[gi % 3]; BWm = BWt[gi % 3]
                    fs = []
                    if not first:
                        fs.append(lambda e: e.matmul(Sp[:], lhsT=negOnes[:], rhs=Rp[:], start=True, stop=False))
                    fs.append(lambda e: e.matmul(Sp[:], lhsT=negU[:], rhs=L[:], start=first, stop=True))
                    fw.op("pe", fs, reads=[BnegOnes, BnegU, BL] + ([] if first else [BRp]), writes=[BSp])
                    fw.op("act", lambda e: e.activation(out=X[:], in_=Sp[:], func=AF.Exp), reads=[BSp], writes=[BX])
                    fw.op("dve", lambda e: e.tensor_tensor(out=Wm[:], in0=E[:], in1=X[:], op=ALU.mult), reads=[BE, BX], writes=[BWm])

                def sgC(i):
                    tl, l, masked, first, last = units[i]
                    gi = gbase + i
                    qs = slice(tl * 512, (tl + 1) * 512)
                    Wm = Wt[gi % 3]; BWm = BWt[gi % 3]
                    fw.op("pe", lambda e: e.matmul(pacc[:], lhsT=V[h2_][:, l, :], rhs=Wm[:], start=first, stop=last),
                          reads=[BV[h2_], BWm], writes=[Bpacc])
                    if last:
                        fw.op("dve", lambda e: e.tensor_copy(out=y_bT[:, hd, qs], in_=pacc[:]), reads=[Bpacc], writes=[By_b])

                sgM(0)
                sgM(1)
                for step in range(NU + 3):
                    if step + 2 < NU:
                        sgM(step + 2)
                    if step < NU:
                        sgA(step)
                    if 0 <= step - 2 < NU:
                        sgB(step - 2)
                    if 0 <= step - 3 < NU:
                        sgC(step - 3)
                    want = min(nj, ((step + 1) * nj + NU - 1) // NU) if nj else 0
                    while jdone < want:
                        jobs[jdone]()
                        jdone += 1
                while jdone < nj:
                    jobs[jdone]()
                    jdone += 1
                gbase += NU
            fw.barrier()

        with contextlib.ExitStack() as st:
            mT = SB(st, "mT", [128, NDC, 1024], BF16); BmT = Buf("mT")
            hto2 = SB(st, "hto2", [128, 2, NDC, 512], BF16); Bhto2 = Buf("hto2")
            for tl in range(2):
                fw.dma("sp", [(hto2[:, tl, :, :].rearrange("p c t -> p (c t)"), hTo[tl * 128:(tl + 1) * 128, :])], Bhto2, reads=[BhTo])
            wga = [SB(st, f"wga{i}", [128, NDC, 128], BF16) for i in range(2)]; Bwga = [Buf(f"wga{i}") for i in range(2)]
            wgb = [SB(st, f"wgb{i}", [128, NDC, 128], BF16) for i in range(2)]; Bwgb = [Buf(f"wgb{i}") for i in range(2)]
            wa = [SB(st, f"wa{i}", [128, 8, 128], BF16) for i in range(2)]; Bwa = [Buf(f"wa{i}") for i in range(2)]
            wb = [SB(st, f"wb{i}", [128, 8, 128], BF16) for i in range(2)]; Bwb = [Buf(f"wb{i}") for i in range(2)]
            sga = SB(st, "sga", [128, 512], F32); Bsga = Buf("sga")
            sgb = SB(st, "sgb", [128, 512], F32); Bsgb = Buf("sgb")
            ta = SB(st, "ta", [128, 512], F32); Bta = Buf("ta")
            tb_ = SB(st, "tb_", [128, 512], F32); Btb = Buf("tb_")
            pga = PS(st, "pga", [128, 512], F32); Bpga = Buf("pga")
            pgb = PS(st, "pgb", [128, 512], F32); Bpgb = Buf("pgb")
            pA = PS(st, "pA", [128, 512], F32); BpA = Buf("pA")
            pB = PS(st, "pB", [128, 512], F32); BpB = Buf("pB")
            w_in_v = w_in.rearrange("(c p) f -> p c f", p=128)
            w_ba_v = w_ba.rearrange("(c p) f -> p c f", p=128)
            w_bb_v = w_bb.rearrange("(c p) f -> p c f", p=128)
            for j in range(NDC):
                i2 = j % 2
                js = slice(j * 128, (j + 1) * 128)
                fw.dma("pool", [(wga[i2][:, q * 4:(q + 1) * 4, :], w_in_v[:, q * 4:(q + 1) * 4, 5120 + j * 128:5120 + (j + 1) * 128]) for q in range(4)], Bwga[i2])
                fw.dma("pool", [(wgb[i2][:, q * 4:(q + 1) * 4, :], w_in_v[:, q * 4:(q + 1) * 4, 7168 + j * 128:7168 + (j + 1) * 128]) for q in range(4)], Bwgb[i2])
                fw.dma("pool", [(wa[i2][:, q * 4:(q + 1) * 4, :], w_ba_v[:, q * 4:(q + 1) * 4, js]) for q in range(2)], Bwa[i2])
                fw.dma("pool", [(wb[i2][:, q * 4:(q + 1) * 4, :], w_bb_v[:, q * 4:(q + 1) * 4, js]) for q in range(2)], Bwb[i2])
                for tl in range(2):
                    qs = slice(tl * 512, (tl + 1) * 512)
                    fw.op("pe", [(lambda e, dc=dc: e.matmul(pga[:], lhsT=wga[i2][:, dc, :], rhs=hto2[:, tl, dc, :],
                                                            start=(dc == 0), stop=(dc == NDC - 1))) for dc in range(NDC)],
                          reads=[Bwga[i2], Bhto2], writes=[Bpga])
                    fw.op("act", lambda e: e.activation(out=sga[:], in_=pga[:], func=AF.Sigmoid), reads=[Bpga], writes=[Bsga])
                    fw.op("pe", [(lambda e, dc=dc: e.matmul(pgb[:], lhsT=wgb[i2][:, dc, :], rhs=hto2[:, tl, dc, :],
                                                            start=(dc == 0), stop=(dc == NDC - 1))) for dc in range(NDC)],
                          reads=[Bwgb[i2], Bhto2], writes=[Bpgb])
                    fw.op("act", lambda e: e.activation(out=sgb[:], in_=pgb[:], func=AF.Sigmoid), reads=[Bpgb], writes=[Bsgb])
                    fw.op("pe", [(lambda e, cc=cc: e.matmul(pA[:], lhsT=wa[i2][:, cc, :], rhs=y_aT[:, cc, qs],
                                                            start=(cc == 0), stop=(cc == 7))) for cc in range(8)],
                          reads=[Bwa[i2], By_a], writes=[BpA])
                    fw.op("dve", lambda e: e.tensor_tensor(out=ta[:], in0=pA[:], in1=sga[:], op=ALU.mult), reads=[BpA, Bsga], writes=[Bta])
                    fw.op("pe", [(lambda e, cc=cc: e.matmul(pB[:], lhsT=wb[i2][:, cc, :], rhs=y_bT[:, cc, qs],
                                                            start=(cc == 0), stop=(cc == 7))) for cc in range(8)],
                          reads=[Bwb[i2], By_b], writes=[BpB])
                    fw.op("dve", lambda e: e.tensor_tensor(out=tb_[:], in0=pB[:], in1=sgb[:], op=ALU.mult), reads=[BpB, Bsgb], writes=[Btb])
                    fw.op("dve", lambda e: e.tensor_tensor(out=mT[:, j, qs], in0=ta[:], in1=tb_[:], op=ALU.add), reads=[Bta, Btb], writes=[BmT])
            fw.dma("sp", [(mTs[:, :], mT[:].rearrange("p c t -> p (c t)"))], BmTs, reads=[BmT])
            if debug:
                fw.dma("sp", [(dbg_ya[:, :], y_aT[:].rearrange("p c t -> p (c t)")), (dbg_yb[:, :], y_bT[:].rearrange("p c t -> p (c t)"))],
                       Bdbg, reads=[By_a, By_b])
            fw.barrier()
        stA.close()

        with contextlib.ExitStack() as st6:
            x1 = SB(st6, "x1", [128, 8, D], F32)
            Bx1 = [Buf(f"x1_{b}") for b in range(8)]
            h2 = SB(st6, "h2", [128, 8, D], BF16)
            Bh2 = [Buf(f"h2_{b}") for b in range(8)]
            LG = SB(st6, "LG", [128, 8, 36], F32); BLG = Buf("LG")
            for b in range(8):
                fw.dma("sp", [(x1[:, b, :], x_own[b * 128:(b + 1) * 128, :])], Bx1[b])

            with contextlib.ExitStack() as st:
                mT = SB(st, "mT2", [128, NDC, 1024], BF16); BmT = Buf("mT2")
                fw.dma("sp", [(mT[:].rearrange("p c t -> p (c t)"), mTs[:, :])], BmT, reads=[BmTs])
                wo = [SB(st, f"wo{i}", [128, NDC, 512], BF16) for i in range(2)]; Bwo = [Buf(f"wo{i}") for i in range(2)]
                po = [PS(st, f"po{i}", [128, 512], F32) for i in range(2)]; Bpo = [Buf(f"po{i}") for i in range(2)]
                w_out_v = w_out.rearrange("(c p) f -> p c f", p=128)
                k = 0
                for et in range(4):
                    es = slice(et * 512, (et + 1) * 512)
                    fw.dma("pool", [(wo[et % 2][:, q * 4:(q + 1) * 4, :], w_out_v[:, q * 4:(q + 1) * 4, es]) for q in range(4)], Bwo[et % 2])
                    for b in range(8):
                        P = po[k % 2]; BP = Bpo[k % 2]
                        fw.op("pe", [(lambda e, dc=dc: e.matmul(P[:], lhsT=mT[:, dc, b * 128:(b + 1) * 128], rhs=wo[et % 2][:, dc, :],
                                                                start=(dc == 0), stop=(dc == NDC - 1))) for dc in range(NDC)],
                              reads=[BmT, Bwo[et % 2]], writes=[BP])
                        fw.op("dve", lambda e: e.tensor_tensor(out=x1[:, b, es], in0=x1[:, b, es], in1=P[:], op=ALU.add),
                              reads=[BP], writes=[Bx1[b]])
                        k += 1
                fw.barrier()

            with contextlib.ExitStack() as st:
                gf = SB(st, "gf", [128, D], F32); Bgf = Buf("gf")
                fw.dma("sp", [(gf[:], g_ffn[0:1, :].partition_broadcast(128))], Bgf)
                wr = SB(st, "wr", [128, NDC, 36], F32); Bwr = Buf("wr")
                fw.dma("sp", [(wr[:, :, 0:4], w_rg.rearrange("(c p) f -> p c f", p=128)),
                              (wr[:, :, 4:36], w_re.rearrange("(c p) f -> p c f", p=128))], Bwr)
                brt = SB(st, "brt", [128, 36], F32); Bbrt = Buf("brt")
                fw.dma("sp", [(brt[:, 0:4], b_rg[0:1, :].partition_broadcast(128)),
                              (brt[:, 4:36], b_re[0:1, :].partition_broadcast(128))], Bbrt)
                junk2 = SB(st, "junk2", [128, D], BF16); Bj2 = Buf("junk2")
                h2f = [SB(st, f"h2f{i}", [128, D], F32) for i in range(2)]; Bh2f = [Buf(f"h2f{i}") for i in range(2)]
                h2T = [SB(st, f"h2T{i}", [128, NDC, 128], F32) for i in range(2)]; Bh2T = [Buf(f"h2T{i}") for i in range(2)]
                st2 = SB(st, "st2", [128, 8, 4], F32); Bst2 = Buf("st2")
                ptf = [PS(st, f"ptf{i}", [128, 512], F32) for i in range(4)]; Bptf = [Buf(f"ptf{i}") for i in range(4)]
                plg = PS(st, "plg", [128, 64], F32); Bplg = Buf("plg")
                for b in range(8):
                    fw.op("act", lambda e: e.activation(out=junk2[:], in_=x1[:, b, :], func=AF.Square, accum_out=st2[:, b, 0:1]),
                          reads=[Bx1[b]], writes=[Bj2, Bst2])
                    fw.op("act", lambda e: e.activation(out=st2[:, b, 1:2], in_=st2[:, b, 0:1], func=AF.Ln, scale=1.0 / D, bias=EPS), reads=[Bst2], writes=[Bst2])
                    fw.op("act", lambda e: e.activation(out=st2[:, b, 2:3], in_=st2[:, b, 1:2], func=AF.Exp, scale=-0.5), reads=[Bst2], writes=[Bst2])
                    HF = h2f[b % 2]; BHF = Bh2f[b % 2]
                    fw.op("dve", lambda e: e.scalar_tensor_tensor(out=HF[:], in0=x1[:, b, :], scalar=st2[:, b, 2:3], in1=gf[:],
                                                                  op0=ALU.mult, op1=ALU.mult), reads=[Bx1[b], Bst2, Bgf], writes=[BHF])
                    fw.op("pool", lambda e: e.tensor_copy(out=h2[:, b, :], in_=HF[:]), reads=[BHF], writes=[Bh2[b]])
                    HT2 = h2T[b % 2]; BHT2 = Bh2T[b % 2]
                    for q4 in range(4):
                        PT = ptf[q4]; BPT = Bptf[q4]
                        fw.op("pe", [(lambda e, i=i: e.transpose(out=PT[:, i * 128:(i + 1) * 128],
                                                                  in_=HF[:, (q4 * 4 + i) * 128:(q4 * 4 + i + 1) * 128], identity=identf[:]))
                                     for i in range(4)], reads=[BHF, Bidentf], writes=[BPT])
                        fw.op("act" if q4 % 2 == 0 else "dve",
                              (lambda e: e.copy(out=HT2[:, q4 * 4:(q4 + 1) * 4, :].rearrange("p c t -> p (c t)"), in_=PT[:])) if q4 % 2 == 0 else
                              (lambda e: e.tensor_copy(out=HT2[:, q4 * 4:(q4 + 1) * 4, :].rearrange("p c t -> p (c t)"), in_=PT[:])),
                              reads=[BPT], writes=[BHT2])
                    fw.op("pe", [(lambda e, dc=dc: e.matmul(plg[:, 0:36], lhsT=HT2[:, dc, :], rhs=wr[:, dc, :],
                                                            start=(dc == 0), stop=(dc == NDC - 1))) for dc in range(NDC)],
                          reads=[BHT2, Bwr], writes=[Bplg])
                    fw.op("dve", lambda e: e.tensor_tensor(out=LG[:, b, :], in0=plg[:, 0:36], in1=brt[:], op=ALU.add),
                          reads=[Bplg, Bbrt], writes=[BLG])
                if debug:
                    fw.dma("sp", [(dbg_x1[:, :], x1[:].rearrange("p b f -> p (b f)")), (dbg_lg[:, :], LG[:].rearrange("p b f -> p (b f)"))],
                           Bdbg, reads=Bx1 + [BLG])
                fw.barrier()

            with contextlib.ExitStack() as st:
                G = SB(st, "G", [128, 8, 32], F32); BG = Buf("G")
                MK = SB(st, "MK", [128, 8, 32], BF16); BMK = Buf("MK")
                MKf = SB(st, "MKf", [128, 8, 32], F32); BMKf = Buf("MKf")
                POS = SB(st, "POS", [128, 8, 32], F32); BPOS = Buf("POS")
                rs = SB(st, "rs", [128, 8, 64], F32); Brs = Buf("rs")
                pb = [PS(st, f"pb{i}", [128, 512], F32) for i in range(8)]; Bpb = [Buf(f"pb{i}") for i in range(8)]
                ppos = pb[0]; Bppos = Bpb[0]
                def route_ops(b):
                    seq = []
                    R_ = rs[:, b, :]
                    lgp = LG[:, b, 0:4]
                    lep = LG[:, b, 4:36]
                    def A(f, w=()):
                        seq.append(("dve", f, list(w)))
                    A(lambda e: e.reduce_max(out=R_[:, 0:1], in_=lgp, axis=AX.X))
                    A(lambda e: e.tensor_scalar(out=R_[:, 1:2], in0=R_[:, 0:1], scalar1=-1.0, scalar2=None, op0=ALU.mult))
                    A(lambda e: e.tensor_scalar(out=R_[:, 4:8], in0=lgp, scalar1=R_[:, 0:1], scalar2=None, op0=ALU.is_equal))
                    seq.append(("act", lambda e: e.activation(out=R_[:, 24:28], in_=lgp, func=AF.Exp, bias=R_[:, 1:2], accum_out=R_[:, 2:3]), []))
                    A(lambda e: e.reciprocal(out=R_[:, 3:4], in_=R_[:, 2:3]))
                    A(lambda e: e.tensor_scalar(out=R_[:, 8:16], in0=lep[:, 0:8], scalar1=R_[:, 4:5], scalar2=None, op0=ALU.mult))
                    for g in range(1, 4):
                        A(lambda e, g=g: e.scalar_tensor_tensor(out=R_[:, 8:16], in0=lep[:, g * 8:(g + 1) * 8], scalar=R_[:, 4 + g:5 + g],
                                                                in1=R_[:, 8:16], op0=ALU.mult, op1=ALU.add))
                    A(lambda e: e.reduce_max(out=R_[:, 16:17], in_=R_[:, 8:16], axis=AX.X))
                    A(lambda e: e.tensor_scalar(out=R_[:, 32:40], in0=R_[:, 8:16], scalar1=R_[:, 16:17], scalar2=None, op0=ALU.is_equal))
                    A(lambda e: e.scalar_tensor_tensor(out=R_[:, 40:48], in0=R_[:, 32:40], scalar=-1.0e30, in1=R_[:, 8:16], op0=ALU.mult, op1=ALU.add))
                    A(lambda e: e.reduce_max(out=R_[:, 17:18], in_=R_[:, 40:48], axis=AX.X))
                    A(lambda e: e.tensor_scalar(out=R_[:, 48:56], in0=R_[:, 40:48], scalar1=R_[:, 17:18], scalar2=None, op0=ALU.is_equal))
                    A(lambda e: e.tensor_tensor(out=R_[:, 18:19], in0=R_[:, 17:18], in1=R_[:, 16:17], op=ALU.subtract))
                    seq.append(("act", lambda e: e.activation(out=R_[:, 19:20], in_=R_[:, 18:19], func=AF.Exp), []))
                    A(lambda e: e.tensor_scalar(out=R_[:, 20:21], in0=R_[:, 19:20], scalar1=1.0, scalar2=None, op0=ALU.add))
                    A(lambda e: e.reciprocal(out=R_[:, 21:22], in_=R_[:, 20:21]))
                    A(lambda e: e.tensor_tensor(out=R_[:, 22:23], in0=R_[:, 19:20], in1=R_[:, 21:22], op=ALU.mult))
                    A(lambda e: e.tensor_tensor(out=R_[:, 23:24], in0=R_[:, 21:22], in1=R_[:, 3:4], op=ALU.mult))
                    A(lambda e: e.tensor_tensor(out=R_[:, 24:25], in0=R_[:, 22:23], in1=R_[:, 3:4], op=ALU.mult))
                    A(lambda e: e.tensor_scalar(out=R_[:, 56:64], in0=R_[:, 32:40], scalar1=R_[:, 23:24], scalar2=None, op0=ALU.mult))
                    A(lambda e: e.scalar_tensor_tensor(out=R_[:, 56:64], in0=R_[:, 48:56], scalar=R_[:, 24:25], in1=R_[:, 56:64], op0=ALU.mult, op1=ALU.add))
                    for g in range(4):
                        A(lambda e, g=g: e.tensor_scalar(out=G[:, b, g * 8:(g + 1) * 8], in0=R_[:, 56:64], scalar1=R_[:, 4 + g:5 + g], scalar2=None, op0=ALU.mult), w=[BGb[b]])
                    A(lambda e: e.tensor_scalar(out=MKf[:, b, :], in0=G[:, b, :], scalar1=0.0, scalar2=None, op0=ALU.is_gt), w=[BMKfb[b]])
                    A(lambda e: e.tensor_copy(out=MK[:, b, :], in_=MKf[:, b, :]), w=[BMKb[b]])
                    return seq

                Brsb = [Buf(f"rs{b}") for b in range(8)]
                BGb = [Buf(f"G{b}") for b in range(8)]
                BMKfb = [Buf(f"MKf{b}") for b in range(8)]
                BMKb = [Buf(f"MK{b}") for b in range(8)]
                seqs = [route_ops(b) for b in range(8)]
                for k in range(len(seqs[0])):
                    for b in range(8):
                        eng_, f_, w_ = seqs[b][k]
                        fw.op(eng_, f_, reads=[BLG, Brsb[b], BGb[b], BMKfb[b]], writes=[Brsb[b]] + w_)
                for b in range(8):
                    fns = [lambda e: e.matmul(ppos[:, 0:32], lhsT=stri[:], rhs=MK[:, b, :], start=True, stop=(b == 0))]
                    for b2 in range(b):
                        fns.append(lambda e, b2=b2: e.matmul(ppos[:, 0:32], lhsT=ones[:], rhs=MK[:, b2, :], start=False, stop=(b2 == b - 1)))
                    fw.op("pe", fns, reads=[Bstri, Bones] + BMKb, writes=[Bppos])
                    fw.op("dve", lambda e: e.tensor_copy(out=POS[:, b, :], in_=ppos[:, 0:32]), reads=[Bppos], writes=[BPOS])

                if debug:
                    fw.dma("sp", [(dbg_g[:, :], G[:].rearrange("p b f -> p (b f)")), (dbg_pos[:, :], POS[:].rearrange("p b f -> p (b f)"))],
                           Bdbg, reads=BGb + [BPOS])
                P01 = [SB(st, f"P01_{i}", [128, 8, 128], BF16) for i in range(2)]; BP01 = [Buf(f"P01_{i}") for i in range(2)]
                Pw = [SB(st, f"Pw_{i}", [128, 8, 128], BF16) for i in range(2)]; BPw = [Buf(f"Pw_{i}") for i in range(2)]
                PwT = [SB(st, f"PwT_{i}", [128, 8, 128], BF16) for i in range(3)]; BPwT = [Buf(f"PwT_{i}") for i in range(3)]
                XgT = SB(st, "XgT", [128, NDC, 128], BF16); BXgT = Buf("XgT")
                NSL = 10
                gu = [SB(st, f"gu{i}", [128, 2, DE], BF16) for i in range(NSL)]; Bgu = [Buf(f"gu{i}") for i in range(NSL)]
                NDL = 6
                dn = [SB(st, f"dn{i}", [128, D], BF16) for i in range(NDL)]; Bdn = [Buf(f"dn{i}") for i in range(NDL)]
                sg = SB(st, "sg", [128, NFC * 128], F32); Bsg = Buf("sg")
                hdT = SB(st, "hdT", [128, NFC, 128], BF16); BhdT = Buf("hdT")
                ybr = [SB(st, f"yb{i}", [128, D], BF16) for i in range(2)]; Bybr = [Buf(f"yb{i}") for i in range(2)]
                w_gate_v = w_gate.rearrange("(e c p) f -> e c p f", p=128, c=NDC)
                w_up_v = w_up.rearrange("(e c p) f -> e c p f", p=128, c=NDC)
                w_down_v = w_down.rearrange("(e c p) f -> e c p f", p=128, c=NFC)
                si = 0
                di = 0
                cstate = {"ci": 0}
                pending = []
                for ex in range(n_moe_experts):
                    i2 = ex % 2
                    for b in range(8):
                        fw.op("dve", [lambda e: e.tensor_scalar(out=P01[i2][:, b, :], in0=iota_s[:], scalar1=POS[:, b, ex:ex + 1],
                                                                scalar2=MKf[:, b, ex:ex + 1], op0=ALU.is_equal, op1=ALU.mult),
                                      lambda e: e.tensor_scalar(out=Pw[i2][:, b, :], in0=iota_s[:], scalar1=POS[:, b, ex:ex + 1],
                                                                scalar2=G[:, b, ex:ex + 1], op0=ALU.is_equal, op1=ALU.mult)],
                              reads=[Biota, BPOS] + BMKfb + BGb, writes=[BP01[i2], BPw[i2]])
                    fw.op("pe", [(lambda e, b=b: e.matmul(pb[b // 4][:, (b % 4) * 128:(b % 4 + 1) * 128], lhsT=Pw[i2][:, b, :], rhs=ident[:],
                                                          start=True, stop=True)) for b in range(8)],
                          reads=[BPw[i2], Bident], writes=[Bpb[0], Bpb[1]])
                    for hh in range(2):
                        fw.op("act", lambda e: e.copy(out=PwT[ex % 3][:, hh * 4:(hh + 1) * 4, :].rearrange("p b t -> p (b t)"), in_=pb[hh][:]),
                              reads=[Bpb[hh]], writes=[BPwT[ex % 3]])
                    for q4 in range(4):
                        PB = pb[4 + q4]; BPB = Bpb[4 + q4]
                        fw.op("pe", [(lambda e, i=i, b=b: e.matmul(PB[:, i * 128:(i + 1) * 128],
                                                                   lhsT=h2[:, b, (q4 * 4 + i) * 128:(q4 * 4 + i + 1) * 128],
                                                                   rhs=P01[i2][:, b, :], start=(b == 0), stop=(b == 7)))
                                     for i in range(4) for b in range(8)], reads=Bh2 + [BP01[i2]], writes=[BPB])
                        fw.op("act" if q4 % 2 == 0 else "dve",
                              (lambda e: e.copy(out=XgT[:, q4 * 4:(q4 + 1) * 4, :].rearrange("p c t -> p (c t)"), in_=PB[:])) if q4 % 2 == 0 else
                              (lambda e: e.tensor_copy(out=XgT[:, q4 * 4:(q4 + 1) * 4, :].rearrange("p c t -> p (c t)"), in_=PB[:])),
                              reads=[BPB], writes=[BXgT])
                    for dc in range(NDC):
                        GU = gu[si % NSL]; BGU = Bgu[si % NSL]
                        fw.dma("pool", [(GU[:, 0, :], w_gate_v[ex, dc]), (GU[:, 1, :], w_up_v[ex, dc])], BGU)
                        fns = []
                        for fc in range(NFC):
                            fns.append(lambda e, fc=fc: e.matmul(pb[fc // 4][:, (fc % 4) * 128:(fc % 4 + 1) * 128], lhsT=GU[:, 0, fc * 128:(fc + 1) * 128],
                                                                 rhs=XgT[:, dc, :], start=(dc == 0 and fc % 4 == 0), stop=(dc == NDC - 1)))
                            fns.append(lambda e, fc=fc: e.matmul(pb[2 + fc // 4][:, (fc % 4) * 128:(fc % 4 + 1) * 128], lhsT=GU[:, 1, fc * 128:(fc + 1) * 128],
                                                                 rhs=XgT[:, dc, :], start=(dc == 0 and fc % 4 == 0), stop=(dc == NDC - 1)))
                        fw.op("pe", fns, reads=[BGU, BXgT], writes=[Bpb[0], Bpb[1], Bpb[2], Bpb[3]])
                        si += 1
                        for _ in range(2):
                            if pending:
                                pending.pop(0)()
                    while pending:
                        pending.pop(0)()
                    for hh in range((NFC + 3) // 4):
                        w_ = min(4, NFC - hh * 4) * 128
                        fw.op("act", lambda e: e.activation(out=sg[:, hh * 512:hh * 512 + w_], in_=pb[hh][:, 0:w_], func=AF.Silu), reads=[Bpb[hh]], writes=[Bsg])
                        fw.op("dve", lambda e: e.tensor_tensor(out=hdT[:, hh * 4:hh * 4 + w_ // 128, :].rearrange("p c t -> p (c t)"),
                                                               in0=sg[:, hh * 512:hh * 512 + w_], in1=pb[2 + hh][:, 0:w_], op=ALU.mult),
                              reads=[Bsg, Bpb[2 + hh]], writes=[BhdT])
                    for fc in range(NFC):
                        DN = dn[di % NDL]; BDN = Bdn[di % NDL]
                        fw.dma("pool", [(DN[:], w_down_v[ex, fc])], BDN)
                        fw.op("pe", [(lambda e, dm=dm: e.matmul(pb[4 + dm][:], lhsT=hdT[:, fc, :], rhs=DN[:, dm * 512:(dm + 1) * 512],
                                                                start=(fc == 0), stop=(fc == NFC - 1))) for dm in range(4)],
                              reads=[BDN, BhdT], writes=[Bpb[4 + dm] for dm in range(4)])
                        di += 1
                    for dm in range(4):
                        fw.op("act", lambda e: e.copy(out=ybr[i2][:, dm * 512:(dm + 1) * 512], in_=pb[4 + dm][:]), reads=[Bpb[4 + dm]], writes=[Bybr[i2]])
                    if ex % 2 == 0 and ex + 1 < n_moe_experts:
                        continue
                    grp_ = [ex] if ex % 2 == 0 else [ex - 1, ex]

                    def mk_item(b, dm, grp_=grp_):
                        def item():
                            PC = pb[4 + cstate["ci"] % 4]; BPC = Bpb[4 + cstate["ci"] % 4]
                            cstate["ci"] += 1
                            fw.op("pe", [(lambda e, k=k, ee=ee: e.matmul(PC[:], lhsT=PwT[ee % 3][:, b, :], rhs=ybr[ee % 2][:, dm * 512:(dm + 1) * 512],
                                                                         start=(k == 0), stop=(k == len(grp_) - 1))) for k, ee in enumerate(grp_)],
                                  reads=[BPwT[ee % 3] for ee in grp_] + [Bybr[ee % 2] for ee in grp_], writes=[BPC])
                            fw.op("dve", lambda e: e.tensor_tensor(out=x1[:, b, dm * 512:(dm + 1) * 512], in0=x1[:, b, dm * 512:(dm + 1) * 512],
                                                                   in1=PC[:], op=ALU.add), reads=[BPC], writes=[Bx1[b]])
                        return item
                    pending.extend(mk_item(b, dm) for b in range(8) for dm in range(4))
                while pending:
                    pending.pop(0)()
                if debug:
                    fw.dma("sp", [(dbg_x2[:, :], x1[:].rearrange("p b f -> p (b f)"))], Bdbg, reads=Bx1)
                fw.barrier()

            with contextlib.ExitStack() as st:
                gfin = SB(st, "gfin", [128, D], F32); Bgfin = Buf("gfin")
                fw.dma("sp", [(gfin[:], g_fin[0:1, :].partition_broadcast(128))], Bgfin)
                junk3 = SB(st, "junk3", [128, D], BF16); Bj3 = Buf("junk3")
                st3 = SB(st, "st3", [128, 8, 4], F32); Bst3 = Buf("st3")
                ot = [SB(st, f"ot{i}", [128, D], F32) for i in range(2)]; Bot = [Buf(f"ot{i}") for i in range(2)]
                Bout = Buf("out")
                for b in range(8):
                    fw.op("act", lambda e: e.activation(out=junk3[:], in_=x1[:, b, :], func=AF.Square, accum_out=st3[:, b, 0:1]),
                          reads=[Bx1[b]], writes=[Bj3, Bst3])
                    fw.op("act", lambda e: e.activation(out=st3[:, b, 1:2], in_=st3[:, b, 0:1], func=AF.Ln, scale=1.0 / D, bias=EPS), reads=[Bst3], writes=[Bst3])
                    fw.op("act", lambda e: e.activation(out=st3[:, b, 2:3], in_=st3[:, b, 1:2], func=AF.Exp, scale=-0.5), reads=[Bst3], writes=[Bst3])
                    O = ot[b % 2]; BO = Bot[b % 2]
                    fw.op("dve", lambda e: e.scalar_tensor_tensor(out=O[:], in0=x1[:, b, :], scalar=st3[:, b, 2:3], in1=gfin[:],
                                                                  op0=ALU.mult, op1=ALU.mult), reads=[Bx1[b], Bst3, Bgfin], writes=[BO])
                    fw.dma("sp", [(out_d[b * 128:(b + 1) * 128, :], O[:])], Bout, reads=[BO])
                fw.barrier()
    return nc


NT_FULL = 16
NCORES = 8


def own_rows(c, NT):
    a = np.arange(c * 512, (c + 1) * 512)
    b = np.arange((NT - 1 - c) * 512, (NT - c) * 512)
    return np.concatenate([a, b])


def make_in_maps(inputs, NT, ncores):
    f = lambda a: np.ascontiguousarray(np.asarray(a, dtype=np.float32))
    x = f(inputs["x"]).reshape(-1, D)
    DE = inputs["w_gate"].shape[-1]
    shared = {
        "x_all": x,
        "g_mix": f(inputs["g_mix"]).reshape(1, D),
        "w_in": f(inputs["w_in"]).reshape(D, 9216),
        "ln_v_g": f(inputs["ln_v_g"]).reshape(1, 1024),
        "ln_v_b": f(inputs["ln_v_b"]).reshape(1, 1024),
        "w_spatial": f(inputs["w_spatial"]).reshape(8, 128, 128),
        "b_spatial": f(inputs["b_spatial"]).reshape(1, 1024),
        "w_branch_a": f(inputs["w_branch_a"]).reshape(1024, D),
        "w_branch_b": f(inputs["w_branch_b"]).reshape(1024, D),
        "w_out": f(inputs["w_out"]).reshape(D, D),
        "g_ffn": f(inputs["g_ffn"]).reshape(1, D),
        "w_router_group": f(inputs["w_router_group"]).reshape(D, 4),
        "b_router_group": f(inputs["b_router_group"]).reshape(1, 4),
        "w_router_expert": f(inputs["w_router_expert"]).reshape(D, 32),
        "b_router_expert": f(inputs["b_router_expert"]).reshape(1, 32),
        "w_gate": f(inputs["w_gate"]).reshape(32 * D, DE),
        "w_up": f(inputs["w_up"]).reshape(32 * D, DE),
        "w_down": f(inputs["w_down"]).reshape(32 * DE, D),
        "g_final": f(inputs["g_final"]).reshape(1, D),
    }
    maps = []
    for c in range(ncores):
        rows = own_rows(c, NT)
        m = dict(shared)
        m["x_own"] = np.ascontiguousarray(x[rows])
        m["qpos"] = rows.astype(np.float32).reshape(1, 1024)
        maps.append(m)
    return maps


def kernel(**inputs):
    NT = NT_FULL
    DE = inputs["w_gate"].shape[-1]
    nc = build_nc(NT, DE)
    in_maps = make_in_maps(inputs, NT, NCORES)
    res = run_bass_kernel_spmd(nc, in_maps, core_ids=list(range(NCORES)))
    out = np.zeros((NT * 512, D), np.float32)
    for c in range(NCORES):
        out[own_rows(c, NT)] = res.results[c]["out"]
    return out.reshape(1, NT * 512, D)
```

```python
import contextlib
import numpy as np
import concourse.bass as bass
import concourse.mybir as mybir
from concourse.bass_utils import run_bass_kernel_spmd

F32 = mybir.dt.float32
BF16 = mybir.dt.bfloat16
AF = mybir.ActivationFunctionType
ALU = mybir.AluOpType
AX = mybir.AxisListType

D = 2048
NDC = 16
EPS = 1e-6
NEG = -30000.0


class Buf:
    __slots__ = ("name", "w", "r", "dsem", "dcnt")

    def __init__(self, name):
        self.name = name
        self.w = None
        self.r = []
        self.dsem = None
        self.dcnt = 0


class FW:
    def __init__(self, nc, stack):
        self.nc = nc
        self.stack = stack
        self.eng = {"pe": nc.tensor, "act": nc.scalar, "dve": nc.vector, "pool": nc.gpsimd, "sp": nc.sync}
        self.sems = {}
        self.cnt = {}
        self.cur = {}
        self.waited = {k: {} for k in self.eng}
        self.nsem = 0
        self.dbufs = []
        self.nins = 0
        for k in ("pe", "act", "dve", "pool"):
            self._new_sem(k)

    def _mk(self, name):
        h = self.stack.enter_context(self.nc.semaphore(name))
        self.nsem += 1
        return h

    def _new_sem(self, k):
        name = f"s_{k}_{self.nsem}"
        self.sems[name] = self._mk(name)
        self.cnt[name] = 0
        self.cur[k] = name

    def _wait(self, k, tok):
        if tok is None:
            return
        s, v = tok
        if self.waited[k].get(s, 0) >= v:
            return
        self.eng[k].wait_ge(self.sems[s], v)
        self.waited[k][s] = v

    def _deps(self, k, reads, writes, extra):
        for b in reads:
            self._wait(k, b.w)
        for b in writes:
            self._wait(k, b.w)
            for t in b.r:
                self._wait(k, t)
        for t in extra:
            self._wait(k, t)

    @staticmethod
    def _commit(tok, reads, writes):
        for b in reads:
            b.r.append(tok)
        for b in writes:
            b.w = tok
            b.r = []

    def op(self, k, fns, reads=(), writes=(), extra=()):
        if callable(fns):
            fns = [fns]
        self._deps(k, reads, writes, extra)
        e = self.eng[k]
        ins = None
        for f in fns:
            ins = f(e)
            self.nins += 1
        s = self.cur[k]
        ins.then_inc(self.sems[s], 1)
        self.cnt[s] += 1
        tok = (s, self.cnt[s])
        self._commit(tok, reads, writes)
        return tok

    def dma(self, q, pairs, dst, reads=(), extra=()):
        self._deps(q, reads, [dst], extra)
        e = self.eng[q]
        if dst.dsem is None:
            dst.dsem = f"d_{dst.name}_{self.nsem}"
            self.sems[dst.dsem] = self._mk(dst.dsem)
            self.dbufs.append(dst)
        for (o, i) in pairs:
            e.dma_start(out=o, in_=i).then_inc(self.sems[dst.dsem], 16)
            dst.dcnt += 16
            self.nins += 1
        tok = (dst.dsem, dst.dcnt)
        self._commit(tok, reads, [dst])
        return tok

    def barrier(self, engines=("pe", "act", "dve", "pool", "sp")):
        toks = [(s, self.cnt[s]) for s in self.cnt if self.cnt[s] > 0]
        toks += [(b.dsem, b.dcnt) for b in self.dbufs if b.dcnt > 0]
        for k in engines:
            for t in toks:
                self._wait(k, t)


def build_nc(NT, DE, n_moe_experts=32, debug=False):
    S = NT * 512
    NB = S // 128
    NFC = DE // 128
    SCALE = 128 ** -0.5
    nc = bass.Bass("TRN2", target_bir_lowering=False)

    def din(name, shape):
        return nc.dram_tensor(name, shape, F32, kind="ExternalInput").ap()

    x_all = din("x_all", [S, D])
    x_own = din("x_own", [1024, D])
    qpos_d = din("qpos", [1, 1024])
    g_mix = din("g_mix", [1, D])
    w_in = din("w_in", [D, 9216])
    ln_g = din("ln_v_g", [1, 1024])
    ln_b = din("ln_v_b", [1, 1024])
    w_sp = din("w_spatial", [8, 128, 128])
    b_sp = din("b_spatial", [1, 1024])
    w_ba = din("w_branch_a", [1024, D])
    w_bb = din("w_branch_b", [1024, D])
    w_out = din("w_out", [D, D])
    g_ffn = din("g_ffn", [1, D])
    w_rg = din("w_router_group", [D, 4])
    b_rg = din("b_router_group", [1, 4])
    w_re = din("w_router_expert", [D, 32])
    b_re = din("b_router_expert", [1, 32])
    w_gate = din("w_gate", [32 * D, DE])
    w_up = din("w_up", [32 * D, DE])
    w_down = din("w_down", [32 * DE, D])
    g_fin = din("g_final", [1, D])
    out_d = nc.dram_tensor("out", [1024, D], F32, kind="ExternalOutput").ap()
    hTs = nc.dram_tensor("hTs", [NT * 128, NDC * 512], BF16).ap()
    hTo = nc.dram_tensor("hTo", [2 * 128, NDC * 512], BF16).ap()
    mTs = nc.dram_tensor("mTs", [128, NDC * 1024], BF16).ap()
    if debug:
        dbg_ya = nc.dram_tensor("dbg_ya", [128, 8 * 1024], BF16, kind="ExternalOutput").ap()
        dbg_yb = nc.dram_tensor("dbg_yb", [128, 8 * 1024], BF16, kind="ExternalOutput").ap()
        dbg_x1 = nc.dram_tensor("dbg_x1", [128, 8 * D], F32, kind="ExternalOutput").ap()
        dbg_lg = nc.dram_tensor("dbg_lg", [128, 8 * 36], F32, kind="ExternalOutput").ap()
        dbg_g = nc.dram_tensor("dbg_g", [128, 8 * 32], F32, kind="ExternalOutput").ap()
        dbg_pos = nc.dram_tensor("dbg_pos", [128, 8 * 32], F32, kind="ExternalOutput").ap()
        dbg_x2 = nc.dram_tensor("dbg_x2", [128, 8 * D], F32, kind="ExternalOutput").ap()
    Bdbg = Buf("dbg")

    with contextlib.ExitStack() as top:
        fw = FW(nc, top)

        def SB(st, name, shape, dt):
            return st.enter_context(nc.sbuf_tensor("sb_" + name, shape, dt))

        def PS(st, name, shape, dt):
            return st.enter_context(nc.psum_tensor("ps_" + name, shape, dt))

        ident = SB(top, "ident", [128, 128], BF16); Bident = Buf("ident")
        identf = SB(top, "identf", [128, 128], F32); Bidentf = Buf("identf")
        negU = SB(top, "negU", [128, 128], BF16); BnegU = Buf("negU")
        negOnes = SB(top, "negOnes", [128, 128], BF16); BnegOnes = Buf("negOnes")
        ones = SB(top, "ones", [128, 128], BF16); Bones = Buf("ones")
        onesf = SB(top, "onesf", [128, 128], F32); Bonesf = Buf("onesf")
        stri = SB(top, "stri", [128, 128], BF16); Bstri = Buf("stri")
        kpos = SB(top, "kpos", [128, NB], F32); Bkpos = Buf("kpos")
        iota_s = SB(top, "iota_s", [128, 128], F32); Biota = Buf("iota")
        stA = contextlib.ExitStack()
        qpos = SB(stA, "qpos", [128, 1024], F32); Bqpos = Buf("qpos")
        y_aT = SB(stA, "y_aT", [128, 8, 1024], BF16); By_a = Buf("y_aT")
        y_bT = SB(stA, "y_bT", [128, 8, 1024], BF16); By_b = Buf("y_bT")
        BmTs = Buf("mTs")

        fw.op("pool", lambda e: e.memset(ones[:], 1.0), writes=[Bones])
        fw.op("pool", lambda e: e.memset(onesf[:], 1.0), writes=[Bonesf])
        fw.op("pool", lambda e: e.memset(negOnes[:], -1.0), writes=[BnegOnes])
        fw.op("pool", lambda e: e.affine_select(out=ident[:], in_=ones[:], pattern=[[-1, 128]],
                                                compare_op=ALU.is_equal, fill=0.0, base=0, channel_multiplier=1),
              reads=[Bones], writes=[Bident])
        fw.op("pool", lambda e: e.affine_select(out=identf[:], in_=onesf[:], pattern=[[-1, 128]],
                                                compare_op=ALU.is_equal, fill=0.0, base=0, channel_multiplier=1),
              reads=[Bonesf], writes=[Bidentf])
        fw.op("pool", lambda e: e.affine_select(out=negU[:], in_=negOnes[:], pattern=[[-1, 128]],
                                                compare_op=ALU.is_ge, fill=0.0, base=0, channel_multiplier=1),
              reads=[BnegOnes], writes=[BnegU])
        fw.op("pool", lambda e: e.affine_select(out=stri[:], in_=ones[:], pattern=[[1, 128]],
                                                compare_op=ALU.is_gt, fill=0.0, base=0, channel_multiplier=-1),
              reads=[Bones], writes=[Bstri])
        fw.op("pool", lambda e: e.iota(kpos[:], pattern=[[128, NB]], base=0, channel_multiplier=1,
                                       allow_small_or_imprecise_dtypes=True), writes=[Bkpos])
        fw.op("pool", lambda e: e.iota(iota_s[:], pattern=[[1, 128]], base=0, channel_multiplier=0,
                                       allow_small_or_imprecise_dtypes=True), writes=[Biota])
        fw.dma("sp", [(qpos[:], qpos_d[0:1, :].partition_broadcast(128))], Bqpos)

        BhTs = Buf("hTs")
        BhTo = Buf("hTo")
        with contextlib.ExitStack() as st:
            gm = SB(st, "gm", [128, D], F32); Bgm = Buf("gm")
            fw.dma("sp", [(gm[:], g_mix[0:1, :].partition_broadcast(128))], Bgm)
            xt = [SB(st, f"xt{i}", [128, D], F32) for i in range(4)]
            Bxt = [Buf(f"xt{i}") for i in range(4)]
            junk = SB(st, "junk", [128, D], BF16); Bjunk = Buf("junk")
            stt = [SB(st, f"stt{i}", [128, 4], F32) for i in range(4)]
            Bstt = [Buf(f"stt{i}") for i in range(4)]
            hb = [SB(st, f"hb{i}", [128, D], BF16) for i in range(3)]
            Bhb = [Buf(f"hb{i}") for i in range(3)]
            hT4 = [SB(st, f"hT4{i}", [128, NDC, 512], BF16) for i in range(2)]
            BhT4 = [Buf(f"hT4{i}") for i in range(2)]
            pt = [PS(st, f"pt{i}", [128, D], BF16) for i in range(2)]
            Bpt = [Buf(f"pt{i}") for i in range(2)]
            blocks = [(tile, blk) for tile in range(NT + 2) for blk in range(4)]

            def p1_s1(bi):
                tile, blk = blocks[bi]
                own = tile >= NT
                src = x_own if own else x_all
                t_loc = tile - NT if own else tile
                r0 = (t_loc * 4 + blk) * 128
                X = xt[bi % 4]; BX = Bxt[bi % 4]
                sT = stt[bi % 4]; BsT = Bstt[bi % 4]
                H = hb[bi % 3]; BH = Bhb[bi % 3]
                fw.dma("sp", [(X[:], src[r0:r0 + 128, :])], BX)
                fw.op("act", lambda e: e.activation(out=junk[:], in_=X[:], func=AF.Square, accum_out=sT[:, 0:1]),
                      reads=[BX], writes=[Bjunk, BsT])
                fw.op("act", lambda e: e.activation(out=sT[:, 1:2], in_=sT[:, 0:1], func=AF.Ln, scale=1.0 / D, bias=EPS),
                      reads=[BsT], writes=[BsT])
                fw.op("act", lambda e: e.activation(out=sT[:, 2:3], in_=sT[:, 1:2], func=AF.Exp, scale=-0.5),
                      reads=[BsT], writes=[BsT])
                fw.op("dve", lambda e: e.scalar_tensor_tensor(out=H[:], in0=X[:], scalar=sT[:, 2:3], in1=gm[:],
                                                              op0=ALU.mult, op1=ALU.mult),
                      reads=[BX, BsT, Bgm], writes=[BH])

            def p1_s2(bi):
                tile, blk = blocks[bi]
                own = tile >= NT
                t_loc = tile - NT if own else tile
                h4 = hT4[tile % 2]; Bh4 = BhT4[tile % 2]
                H = hb[bi % 3]; BH = Bhb[bi % 3]
                P = pt[bi % 2]; BP = Bpt[bi % 2]
                fw.op("pe", [(lambda e, dc=dc: e.transpose(out=P[:, dc * 128:(dc + 1) * 128],
                                                            in_=H[:, dc * 128:(dc + 1) * 128], identity=ident[:]))
                             for dc in range(NDC)], reads=[BH, Bident], writes=[BP])
                if bi % 2 == 0:
                    fw.op("act", lambda e: e.copy(out=h4[:, :, blk * 128:(blk + 1) * 128],
                                                  in_=P[:].rearrange("p (c t) -> p c t", c=NDC)),
                          reads=[BP], writes=[Bh4])
                else:
                    fw.op("dve", lambda e: e.tensor_copy(out=h4[:, :, blk * 128:(blk + 1) * 128],
                                                         in_=P[:].rearrange("p (c t) -> p c t", c=NDC)),
                          reads=[BP], writes=[Bh4])
                if blk == 3:
                    dstd = hTo if own else hTs
                    fw.dma("pool", [(dstd[t_loc * 128:(t_loc + 1) * 128, :], h4[:].rearrange("p c t -> p (c t)"))],
                           BhTo if own else BhTs, reads=[Bh4])

            p1_s1(0)
            for bi in range(len(blocks)):
                if bi + 1 < len(blocks):
                    p1_s1(bi + 1)
                p1_s2(bi)
            fw.barrier()

        with contextlib.ExitStack() as st:
            Wu = SB(st, "Wu", [128, NDC, 1024], BF16); BWu = Buf("Wu")
            Wv = SB(st, "Wv", [128, NDC, 1024], BF16); BWv = Buf("Wv")
            w_in_v = w_in.rearrange("(c p) f -> p c f", p=128)
            for half in range(2):
                fw.dma("pool", [(Wu[:, half * 8:(half + 1) * 8, :], w_in_v[:, half * 8:(half + 1) * 8, 0:1024])], BWu)
                fw.dma("pool", [(Wv[:, half * 8:(half + 1) * 8, :], w_in_v[:, half * 8:(half + 1) * 8, 1024:2048])], BWv)
            lg_t = SB(st, "lg_t", [128, 1024], F32); Blg = Buf("lng")
            lb_t = SB(st, "lb_t", [128, 1024], F32); Blb = Buf("lnb")
            fw.dma("sp", [(lg_t[:], ln_g[0:1, :].partition_broadcast(128))], Blg)
            fw.dma("sp", [(lb_t[:], ln_b[0:1, :].partition_broadcast(128))], Blb)
            bsr_f = SB(st, "bsr_f", [1, 1024], F32); Bbsrf = Buf("bsrf")
            bsr = SB(st, "bsr", [1, 1024], BF16); Bbsr = Buf("bsr")
            fw.dma("sp", [(bsr_f[:], b_sp[0:1, :])], Bbsrf)
            fw.op("dve", lambda e: e.tensor_copy(out=bsr[:], in_=bsr_f[:]), reads=[Bbsrf], writes=[Bbsr])
            wsl = SB(st, "wsl", [128, 8, 128], F32); Bwsl = Buf("wsl")
            fw.dma("sp", [(wsl[:], w_sp.rearrange("g t s -> t g s"))], Bwsl)
            wsT = SB(st, "wsT", [128, 8, 128], BF16); BwsT = Buf("wsT")
            wsTf = SB(st, "wsTf", [128, 8, 128], F32); BwsTf = Buf("wsTf")
            pw = PS(st, "pw", [128, 1024], F32); Bpw = Buf("pw")
            fw.op("pe", [(lambda e, g=g: e.transpose(out=pw[:, g * 128:(g + 1) * 128], in_=wsl[:, g, :], identity=identf[:]))
                         for g in range(8)], reads=[Bwsl, Bidentf], writes=[Bpw])
            fw.op("dve", lambda e: e.tensor_copy(out=wsTf[:].rearrange("p g t -> p (g t)"), in_=pw[:]), reads=[Bpw], writes=[BwsTf])
            for g in range(8):
                fw.op("pool", lambda e: e.affine_select(out=wsT[:, g, :], in_=wsTf[:, g, :], pattern=[[1, 128]],
                                                        compare_op=ALU.is_ge, fill=0.0, base=0, channel_multiplier=-1),
                      reads=[BwsTf], writes=[BwsT])
            hto = SB(st, "hto", [128, NDC, 512], BF16); Bhto = Buf("hto")
            guT = SB(st, "guT", [128, 8, 512], BF16); BguT = Buf("guT")
            pu = [PS(st, f"pu{i}", [128, 512], F32) for i in range(2)]
            Bpu = [Buf(f"pu{i}") for i in range(2)]
            pv = PS(st, "pv", [128, 1024], F32); Bpv = Buf("pv")
            pm = PS(st, "pm", [128, 1024], F32); Bpm = Buf("pm")
            t1 = SB(st, "t1", [128, 1024], F32); Bt1 = Buf("t1")
            t2 = SB(st, "t2", [128, 1024], F32); Bt2 = Buf("t2")
            gv = SB(st, "gv", [128, 1024], F32); Bgv = Buf("gv")
            vn = SB(st, "vn", [128, 1024], BF16); Bvn = Buf("vn")
            u1 = SB(st, "u1", [128, 512], F32); Bu1 = Buf("u1")
            u2 = SB(st, "u2", [128, 512], F32); Bu2 = Buf("u2")
            sv = SB(st, "sv", [128, 8], F32); Bsv = Buf("sv")
            GC = 1.5957691216057308

            def gelu_ops(dst_ap, src_ap, a_ap, b_ap, reads, Ba, Bb, Bdst):
                fw.op("act", lambda e: e.copy(out=b_ap, in_=src_ap), reads=reads, writes=[Bb])
                fw.op("dve", lambda e: e.tensor_tensor(out=a_ap, in0=b_ap, in1=b_ap, op=ALU.mult), reads=[Bb], writes=[Ba])
                fw.op("dve", lambda e: e.tensor_scalar(out=a_ap, in0=a_ap, scalar1=0.044715, scalar2=1.0, op0=ALU.mult, op1=ALU.add),
                      reads=[Ba], writes=[Ba])
                fw.op("dve", lambda e: e.tensor_tensor(out=a_ap, in0=a_ap, in1=b_ap, op=ALU.mult), reads=[Ba, Bb], writes=[Ba])
                fw.op("act", lambda e: e.activation(out=a_ap, in_=a_ap, func=AF.Sigmoid, scale=GC), reads=[Ba], writes=[Ba])
                fw.op("dve", lambda e: e.tensor_tensor(out=dst_ap, in0=a_ap, in1=b_ap, op=ALU.mult), reads=[Ba, Bb], writes=[Bdst])

            u1r = [u1, SB(st, "u1b", [128, 512], F32)]; Bu1r = [Bu1, Buf("u1b")]
            u2r = [u2, SB(st, "u2b", [128, 512], F32)]; Bu2r = [Bu2, Buf("u2b")]
            t1r = [t1, SB(st, "t1b", [128, 1024], F32)]; Bt1r = [Bt1, Buf("t1b")]
            t2r = [t2, SB(st, "t2b", [128, 1024], F32)]; Bt2r = [Bt2, Buf("t2b")]
            pvr = [pv, pw]; Bpvr = [Bpv, Bpw]

            def gelu_s1(a_ap, b_ap, src_ap, Ba, Bb, Bsrc):
                fw.op("act", lambda e: e.copy(out=b_ap, in_=src_ap), reads=[Bsrc], writes=[Bb])
                fw.op("dve", lambda e: e.tensor_tensor(out=a_ap, in0=b_ap, in1=b_ap, op=ALU.mult), reads=[Bb], writes=[Ba])
                fw.op("dve", lambda e: e.tensor_scalar(out=a_ap, in0=a_ap, scalar1=0.044715, scalar2=1.0, op0=ALU.mult, op1=ALU.add),
                      reads=[Ba], writes=[Ba])
                fw.op("dve", lambda e: e.tensor_tensor(out=a_ap, in0=a_ap, in1=b_ap, op=ALU.mult), reads=[Ba, Bb], writes=[Ba])

            def gelu_s2(dst_ap, a_ap, b_ap, Ba, Bb, Bdst):
                fw.op("act", lambda e: e.activation(out=a_ap, in_=a_ap, func=AF.Sigmoid, scale=GC), reads=[Ba], writes=[Ba])
                fw.op("dve", lambda e: e.tensor_tensor(out=dst_ap, in0=a_ap, in1=b_ap, op=ALU.mult), reads=[Ba, Bb], writes=[Bdst])

            ucnt = {"u": 0, "v": 0}
            for tl in range(2):
                fw.dma("sp", [(hto[:].rearrange("p c t -> p (c t)"), hTo[tl * 128:(tl + 1) * 128, :])], Bhto, reads=[BhTo])

                def u_s1(cc):
                    k = (ucnt["u"] + cc) % 2
                    P = pu[k]; BP = Bpu[k]
                    fw.op("pe", [(lambda e, dc=dc: e.matmul(P[:], lhsT=Wu[:, dc, cc * 128:(cc + 1) * 128], rhs=hto[:, dc, :],
                                                            start=(dc == 0), stop=(dc == NDC - 1))) for dc in range(NDC)],
                          reads=[BWu, Bhto], writes=[BP])
                    gelu_s1(u1r[k][:], u2r[k][:], P[:], Bu1r[k], Bu2r[k], BP)

                def u_s2(cc):
                    k = (ucnt["u"] + cc) % 2
                    gelu_s2(guT[:, cc, :], u1r[k][:], u2r[k][:], Bu1r[k], Bu2r[k], BguT)

                u_s1(0)
                for cc in range(8):
                    if cc + 1 < 8:
                        u_s1(cc + 1)
                    u_s2(cc)
                ucnt["u"] += 8

                def v_s1(blk):
                    k = (ucnt["v"] + blk) % 2
                    PV = pvr[k]; BPV = Bpvr[k]
                    fw.op("pe", [(lambda e, dc=dc, hf=hf: e.matmul(PV[:, hf * 512:(hf + 1) * 512], lhsT=hto[:, dc, blk * 128:(blk + 1) * 128],
                                                                  rhs=Wv[:, dc, hf * 512:(hf + 1) * 512],
                                                                  start=(dc == 0), stop=(dc == NDC - 1)))
                                 for hf in range(2) for dc in range(NDC)], reads=[BWv, Bhto], writes=[BPV])
                    gelu_s1(t1r[k][:], t2r[k][:], PV[:], Bt1r[k], Bt2r[k], BPV)

                def v_s2(blk):
                    k = (ucnt["v"] + blk) % 2
                    tb = tl * 4 + blk
                    T1 = t1r[k]; T2 = t2r[k]; BT1 = Bt1r[k]; BT2 = Bt2r[k]
                    gelu_s2(gv[:], T1[:], T2[:], BT1, BT2, Bgv)
                    fw.op("act", lambda e: e.activation(out=T2[:], in_=gv[:], func=AF.Copy, accum_out=sv[:, 0:1]), reads=[Bgv], writes=[BT2, Bsv])
                    fw.op("act", lambda e: e.activation(out=T2[:], in_=gv[:], func=AF.Square, accum_out=sv[:, 1:2]), reads=[Bgv], writes=[BT2, Bsv])
                    fw.op("dve", lambda e: e.tensor_scalar(out=sv[:, 2:3], in0=sv[:, 0:1], scalar1=1.0 / 1024, scalar2=None, op0=ALU.mult), reads=[Bsv], writes=[Bsv])
                    fw.op("dve", lambda e: e.tensor_tensor(out=sv[:, 3:4], in0=sv[:, 2:3], in1=sv[:, 2:3], op=ALU.mult), reads=[Bsv], writes=[Bsv])
                    fw.op("dve", lambda e: e.scalar_tensor_tensor(out=sv[:, 4:5], in0=sv[:, 1:2], scalar=1.0 / 1024, in1=sv[:, 3:4],
                                                                  op0=ALU.mult, op1=ALU.subtract), reads=[Bsv], writes=[Bsv])
                    fw.op("act", lambda e: e.activation(out=sv[:, 5:6], in_=sv[:, 4:5], func=AF.Ln, bias=EPS), reads=[Bsv], writes=[Bsv])
                    fw.op("act", lambda e: e.activation(out=sv[:, 6:7], in_=sv[:, 5:6], func=AF.Exp, scale=-0.5), reads=[Bsv], writes=[Bsv])
                    fw.op("dve", lambda e: e.tensor_scalar(out=T1[:], in0=gv[:], scalar1=sv[:, 2:3], scalar2=sv[:, 6:7],
                                                           op0=ALU.subtract, op1=ALU.mult), reads=[Bgv, Bsv], writes=[BT1])
                    fw.op("dve", lambda e: e.tensor_tensor(out=T1[:], in0=T1[:], in1=lg_t[:], op=ALU.mult), reads=[Blg], writes=[BT1])
                    fw.op("dve", lambda e: e.tensor_tensor(out=vn[:], in0=T1[:], in1=lb_t[:], op=ALU.add), reads=[BT1, Blb], writes=[Bvn])
                    fns = []
                    for g in range(8):
                        fns.append(lambda e, g=g: e.matmul(pm[:, g * 128:(g + 1) * 128], lhsT=vn[:, g * 128:(g + 1) * 128],
                                                           rhs=wsT[:, g, :], start=True, stop=False))
                        fns.append(lambda e, g=g: e.matmul(pm[:, g * 128:(g + 1) * 128], lhsT=ones[0:1, :],
                                                           rhs=bsr[0:1, g * 128:(g + 1) * 128], start=False, stop=True))
                    fw.op("pe", fns, reads=[Bvn, BwsT, Bones, Bbsr], writes=[Bpm])
                    fw.op("dve", lambda e: e.tensor_tensor(out=y_aT[:, :, tb * 128:(tb + 1) * 128],
                                                           in0=pm[:].rearrange("p (g t) -> p g t", g=8),
                                                           in1=guT[:, :, blk * 128:(blk + 1) * 128], op=ALU.mult),
                          reads=[Bpm, BguT], writes=[By_a])

                v_s1(0)
                for blk in range(4):
                    if blk + 1 < 4:
                        v_s1(blk + 1)
                    v_s2(blk)
                ucnt["v"] += 4
            fw.barrier()

        with contextlib.ExitStack() as st:
            KT = [SB(st, f"KT{i}", [128, S], BF16) for i in range(2)]; BKT = [Buf(f"KT{i}") for i in range(2)]
            V = [SB(st, f"V{i}", [128, NB, 128], BF16) for i in range(2)]; BV = [Buf(f"V{i}") for i in range(2)]
            QT = [SB(st, f"QT{i}", [128, 1024], BF16) for i in range(2)]; BQT = [Buf(f"QT{i}") for i in range(2)]
            Wq = [SB(st, f"Wq{i}", [128, NDC, 128], BF16) for i in range(2)]; BWq = [Buf(f"Wq{i}") for i in range(2)]
            Wk = [SB(st, f"Wk{i}", [128, NDC, 128], BF16) for i in range(2)]; BWk = [Buf(f"Wk{i}") for i in range(2)]
            Wvh = [SB(st, f"Wvh{i}", [128, NDC, 128], BF16) for i in range(2)]; BWvh = [Buf(f"Wvh{i}") for i in range(2)]
            htl = [SB(st, f"htl{i}", [128, NDC, 512], BF16) for i in range(2)]
            Bhtl = [Buf(f"htl{i}") for i in range(2)]
            Et = [SB(st, f"Et{i}", [128, 512], F32) for i in range(4)]; BEt = [Buf(f"Et{i}") for i in range(4)]
            Xt = [SB(st, f"Xt{i}", [128, 512], F32) for i in range(2)]; BXt = [Buf(f"Xt{i}") for i in range(2)]
            Lt = [SB(st, f"Lt{i}", [128, 512], BF16) for i in range(4)]; BLt = [Buf(f"Lt{i}") for i in range(4)]
            Rt = [SB(st, f"Rt{i}", [128, 512], BF16) for i in range(5)]; BRt = [Buf(f"Rt{i}") for i in range(5)]
            Wt = [SB(st, f"Wt{i}", [128, 512], BF16) for i in range(3)]; BWt = [Buf(f"Wt{i}") for i in range(3)]
            Mt = [SB(st, f"Mt{i}", [128, 512], BF16) for i in range(6)]; BMt = [Buf(f"Mt{i}") for i in range(6)]
            pz = [PS(st, f"pz{i}", [128, 512], F32) for i in range(2)]; Bpz = [Buf(f"pz{i}") for i in range(2)]
            pS = [PS(st, f"pS{i}", [128, 512], F32) for i in range(2)]; BpS = [Buf(f"pS{i}") for i in range(2)]
            pacc = PS(st, "pacc", [128, 512], F32); Bpacc = Buf("pacc")
            pk = [PS(st, f"pk{i}", [128, 512], F32) for i in range(2)]; Bpk = [Buf(f"pk{i}") for i in range(2)]
            ppv = PS(st, "ppv", [128, 512], F32); Bppv = Buf("ppv")
            w_in_v = w_in.rearrange("(c p) f -> p c f", p=128)
            cnt = {"ht": 0, "pk": 0}

            def load_w(hd):
                h2_ = hd % 2
                for (Wt_, BWt_, off) in ((Wq[h2_], BWq[h2_], 2048), (Wk[h2_], BWk[h2_], 3072), (Wvh[h2_], BWvh[h2_], 4096)):
                    fw.dma("pool", [(Wt_[:, q * 4:(q + 1) * 4, :], w_in_v[:, q * 4:(q + 1) * 4, off + hd * 128:off + (hd + 1) * 128])
                                    for q in range(4)], BWt_)

            def proj_tile_pieces(hd, tile):
                h2_ = hd % 2
                stt_ = {}

                def p0():
                    HT = htl[cnt["ht"] % 2]; BHT = Bhtl[cnt["ht"] % 2]; cnt["ht"] += 1
                    P = pk[cnt["pk"] % 2]; BP = Bpk[cnt["pk"] % 2]; cnt["pk"] += 1
                    stt_.update(HT=HT, BHT=BHT, P=P, BP=BP)
                    fw.dma("sp", [(HT[:].rearrange("p c t -> p (c t)"), hTs[tile * 128:(tile + 1) * 128, :])], BHT, reads=[BhTs])
                    fw.op("pe", [(lambda e, dc=dc: e.matmul(P[:], lhsT=Wk[h2_][:, dc, :], rhs=HT[:, dc, :],
                                                            start=(dc == 0), stop=False)) for dc in range(NDC // 2)],
                          reads=[BWk[h2_], BHT], writes=[BP])

                def p1():
                    HT, BHT, P, BP = stt_["HT"], stt_["BHT"], stt_["P"], stt_["BP"]
                    fw.op("pe", [(lambda e, dc=dc: e.matmul(P[:], lhsT=Wk[h2_][:, dc, :], rhs=HT[:, dc, :],
                                                            start=False, stop=(dc == NDC - 1))) for dc in range(NDC // 2, NDC)],
                          reads=[BWk[h2_], BHT], writes=[BP])
                    fw.op("act", lambda e: e.copy(out=KT[h2_][:, tile * 512:(tile + 1) * 512], in_=P[:]), reads=[BP], writes=[BKT[h2_]])

                def pv_(b):
                    def f():
                        HT, BHT = stt_["HT"], stt_["BHT"]
                        fw.op("pe", [(lambda e, dc=dc: e.matmul(ppv[:, b * 128:(b + 1) * 128], lhsT=HT[:, dc, b * 128:(b + 1) * 128],
                                                                rhs=Wvh[h2_][:, dc, :], start=(dc == 0), stop=(dc == NDC - 1)))
                                     for dc in range(NDC)], reads=[BWvh[h2_], BHT], writes=[Bppv])
                        if b == 3:
                            fw.op("dve", lambda e: e.tensor_copy(out=V[h2_][:, tile * 4:(tile + 1) * 4, :].rearrange("p b d -> p (b d)"), in_=ppv[:]),
                                  reads=[Bppv], writes=[BV[h2_]])
                    return f
                return [p0, p1, pv_(0), pv_(1), pv_(2), pv_(3)]

            def proj_q_pieces(hd, tl):
                h2_ = hd % 2
                stt_ = {}

                def p0():
                    HT = htl[cnt["ht"] % 2]; BHT = Bhtl[cnt["ht"] % 2]; cnt["ht"] += 1
                    P = pk[cnt["pk"] % 2]; BP = Bpk[cnt["pk"] % 2]; cnt["pk"] += 1
                    stt_.update(HT=HT, BHT=BHT, P=P, BP=BP)
                    fw.dma("sp", [(HT[:].rearrange("p c t -> p (c t)"), hTo[tl * 128:(tl + 1) * 128, :])], BHT, reads=[BhTo])
                    fw.op("pe", [(lambda e, dc=dc: e.matmul(P[:], lhsT=Wq[h2_][:, dc, :], rhs=HT[:, dc, :],
                                                            start=(dc == 0), stop=False)) for dc in range(NDC // 2)],
                          reads=[BWq[h2_], BHT], writes=[BP])

                def p1():
                    HT, BHT, P, BP = stt_["HT"], stt_["BHT"], stt_["P"], stt_["BP"]
                    fw.op("pe", [(lambda e, dc=dc: e.matmul(P[:], lhsT=Wq[h2_][:, dc, :], rhs=HT[:, dc, :],
                                                            start=False, stop=(dc == NDC - 1))) for dc in range(NDC // 2, NDC)],
                          reads=[BWq[h2_], BHT], writes=[BP])
                    fw.op("act", lambda e: e.mul(out=QT[h2_][:, tl * 512:(tl + 1) * 512], in_=P[:], mul=SCALE), reads=[BP], writes=[BQT[h2_]])
                return [p0, p1]

            def proj_jobs(hd):
                jobs = []
                for t in range(NT):
                    jobs += proj_tile_pieces(hd, t)
                for t in range(2):
                    jobs += proj_q_pieces(hd, t)
                return jobs

            units = []
            for tl in range(2):
                nkb = NB // 2 if tl == 0 else NB
                for un in range(nkb):
                    l = nkb - 1 - un
                    units.append((tl, l, (tl == 0) or (l >= NB // 2), un == 0, un == nkb - 1))
            NU = len(units)
            gbase = 0
            load_w(0)
            for j in proj_jobs(0):
                j()
            for hd in range(8):
                h2_ = hd % 2
                jobs = []
                if hd + 1 < 8:
                    load_w(hd + 1)
                    jobs = proj_jobs(hd + 1)
                nj = len(jobs)
                jdone = 0

                def sgM(i):
                    tl, l, masked, first, last = units[i]
                    gi = gbase + i
                    qs = slice(tl * 512, (tl + 1) * 512)
                    M = Mt[gi % 6]; BM = BMt[gi % 6]
                    if masked:
                        fw.op("dve", lambda e: e.tensor_scalar(out=M[:], in0=qpos[:, qs], scalar1=kpos[:, l:l + 1], scalar2=1.0,
                                                               op0=ALU.is_gt, op1=ALU.mult), reads=[Bqpos, Bkpos], writes=[BM])

                def sgA(i):
                    tl, l, masked, first, last = units[i]
                    gi = gbase + i
                    qs = slice(tl * 512, (tl + 1) * 512); ks = slice(l * 128, (l + 1) * 128)
                    Z = pz[gi % 2]; BZ = Bpz[gi % 2]; E = Et[gi % 4]; BE = BEt[gi % 4]; L = Lt[gi % 4]; BL = BLt[gi % 4]
                    M = Mt[gi % 6]; BM = BMt[gi % 6]; Rp = Rt[gi % 5]; BRp = BRt[gi % 5]; Rn = Rt[(gi + 1) % 5]; BRn = BRt[(gi + 1) % 5]
                    if first:
                        fw.op("pool", lambda e: e.memset(Rp[:], 0.0), writes=[BRp])
                    fw.op("pe", lambda e: e.matmul(Z[:], lhsT=KT[h2_][:, ks], rhs=QT[h2_][:, qs], start=True, stop=True),
                          reads=[BKT[h2_], BQT[h2_]], writes=[BZ])
                    fw.op("act", lambda e: e.activation(out=E[:], in_=Z[:], func=AF.Exp), reads=[BZ], writes=[BE])
                    fw.op("act", lambda e: e.activation(out=L[:], in_=E[:], func=AF.Ln, bias=1.0), reads=[BE], writes=[BL])
                    if masked:
                        fw.op("dve", lambda e: e.tensor_tensor(out=L[:], in0=L[:], in1=M[:], op=ALU.mult), reads=[BM], writes=[BL])
                    if not last:
                        fw.op("pool", lambda e: e.tensor_tensor(out=Rn[:], in0=Rp[:], in1=L[:], op=ALU.add), reads=[BRp, BL], writes=[BRn])

                def sgB(i):
                    tl, l, masked, first, last = units[i]
                    gi = gbase + i
                    E = Et[gi % 4]; BE = BEt[gi % 4]; L = Lt[gi % 4]; BL = BLt[gi % 4]; Rp = Rt[gi % 5]; BRp = BRt[gi % 5]
                    Sp = pS[gi % 2]; BSp = BpS[gi % 2]; X = Xt[gi % 2]; BX = BXt[gi % 2]; Wm = Wt[gi % 3]; BWm = BWt[gi % 3]
                    fs = []
                    if not first:
                        fs.append(lambda e: e.matmul(Sp[:], lhsT=negOnes[:], rhs=Rp[:], start=True, stop=False))
                    fs.append(lambda e: e.matmul(Sp[:], lhsT=negU[:], rhs=L[:], start=first, stop=True))
                    fw.op("pe", fs, reads=[BnegOnes, BnegU, BL] + ([] if first else [BRp]), writes=[BSp])
                    fw.op("act", lambda e: e.activation(out=X[:], in_=Sp[:], func=AF.Exp), reads=[BSp], writes=[BX])
                    fw.op("dve", lambda e: e.tensor_tensor(out=Wm[:], in0=E[:], in1=X[:], op=ALU.mult), reads=[BE, BX], writes=[BWm])
                    if masked:
                        M = Mt[gi % 6]; BM = BMt[gi % 6]
                        fw.op("dve", lambda e: e.tensor_tensor(out=Wm[:], in0=Wm[:], in1=M[:], op=ALU.mult), reads=[BM], writes=[BWm])

                def sgC(i):
                    tl, l, masked, first, last = units[i]
                    gi = gbase + i
                    qs = slice(tl * 512, (tl + 1) * 512)
                    Wm = Wt[gi % 3]; BWm = BWt[gi % 3]
                    fw.op("pe", lambda e: e.matmul(pacc[:], lhsT=V[h2_][:, l, :], rhs=Wm[:], start=first, stop=last),
                          reads=[BV[h2_], BWm], writes=[Bpacc])
                    if last:
                        fw.op("dve", lambda e: e.tensor_copy(out=y_bT[:, hd, qs], in_=pacc[:]), reads=[Bpacc], writes=[By_b])

                sgM(0)
                sgM(1)
                for step in range(NU + 3):
                    if step + 2 < NU:
                        sgM(step + 2)
                    if step < NU:
                        sgA(step)
                    if 0 <= step - 2 < NU:
                        sgB(step - 2)
                    if 0 <= step - 3 < NU:
                        sgC(step - 3)
                    want = min(nj, ((step + 1) * nj + NU - 1) // NU) if nj else 0
                    while jdone < want:
                        jobs[jdone]()
                        jdone += 1
                while jdone < nj:
                    jobs[jdone]()
                    jdone += 1
                gbase += NU
            fw.barrier()

        with contextlib.ExitStack() as st:
            mT = SB(st, "mT", [128, NDC, 1024], BF16); BmT = Buf("mT")
            hto2 = SB(st, "hto2", [128, 2, NDC, 512], BF16); Bhto2 = Buf("hto2")
            for tl in range(2):
                fw.dma("sp", [(hto2[:, tl, :, :].rearrange("p c t -> p (c t)"), hTo[tl * 128:(tl + 1) * 128, :])], Bhto2, reads=[BhTo])
            wga = [SB(st, f"wga{i}", [128, NDC, 128], BF16) for i in range(2)]; Bwga = [Buf(f"wga{i}") for i in range(2)]
            wgb = [SB(st, f"wgb{i}", [128, NDC, 128], BF16) for i in range(2)]; Bwgb = [Buf(f"wgb{i}") for i in range(2)]
            wa = [SB(st, f"wa{i}", [128, 8, 128], BF16) for i in range(2)]; Bwa = [Buf(f"wa{i}") for i in range(2)]
            wb = [SB(st, f"wb{i}", [128, 8, 128], BF16) for i in range(2)]; Bwb = [Buf(f"wb{i}") for i in range(2)]
            sga = SB(st, "sga", [128, 512], F32); Bsga = Buf("sga")
            sgb = SB(st, "sgb", [128, 512], F32); Bsgb = Buf("sgb")
            ta = SB(st, "ta", [128, 512], F32); Bta = Buf("ta")
            tb_ = SB(st, "tb_", [128, 512], F32); Btb = Buf("tb_")
            pga = PS(st, "pga", [128, 512], F32); Bpga = Buf("pga")
            pgb = PS(st, "pgb", [128, 512], F32); Bpgb = Buf("pgb")
            pA = PS(st, "pA", [128, 512], F32); BpA = Buf("pA")
            pB = PS(st, "pB", [128, 512], F32); BpB = Buf("pB")
            w_in_v = w_in.rearrange("(c p) f -> p c f", p=128)
            w_ba_v = w_ba.rearrange("(c p) f -> p c f", p=128)
            w_bb_v = w_bb.rearrange("(c p) f -> p c f", p=128)
            for j in range(NDC):
                i2 = j % 2
                js = slice(j * 128, (j + 1) * 128)
                fw.dma("pool", [(wga[i2][:, q * 4:(q + 1) * 4, :], w_in_v[:, q * 4:(q + 1) * 4, 5120 + j * 128:5120 + (j + 1) * 128]) for q in range(4)], Bwga[i2])
                fw.dma("pool", [(wgb[i2][:, q * 4:(q + 1) * 4, :], w_in_v[:, q * 4:(q + 1) * 4, 7168 + j * 128:7168 + (j + 1) * 128]) for q in range(4)], Bwgb[i2])
                fw.dma("pool", [(wa[i2][:, q * 4:(q + 1) * 4, :], w_ba_v[:, q * 4:(q + 1) * 4, js]) for q in range(2)], Bwa[i2])
                fw.dma("pool", [(wb[i2][:, q * 4:(q + 1) * 4, :], w_bb_v[:, q * 4:(q + 1) * 4, js]) for q in range(2)], Bwb[i2])
                for tl in range(2):
                    qs = slice(tl * 512, (tl + 1) * 512)
                    fw.op("pe", [(lambda e, dc=dc: e.matmul(pga[:], lhsT=wga[i2][:, dc, :], rhs=hto2[:, tl, dc, :],
                                                            start=(dc == 0), stop=(dc == NDC - 1))) for dc in range(NDC)],
                          reads=[Bwga[i2], Bhto2], writes=[Bpga])
                    fw.op("act", lambda e: e.activation(out=sga[:], in_=pga[:], func=AF.Sigmoid), reads=[Bpga], writes=[Bsga])
                    fw.op("pe", [(lambda e, dc=dc: e.matmul(pgb[:], lhsT=wgb[i2][:, dc, :], rhs=hto2[:, tl, dc, :],
                                                            start=(dc == 0), stop=(dc == NDC - 1))) for dc in range(NDC)],
                          reads=[Bwgb[i2], Bhto2], writes=[Bpgb])
                    fw.op("act", lambda e: e.activation(out=sgb[:], in_=pgb[:], func=AF.Sigmoid), reads=[Bpgb], writes=[Bsgb])
                    fw.op("pe", [(lambda e, cc=cc: e.matmul(pA[:], lhsT=wa[i2][:, cc, :], rhs=y_aT[:, cc, qs],
                                                            start=(cc == 0), stop=(cc == 7))) for cc in range(8)],
                          reads=[Bwa[i2], By_a], writes=[BpA])
                    fw.op("dve", lambda e: e.tensor_tensor(out=ta[:], in0=pA[:], in1=sga[:], op=ALU.mult), reads=[BpA, Bsga], writes=[Bta])
                    fw.op("pe", [(lambda e, cc=cc: e.matmul(pB[:], lhsT=wb[i2][:, cc, :], rhs=y_bT[:, cc, qs],
                                                            start=(cc == 0), stop=(cc == 7))) for cc in range(8)],
                          reads=[Bwb[i2], By_b], writes=[BpB])
                    fw.op("dve", lambda e: e.tensor_tensor(out=tb_[:], in0=pB[:], in1=sgb[:], op=ALU.mult), reads=[BpB, Bsgb], writes=[Btb])
                    fw.op("dve", lambda e: e.tensor_tensor(out=mT[:, j, qs], in0=ta[:], in1=tb_[:], op=ALU.add), reads=[Bta, Btb], writes=[BmT])
            fw.dma("sp", [(mTs[:, :], mT[:].rearrange("p c t -> p (c t)"))], BmTs, reads=[BmT])
            if debug:
                fw.dma("sp", [(dbg_ya[:, :], y_aT[:].rearrange("p c t -> p (c t)")), (dbg_yb[:, :], y_bT[:].rearrange("p c t -> p (c t)"))],
                       Bdbg, reads=[By_a, By_b])
            fw.barrier()
        stA.close()

        with contextlib.ExitStack() as st6:
            x1 = SB(st6, "x1", [128, 8, D], F32)
            Bx1 = [Buf(f"x1_{b}") for b in range(8)]
            h2 = SB(st6, "h2", [128, 8, D], BF16)
            Bh2 = [Buf(f"h2_{b}") for b in range(8)]
            LG = SB(st6, "LG", [128, 8, 36], F32); BLG = Buf("LG")
            for b in range(8):
                fw.dma("sp", [(x1[:, b, :], x_own[b * 128:(b + 1) * 128, :])], Bx1[b])

            with contextlib.ExitStack() as st:
                mT = SB(st, "mT2", [128, NDC, 1024], BF16); BmT = Buf("mT2")
                fw.dma("sp", [(mT[:].rearrange("p c t -> p (c t)"), mTs[:, :])], BmT, reads=[BmTs])
                wo = [SB(st, f"wo{i}", [128, NDC, 256], BF16) for i in range(2)]; Bwo = [Buf(f"wo{i}") for i in range(2)]
                po = [PS(st, f"po{i}", [128, 256], F32) for i in range(2)]; Bpo = [Buf(f"po{i}") for i in range(2)]
                w_out_v = w_out.rearrange("(c p) f -> p c f", p=128)
                k = 0
                for et in range(8):
                    es = slice(et * 256, (et + 1) * 256)
                    fw.dma("pool", [(wo[et % 2][:, q * 4:(q + 1) * 4, :], w_out_v[:, q * 4:(q + 1) * 4, es]) for q in range(4)], Bwo[et % 2])
                    for b in range(8):
                        P = po[k % 2]; BP = Bpo[k % 2]
                        fw.op("pe", [(lambda e, dc=dc: e.matmul(P[:], lhsT=mT[:, dc, b * 128:(b + 1) * 128], rhs=wo[et % 2][:, dc, :],
                                                                start=(dc == 0), stop=(dc == NDC - 1))) for dc in range(NDC)],
                              reads=[BmT, Bwo[et % 2]], writes=[BP])
                        fw.op("dve", lambda e: e.tensor_tensor(out=x1[:, b, es], in0=x1[:, b, es], in1=P[:], op=ALU.add),
                              reads=[BP], writes=[Bx1[b]])
                        k += 1
                fw.barrier()

            with contextlib.ExitStack() as st:
                gf = SB(st, "gf", [128, D], F32); Bgf = Buf("gf")
                fw.dma("sp", [(gf[:], g_ffn[0:1, :].partition_broadcast(128))], Bgf)
                wr = SB(st, "wr", [128, NDC, 36], F32); Bwr = Buf("wr")
                fw.dma("sp", [(wr[:, :, 0:4], w_rg.rearrange("(c p) f -> p c f", p=128)),
                              (wr[:, :, 4:36], w_re.rearrange("(c p) f -> p c f", p=128))], Bwr)
                brt = SB(st, "brt", [128, 36], F32); Bbrt = Buf("brt")
                fw.dma("sp", [(brt[:, 0:4], b_rg[0:1, :].partition_broadcast(128)),
                              (brt[:, 4:36], b_re[0:1, :].partition_broadcast(128))], Bbrt)
                junk2 = SB(st, "junk2", [128, D], BF16); Bj2 = Buf("junk2")
                h2f = [SB(st, f"h2f{i}", [128, D], F32) for i in range(2)]; Bh2f = [Buf(f"h2f{i}") for i in range(2)]
                h2T = [SB(st, f"h2T{i}", [128, NDC, 128], F32) for i in range(2)]; Bh2T = [Buf(f"h2T{i}") for i in range(2)]
                st2 = SB(st, "st2", [128, 8, 4], F32); Bst2 = Buf("st2")
                ptf = [PS(st, f"ptf{i}", [128, 512], F32) for i in range(4)]; Bptf = [Buf(f"ptf{i}") for i in range(4)]
                plg = PS(st, "plg", [128, 64], F32); Bplg = Buf("plg")
                for b in range(8):
                    fw.op("act", lambda e: e.activation(out=junk2[:], in_=x1[:, b, :], func=AF.Square, accum_out=st2[:, b, 0:1]),
                          reads=[Bx1[b]], writes=[Bj2, Bst2])
                    fw.op("act", lambda e: e.activation(out=st2[:, b, 1:2], in_=st2[:, b, 0:1], func=AF.Ln, scale=1.0 / D, bias=EPS), reads=[Bst2], writes=[Bst2])
                    fw.op("act", lambda e: e.activation(out=st2[:, b, 2:3], in_=st2[:, b, 1:2], func=AF.Exp, scale=-0.5), reads=[Bst2], writes=[Bst2])
                    HF = h2f[b % 2]; BHF = Bh2f[b % 2]
                    fw.op("dve", lambda e: e.scalar_tensor_tensor(out=HF[:], in0=x1[:, b, :], scalar=st2[:, b, 2:3], in1=gf[:],
                                                                  op0=ALU.mult, op1=ALU.mult), reads=[Bx1[b], Bst2, Bgf], writes=[BHF])
                    fw.op("pool", lambda e: e.tensor_copy(out=h2[:, b, :], in_=HF[:]), reads=[BHF], writes=[Bh2[b]])
                    HT2 = h2T[b % 2]; BHT2 = Bh2T[b % 2]
                    for q4 in range(4):
                        PT = ptf[q4]; BPT = Bptf[q4]
                        fw.op("pe", [(lambda e, i=i: e.transpose(out=PT[:, i * 128:(i + 1) * 128],
                                                                  in_=HF[:, (q4 * 4 + i) * 128:(q4 * 4 + i + 1) * 128], identity=identf[:]))
                                     for i in range(4)], reads=[BHF, Bidentf], writes=[BPT])
                        fw.op("act" if q4 % 2 == 0 else "dve",
                              (lambda e: e.copy(out=HT2[:, q4 * 4:(q4 + 1) * 4, :].rearrange("p c t -> p (c t)"), in_=PT[:])) if q4 % 2 == 0 else
                              (lambda e: e.tensor_copy(out=HT2[:, q4 * 4:(q4 + 1) * 4, :].rearrange("p c t -> p (c t)"), in_=PT[:])),
                              reads=[BPT], writes=[BHT2])
                    fw.op("pe", [(lambda e, dc=dc: e.matmul(plg[:, 0:36], lhsT=HT2[:, dc, :], rhs=wr[:, dc, :],
                                                            start=(dc == 0), stop=(dc == NDC - 1))) for dc in range(NDC)],
                          reads=[BHT2, Bwr], writes=[Bplg])
                    fw.op("dve", lambda e: e.tensor_tensor(out=LG[:, b, :], in0=plg[:, 0:36], in1=brt[:], op=ALU.add),
                          reads=[Bplg, Bbrt], writes=[BLG])
                if debug:
                    fw.dma("sp", [(dbg_x1[:, :], x1[:].rearrange("p b f -> p (b f)")), (dbg_lg[:, :], LG[:].rearrange("p b f -> p (b f)"))],
                           Bdbg, reads=Bx1 + [BLG])
                fw.barrier()

            with contextlib.ExitStack() as st:
                G = SB(st, "G", [128, 8, 32], F32); BG = Buf("G")
                MK = SB(st, "MK", [128, 8, 32], BF16); BMK = Buf("MK")
                MKf = SB(st, "MKf", [128, 8, 32], F32); BMKf = Buf("MKf")
                POS = SB(st, "POS", [128, 8, 32], F32); BPOS = Buf("POS")
                rs = SB(st, "rs", [128, 8, 64], F32); Brs = Buf("rs")
                pb = [PS(st, f"pb{i}", [128, 512], F32) for i in range(8)]; Bpb = [Buf(f"pb{i}") for i in range(8)]
                ppos = pb[0]; Bppos = Bpb[0]
                def route_ops(b):
                    seq = []
                    R_ = rs[:, b, :]
                    lgp = LG[:, b, 0:4]
                    lep = LG[:, b, 4:36]
                    def A(f, w=()):
                        seq.append(("dve", f, list(w)))
                    A(lambda e: e.reduce_max(out=R_[:, 0:1], in_=lgp, axis=AX.X))
                    A(lambda e: e.tensor_scalar(out=R_[:, 1:2], in0=R_[:, 0:1], scalar1=-1.0, scalar2=None, op0=ALU.mult))
                    A(lambda e: e.tensor_scalar(out=R_[:, 4:8], in0=lgp, scalar1=R_[:, 0:1], scalar2=None, op0=ALU.is_equal))
                    seq.append(("act", lambda e: e.activation(out=R_[:, 24:28], in_=lgp, func=AF.Exp, bias=R_[:, 1:2], accum_out=R_[:, 2:3]), []))
                    A(lambda e: e.reciprocal(out=R_[:, 3:4], in_=R_[:, 2:3]))
                    A(lambda e: e.tensor_scalar(out=R_[:, 8:16], in0=lep[:, 0:8], scalar1=R_[:, 4:5], scalar2=None, op0=ALU.mult))
                    for g in range(1, 4):
                        A(lambda e, g=g: e.scalar_tensor_tensor(out=R_[:, 8:16], in0=lep[:, g * 8:(g + 1) * 8], scalar=R_[:, 4 + g:5 + g],
                                                                in1=R_[:, 8:16], op0=ALU.mult, op1=ALU.add))
                    A(lambda e: e.reduce_max(out=R_[:, 16:17], in_=R_[:, 8:16], axis=AX.X))
                    A(lambda e: e.tensor_scalar(out=R_[:, 32:40], in0=R_[:, 8:16], scalar1=R_[:, 16:17], scalar2=None, op0=ALU.is_equal))
                    A(lambda e: e.scalar_tensor_tensor(out=R_[:, 40:48], in0=R_[:, 32:40], scalar=-1.0e30, in1=R_[:, 8:16], op0=ALU.mult, op1=ALU.add))
                    A(lambda e: e.reduce_max(out=R_[:, 17:18], in_=R_[:, 40:48], axis=AX.X))
                    A(lambda e: e.tensor_scalar(out=R_[:, 48:56], in0=R_[:, 40:48], scalar1=R_[:, 17:18], scalar2=None, op0=ALU.is_equal))
                    A(lambda e: e.tensor_tensor(out=R_[:, 18:19], in0=R_[:, 17:18], in1=R_[:, 16:17], op=ALU.subtract))
                    seq.append(("act", lambda e: e.activation(out=R_[:, 19:20], in_=R_[:, 18:19], func=AF.Exp), []))
                    A(lambda e: e.tensor_scalar(out=R_[:, 20:21], in0=R_[:, 19:20], scalar1=1.0, scalar2=None, op0=ALU.add))
                    A(lambda e: e.reciprocal(out=R_[:, 21:22], in_=R_[:, 20:21]))
                    A(lambda e: e.tensor_tensor(out=R_[:, 22:23], in0=R_[:, 19:20], in1=R_[:, 21:22], op=ALU.mult))
                    A(lambda e: e.tensor_tensor(out=R_[:, 23:24], in0=R_[:, 21:22], in1=R_[:, 3:4], op=ALU.mult))
                    A(lambda e: e.tensor_tensor(out=R_[:, 24:25], in0=R_[:, 22:23], in1=R_[:, 3:4], op=ALU.mult))
                    A(lambda e: e.tensor_scalar(out=R_[:, 56:64], in0=R_[:, 32:40], scalar1=R_[:, 23:24], scalar2=None, op0=ALU.mult))
                    A(lambda e: e.scalar_tensor_tensor(out=R_[:, 56:64], in0=R_[:, 48:56], scalar=R_[:, 24:25], in1=R_[:, 56:64], op0=ALU.mult, op1=ALU.add))
                    for g in range(4):
                        A(lambda e, g=g: e.tensor_scalar(out=G[:, b, g * 8:(g + 1) * 8], in0=R_[:, 56:64], scalar1=R_[:, 4 + g:5 + g], scalar2=None, op0=ALU.mult), w=[BGb[b]])
                    A(lambda e: e.tensor_scalar(out=MKf[:, b, :], in0=G[:, b, :], scalar1=0.0, scalar2=None, op0=ALU.is_gt), w=[BMKfb[b]])
                    A(lambda e: e.tensor_copy(out=MK[:, b, :], in_=MKf[:, b, :]), w=[BMKb[b]])
                    return seq

                Brsb = [Buf(f"rs{b}") for b in range(8)]
                BGb = [Buf(f"G{b}") for b in range(8)]
                BMKfb = [Buf(f"MKf{b}") for b in range(8)]
                BMKb = [Buf(f"MK{b}") for b in range(8)]
                seqs = [route_ops(b) for b in range(8)]
                for k in range(len(seqs[0])):
                    for b in range(8):
                        eng_, f_, w_ = seqs[b][k]
                        fw.op(eng_, f_, reads=[BLG, Brsb[b], BGb[b], BMKfb[b]], writes=[Brsb[b]] + w_)
                for b in range(8):
                    fns = [lambda e: e.matmul(ppos[:, 0:32], lhsT=stri[:], rhs=MK[:, b, :], start=True, stop=(b == 0))]
                    for b2 in range(b):
                        fns.append(lambda e, b2=b2: e.matmul(ppos[:, 0:32], lhsT=ones[:], rhs=MK[:, b2, :], start=False, stop=(b2 == b - 1)))
                    fw.op("pe", fns, reads=[Bstri, Bones] + BMKb, writes=[Bppos])
                    fw.op("dve", lambda e: e.tensor_copy(out=POS[:, b, :], in_=ppos[:, 0:32]), reads=[Bppos], writes=[BPOS])

                if debug:
                    fw.dma("sp", [(dbg_g[:, :], G[:].rearrange("p b f -> p (b f)")), (dbg_pos[:, :], POS[:].rearrange("p b f -> p (b f)"))],
                           Bdbg, reads=BGb + [BPOS])
                P01 = [SB(st, f"P01_{i}", [128, 8, 128], BF16) for i in range(2)]; BP01 = [Buf(f"P01_{i}") for i in range(2)]
                Pw = [SB(st, f"Pw_{i}", [128, 8, 128], BF16) for i in range(2)]; BPw = [Buf(f"Pw_{i}") for i in range(2)]
                PwT = [SB(st, f"PwT_{i}", [128, 8, 128], BF16) for i in range(3)]; BPwT = [Buf(f"PwT_{i}") for i in range(3)]
                XgT = SB(st, "XgT", [128, NDC, 128], BF16); BXgT = Buf("XgT")
                NSL = 10
                gu = [SB(st, f"gu{i}", [128, 2, DE], BF16) for i in range(NSL)]; Bgu = [Buf(f"gu{i}") for i in range(NSL)]
                NDL = 6
                dn = [SB(st, f"dn{i}", [128, D], BF16) for i in range(NDL)]; Bdn = [Buf(f"dn{i}") for i in range(NDL)]
                sg = SB(st, "sg", [128, NFC * 128], F32); Bsg = Buf("sg")
                hdT = SB(st, "hdT", [128, NFC, 128], BF16); BhdT = Buf("hdT")
                ybr = [SB(st, f"yb{i}", [128, D], BF16) for i in range(2)]; Bybr = [Buf(f"yb{i}") for i in range(2)]
                w_gate_v = w_gate.rearrange("(e c p) f -> e c p f", p=128, c=NDC)
                w_up_v = w_up.rearrange("(e c p) f -> e c p f", p=128, c=NDC)
                w_down_v = w_down.rearrange("(e c p) f -> e c p f", p=128, c=NFC)
                si = 0
                di = 0
                cstate = {"ci": 0}
                pending = []
                for ex in range(n_moe_experts):
                    i2 = ex % 2
                    for b in range(8):
                        fw.op("dve", [lambda e: e.tensor_scalar(out=P01[i2][:, b, :], in0=iota_s[:], scalar1=POS[:, b, ex:ex + 1],
                                                                scalar2=MKf[:, b, ex:ex + 1], op0=ALU.is_equal, op1=ALU.mult),
                                      lambda e: e.tensor_scalar(out=Pw[i2][:, b, :], in0=iota_s[:], scalar1=POS[:, b, ex:ex + 1],
                                                                scalar2=G[:, b, ex:ex + 1], op0=ALU.is_equal, op1=ALU.mult)],
                              reads=[Biota, BPOS] + BMKfb + BGb, writes=[BP01[i2], BPw[i2]])
                    fw.op("pe", [(lambda e, b=b: e.matmul(pb[b // 4][:, (b % 4) * 128:(b % 4 + 1) * 128], lhsT=Pw[i2][:, b, :], rhs=ident[:],
                                                          start=True, stop=True)) for b in range(8)],
                          reads=[BPw[i2], Bident], writes=[Bpb[0], Bpb[1]])
                    for hh in range(2):
                        fw.op("act", lambda e: e.copy(out=PwT[ex % 3][:, hh * 4:(hh + 1) * 4, :].rearrange("p b t -> p (b t)"), in_=pb[hh][:]),
                              reads=[Bpb[hh]], writes=[BPwT[ex % 3]])
                    for q4 in range(4):
                        PB = pb[4 + q4]; BPB = Bpb[4 + q4]
                        fw.op("pe", [(lambda e, i=i, b=b: e.matmul(PB[:, i * 128:(i + 1) * 128],
                                                                   lhsT=h2[:, b, (q4 * 4 + i) * 128:(q4 * 4 + i + 1) * 128],
                                                                   rhs=P01[i2][:, b, :], start=(b == 0), stop=(b == 7)))
                                     for i in range(4) for b in range(8)], reads=Bh2 + [BP01[i2]], writes=[BPB])
                        fw.op("act" if q4 % 2 == 0 else "dve",
                              (lambda e: e.copy(out=XgT[:, q4 * 4:(q4 + 1) * 4, :].rearrange("p c t -> p (c t)"), in_=PB[:])) if q4 % 2 == 0 else
                              (lambda e: e.tensor_copy(out=XgT[:, q4 * 4:(q4 + 1) * 4, :].rearrange("p c t -> p (c t)"), in_=PB[:])),
                              reads=[BPB], writes=[BXgT])
                    for dc in range(NDC):
                        GU = gu[si % NSL]; BGU = Bgu[si % NSL]
                        fw.dma("pool", [(GU[:, 0, :], w_gate_v[ex, dc]), (GU[:, 1, :], w_up_v[ex, dc])], BGU)
                        fns = []
                        for fc in range(NFC):
                            fns.append(lambda e, fc=fc: e.matmul(pb[fc // 4][:, (fc % 4) * 128:(fc % 4 + 1) * 128], lhsT=GU[:, 0, fc * 128:(fc + 1) * 128],
                                                                 rhs=XgT[:, dc, :], start=(dc == 0 and fc % 4 == 0), stop=(dc == NDC - 1)))
                            fns.append(lambda e, fc=fc: e.matmul(pb[2 + fc // 4][:, (fc % 4) * 128:(fc % 4 + 1) * 128], lhsT=GU[:, 1, fc * 128:(fc + 1) * 128],
                                                                 rhs=XgT[:, dc, :], start=(dc == 0 and fc % 4 == 0), stop=(dc == NDC - 1)))
                        fw.op("pe", fns, reads=[BGU, BXgT], writes=[Bpb[0], Bpb[1], Bpb[2], Bpb[3]])
                        si += 1
                        for _ in range(2):
                            if pending:
                                pending.pop(0)()
                    while pending:
                        pending.pop(0)()
                    for hh in range((NFC + 3) // 4):
                        w_ = min(4, NFC - hh * 4) * 128
                        fw.op("act", lambda e: e.activation(out=sg[:, hh * 512:hh * 512 + w_], in_=pb[hh][:, 0:w_], func=AF.Silu), reads=[Bpb[hh]], writes=[Bsg])
                        fw.op("dve", lambda e: e.tensor_tensor(out=hdT[:, hh * 4:hh * 4 + w_ // 128, :].rearrange("p c t -> p (c t)"),
                                                               in0=sg[:, hh * 512:hh * 512 + w_], in1=pb[2 + hh][:, 0:w_], op=ALU.mult),
                              reads=[Bsg, Bpb[2 + hh]], writes=[BhdT])
                    for fc in range(NFC):
                        DN = dn[di % NDL]; BDN = Bdn[di % NDL]
                        fw.dma("pool", [(DN[:], w_down_v[ex, fc])], BDN)
                        fw.op("pe", [(lambda e, dm=dm: e.matmul(pb[4 + dm][:], lhsT=hdT[:, fc, :], rhs=DN[:, dm * 512:(dm + 1) * 512],
                                                                start=(fc == 0), stop=(fc == NFC - 1))) for dm in range(4)],
                              reads=[BDN, BhdT], writes=[Bpb[4 + dm] for dm in range(4)])
                        di += 1
                    for dm in range(4):
                        fw.op("act", lambda e: e.copy(out=ybr[i2][:, dm * 512:(dm + 1) * 512], in_=pb[4 + dm][:]), reads=[Bpb[4 + dm]], writes=[Bybr[i2]])
                    if ex % 2 == 0 and ex + 1 < n_moe_experts:
                        continue
                    grp_ = [ex] if ex % 2 == 0 else [ex - 1, ex]

                    def mk_item(b, dm, grp_=grp_):
                        def item():
                            PC = pb[4 + cstate["ci"] % 4]; BPC = Bpb[4 + cstate["ci"] % 4]
                            cstate["ci"] += 1
                            fw.op("pe", [(lambda e, k=k, ee=ee: e.matmul(PC[:], lhsT=PwT[ee % 3][:, b, :], rhs=ybr[ee % 2][:, dm * 512:(dm + 1) * 512],
                                                                         start=(k == 0), stop=(k == len(grp_) - 1))) for k, ee in enumerate(grp_)],
                                  reads=[BPwT[ee % 3] for ee in grp_] + [Bybr[ee % 2] for ee in grp_], writes=[BPC])
                            fw.op("dve", lambda e: e.tensor_tensor(out=x1[:, b, dm * 512:(dm + 1) * 512], in0=x1[:, b, dm * 512:(dm + 1) * 512],
                                                                   in1=PC[:], op=ALU.add), reads=[BPC], writes=[Bx1[b]])
                        return item
                    pending.extend(mk_item(b, dm) for b in range(8) for dm in range(4))
                while pending:
                    pending.pop(0)()
                if debug:
                    fw.dma("sp", [(dbg_x2[:, :], x1[:].rearrange("p b f -> p (b f)"))], Bdbg, reads=Bx1)
                fw.barrier()

            with contextlib.ExitStack() as st:
                gfin = SB(st, "gfin", [128, D], F32); Bgfin = Buf("gfin")
                fw.dma("sp", [(gfin[:], g_fin[0:1, :].partition_broadcast(128))], Bgfin)
                junk3 = SB(st, "junk3", [128, D], BF16); Bj3 = Buf("junk3")
                st3 = SB(st, "st3", [128, 8, 4], F32); Bst3 = Buf("st3")
                ot = [SB(st, f"ot{i}", [128, D], F32) for i in range(2)]; Bot = [Buf(f"ot{i}") for i in range(2)]
                Bout = Buf("out")
                for b in range(8):
                    fw.op("act", lambda e: e.activation(out=junk3[:], in_=x1[:, b, :], func=AF.Square, accum_out=st3[:, b, 0:1]),
                          reads=[Bx1[b]], writes=[Bj3, Bst3])
                    fw.op("act", lambda e: e.activation(out=st3[:, b, 1:2], in_=st3[:, b, 0:1], func=AF.Ln, scale=1.0 / D, bias=EPS), reads=[Bst3], writes=[Bst3])
                    fw.op("act", lambda e: e.activation(out=st3[:, b, 2:3], in_=st3[:, b, 1:2], func=AF.Exp, scale=-0.5), reads=[Bst3], writes=[Bst3])
                    O = ot[b % 2]; BO = Bot[b % 2]
                    fw.op("dve", lambda e: e.scalar_tensor_tensor(out=O[:], in0=x1[:, b, :], scalar=st3[:, b, 2:3], in1=gfin[:],
                                                                  op0=ALU.mult, op1=ALU.mult), reads=[Bx1[b], Bst3, Bgfin], writes=[BO])
                    fw.dma("sp", [(out_d[b * 128:(b + 1) * 128, :], O[:])], Bout, reads=[BO])
                fw.barrier()
    return nc


NT_FULL = 16
NCORES = 8


def own_rows(c, NT):
    a = np.arange(c * 512, (c + 1) * 512)
    b = np.arange((NT - 1 - c) * 512, (NT - c) * 512)
    return np.concatenate([a, b])


def make_in_maps(inputs, NT, ncores):
    f = lambda a: np.ascontiguousarray(np.asarray(a, dtype=np.float32))
    x = f(inputs["x"]).reshape(-1, D)
    DE = inputs["w_gate"].shape[-1]
    shared = {
        "x_all": x,
        "g_mix": f(inputs["g_mix"]).reshape(1, D),
        "w_in": f(inputs["w_in"]).reshape(D, 9216),
        "ln_v_g": f(inputs["ln_v_g"]).reshape(1, 1024),
        "ln_v_b": f(inputs["ln_v_b"]).reshape(1, 1024),
        "w_spatial": f(inputs["w_spatial"]).reshape(8, 128, 128),
        "b_spatial": f(inputs["b_spatial"]).reshape(1, 1024),
        "w_branch_a": f(inputs["w_branch_a"]).reshape(1024, D),
        "w_branch_b": f(inputs["w_branch_b"]).reshape(1024, D),
        "w_out": f(inputs["w_out"]).reshape(D, D),
        "g_ffn": f(inputs["g_ffn"]).reshape(1, D),
        "w_router_group": f(inputs["w_router_group"]).reshape(D, 4),
        "b_router_group": f(inputs["b_router_group"]).reshape(1, 4),
        "w_router_expert": f(inputs["w_router_expert"]).reshape(D, 32),
        "b_router_expert": f(inputs["b_router_expert"]).reshape(1, 32),
        "w_gate": f(inputs["w_gate"]).reshape(32 * D, DE),
        "w_up": f(inputs["w_up"]).reshape(32 * D, DE),
        "w_down": f(inputs["w_down"]).reshape(32 * DE, D),
        "g_final": f(inputs["g_final"]).reshape(1, D),
    }
    maps = []
    for c in range(ncores):
        rows = own_rows(c, NT)
        m = dict(shared)
        m["x_own"] = np.ascontiguousarray(x[rows])
        m["qpos"] = rows.astype(np.float32).reshape(1, 1024)
        maps.append(m)
    return maps


def kernel(**inputs):
    NT = NT_FULL
    DE = inputs["w_gate"].shape[-1]
    nc = build_nc(NT, DE)
    in_maps = make_in_maps(inputs, NT, NCORES)
    res = run_bass_kernel_spmd(nc, in_maps, core_ids=list(range(NCORES)))
    out = np.zeros((NT * 512, D), np.float32)
    for c in range(NCORES):
        out[own_rows(c, NT)] = res.results[c]["out"]
    return out.reshape(1, NT * 512, D)
```

```python
import contextlib
import numpy as np
import concourse.bass as bass
import concourse.mybir as mybir
from concourse.bass_utils import run_bass_kernel_spmd

F32 = mybir.dt.float32
BF16 = mybir.dt.bfloat16
AF = mybir.ActivationFunctionType
ALU = mybir.AluOpType
AX = mybir.AxisListType

D = 2048
NDC = 16
EPS = 1e-6
NEG = -30000.0


class Buf:
    __slots__ = ("name", "w", "r", "dsem", "dcnt")

    def __init__(self, name):
        self.name = name
        self.w = None
        self.r = []
        self.dsem = None
        self.dcnt = 0


class FW:
    def __init__(self, nc, stack):
        self.nc = nc
        self.stack = stack
        self.eng = {"pe": nc.tensor, "act": nc.scalar, "dve": nc.vector, "pool": nc.gpsimd, "sp": nc.sync}
        self.sems = {}
        self.cnt = {}
        self.cur = {}
        self.waited = {k: {} for k in self.eng}
        self.nsem = 0
        self.dbufs = []
        self.nins = 0
        for k in ("pe", "act", "dve", "pool"):
            self._new_sem(k)

    def _mk(self, name):
        h = self.stack.enter_context(self.nc.semaphore(name))
        self.nsem += 1
        return h

    def _new_sem(self, k):
        name = f"s_{k}_{self.nsem}"
        self.sems[name] = self._mk(name)
        self.cnt[name] = 0
        self.cur[k] = name

    def _wait(self, k, tok):
        if tok is None:
            return
        s, v = tok
        if self.waited[k].get(s, 0) >= v:
            return
        self.eng[k].wait_ge(self.sems[s], v)
        self.waited[k][s] = v

    def _deps(self, k, reads, writes, extra):
        for b in reads:
            self._wait(k, b.w)
        for b in writes:
            self._wait(k, b.w)
            for t in b.r:
                self._wait(k, t)
        for t in extra:
            self._wait(k, t)

    @staticmethod
    def _commit(tok, reads, writes):
        for b in reads:
            b.r.append(tok)
        for b in writes:
            b.w = tok
            b.r = []

    def op(self, k, fns, reads=(), writes=(), extra=()):
        if callable(fns):
            fns = [fns]
        self._deps(k, reads, writes, extra)
        e = self.eng[k]
        ins = None
        for f in fns:
            ins = f(e)
            self.nins += 1
        s = self.cur[k]
        ins.then_inc(self.sems[s], 1)
        self.cnt[s] += 1
        tok = (s, self.cnt[s])
        self._commit(tok, reads, writes)
        return tok

    def dma(self, q, pairs, dst, reads=(), extra=()):
        self._deps(q, reads, [dst], extra)
        e = self.eng[q]
        if dst.dsem is None:
            dst.dsem = f"d_{dst.name}_{self.nsem}"
            self.sems[dst.dsem] = self._mk(dst.dsem)
            self.dbufs.append(dst)
        for (o, i) in pairs:
            e.dma_start(out=o, in_=i).then_inc(self.sems[dst.dsem], 16)
            dst.dcnt += 16
            self.nins += 1
        tok = (dst.dsem, dst.dcnt)
        self._commit(tok, reads, [dst])
        return tok

    def barrier(self, engines=("pe", "act", "dve", "pool", "sp")):
        toks = [(s, self.cnt[s]) for s in self.cnt if self.cnt[s] > 0]
        toks += [(b.dsem, b.dcnt) for b in self.dbufs if b.dcnt > 0]
        for k in engines:
            for t in toks:
                self._wait(k, t)


def build_nc(NT, DE, n_moe_experts=32, debug=False):
    S = NT * 512
    NB = S // 128
    NFC = DE // 128
    SCALE = 128 ** -0.5
    nc = bass.Bass("TRN2", target_bir_lowering=False)

    def din(name, shape):
        return nc.dram_tensor(name, shape, F32, kind="ExternalInput").ap()

    x_all = din("x_all", [S, D])
    x_own = din("x_own", [1024, D])
    qpos_d = din("qpos", [1, 1024])
    g_mix = din("g_mix", [1, D])
    w_in = din("w_in", [D, 9216])
    ln_g = din("ln_v_g", [1, 1024])
    ln_b = din("ln_v_b", [1, 1024])
    w_sp = din("w_spatial", [8, 128, 128])
    b_sp = din("b_spatial", [1, 1024])
    w_ba = din("w_branch_a", [1024, D])
    w_bb = din("w_branch_b", [1024, D])
    w_out = din("w_out", [D, D])
    g_ffn = din("g_ffn", [1, D])
    w_rg = din("w_router_group", [D, 4])
    b_rg = din("b_router_group", [1, 4])
    w_re = din("w_router_expert", [D, 32])
    b_re = din("b_router_expert", [1, 32])
    w_gate = din("w_gate", [32 * D, DE])
    w_up = din("w_up", [32 * D, DE])
    w_down = din("w_down", [32 * DE, D])
    g_fin = din("g_final", [1, D])
    out_d = nc.dram_tensor("out", [1024, D], F32, kind="ExternalOutput").ap()
    hTs = nc.dram_tensor("hTs", [NT * 128, NDC * 512], BF16).ap()
    hTo = nc.dram_tensor("hTo", [2 * 128, NDC * 512], BF16).ap()
    mTs = nc.dram_tensor("mTs", [128, NDC * 1024], BF16).ap()
    wdb = nc.dram_tensor("wdb", [32 * DE, D], BF16).ap()
    if debug:
        dbg_ya = nc.dram_tensor("dbg_ya", [128, 8 * 1024], BF16, kind="ExternalOutput").ap()
        dbg_yb = nc.dram_tensor("dbg_yb", [128, 8 * 1024], BF16, kind="ExternalOutput").ap()
        dbg_x1 = nc.dram_tensor("dbg_x1", [128, 8 * D], F32, kind="ExternalOutput").ap()
        dbg_lg = nc.dram_tensor("dbg_lg", [128, 8 * 36], F32, kind="ExternalOutput").ap()
        dbg_g = nc.dram_tensor("dbg_g", [128, 8 * 32], F32, kind="ExternalOutput").ap()
        dbg_pos = nc.dram_tensor("dbg_pos", [128, 8 * 32], F32, kind="ExternalOutput").ap()
        dbg_x2 = nc.dram_tensor("dbg_x2", [128, 8 * D], F32, kind="ExternalOutput").ap()
    Bdbg = Buf("dbg")

    with contextlib.ExitStack() as top:
        fw = FW(nc, top)

        def SB(st, name, shape, dt):
            return st.enter_context(nc.sbuf_tensor("sb_" + name, shape, dt))

        def PS(st, name, shape, dt):
            return st.enter_context(nc.psum_tensor("ps_" + name, shape, dt))

        ident = SB(top, "ident", [128, 128], BF16); Bident = Buf("ident")
        identf = SB(top, "identf", [128, 128], F32); Bidentf = Buf("identf")
        negU = SB(top, "negU", [128, 128], BF16); BnegU = Buf("negU")
        negOnes = SB(top, "negOnes", [128, 128], BF16); BnegOnes = Buf("negOnes")
        ones = SB(top, "ones", [128, 128], BF16); Bones = Buf("ones")
        onesf = SB(top, "onesf", [128, 128], F32); Bonesf = Buf("onesf")
        stri = SB(top, "stri", [128, 128], BF16); Bstri = Buf("stri")
        kpos = SB(top, "kpos", [128, NB], F32); Bkpos = Buf("kpos")
        iota_s = SB(top, "iota_s", [128, 128], F32); Biota = Buf("iota")
        stA = contextlib.ExitStack()
        qpos = SB(stA, "qpos", [128, 1024], F32); Bqpos = Buf("qpos")
        y_aT = SB(stA, "y_aT", [128, 8, 1024], BF16); By_a = Buf("y_aT")
        y_bT = SB(stA, "y_bT", [128, 8, 1024], BF16); By_b = Buf("y_bT")
        BmTs = Buf("mTs")

        fw.op("pool", lambda e: e.memset(ones[:], 1.0), writes=[Bones])
        fw.op("pool", lambda e: e.memset(onesf[:], 1.0), writes=[Bonesf])
        fw.op("pool", lambda e: e.memset(negOnes[:], -1.0), writes=[BnegOnes])
        fw.op("pool", lambda e: e.affine_select(out=ident[:], in_=ones[:], pattern=[[-1, 128]],
                                                compare_op=ALU.is_equal, fill=0.0, base=0, channel_multiplier=1),
              reads=[Bones], writes=[Bident])
        fw.op("pool", lambda e: e.affine_select(out=identf[:], in_=onesf[:], pattern=[[-1, 128]],
                                                compare_op=ALU.is_equal, fill=0.0, base=0, channel_multiplier=1),
              reads=[Bonesf], writes=[Bidentf])
        fw.op("pool", lambda e: e.affine_select(out=negU[:], in_=negOnes[:], pattern=[[-1, 128]],
                                                compare_op=ALU.is_ge, fill=0.0, base=0, channel_multiplier=1),
              reads=[BnegOnes], writes=[BnegU])
        fw.op("pool", lambda e: e.affine_select(out=stri[:], in_=ones[:], pattern=[[1, 128]],
                                                compare_op=ALU.is_gt, fill=0.0, base=0, channel_multiplier=-1),
              reads=[Bones], writes=[Bstri])
        fw.op("pool", lambda e: e.iota(kpos[:], pattern=[[128, NB]], base=0, channel_multiplier=1,
                                       allow_small_or_imprecise_dtypes=True), writes=[Bkpos])
        fw.op("pool", lambda e: e.iota(iota_s[:], pattern=[[1, 128]], base=0, channel_multiplier=0,
                                       allow_small_or_imprecise_dtypes=True), writes=[Biota])
        fw.dma("sp", [(qpos[:], qpos_d[0:1, :].partition_broadcast(128))], Bqpos)

        BhTs = Buf("hTs")
        BhTo = Buf("hTo")
        with contextlib.ExitStack() as st:
            gm = SB(st, "gm", [128, D], F32); Bgm = Buf("gm")
            fw.dma("sp", [(gm[:], g_mix[0:1, :].partition_broadcast(128))], Bgm)
            xt = [SB(st, f"xt{i}", [128, D], F32) for i in range(4)]
            Bxt = [Buf(f"xt{i}") for i in range(4)]
            junk = SB(st, "junk", [128, D], BF16); Bjunk = Buf("junk")
            stt = [SB(st, f"stt{i}", [128, 4], F32) for i in range(4)]
            Bstt = [Buf(f"stt{i}") for i in range(4)]
            hb = [SB(st, f"hb{i}", [128, D], BF16) for i in range(3)]
            Bhb = [Buf(f"hb{i}") for i in range(3)]
            hT4 = [SB(st, f"hT4{i}", [128, NDC, 512], BF16) for i in range(2)]
            BhT4 = [Buf(f"hT4{i}") for i in range(2)]
            pt = [PS(st, f"pt{i}", [128, D], BF16) for i in range(2)]
            Bpt = [Buf(f"pt{i}") for i in range(2)]
            blocks = [(tile, blk) for tile in range(NT + 2) for blk in range(4)]

            def p1_s1(bi):
                tile, blk = blocks[bi]
                own = tile >= NT
                src = x_own if own else x_all
                t_loc = tile - NT if own else tile
                r0 = (t_loc * 4 + blk) * 128
                X = xt[bi % 4]; BX = Bxt[bi % 4]
                sT = stt[bi % 4]; BsT = Bstt[bi % 4]
                H = hb[bi % 3]; BH = Bhb[bi % 3]
                fw.dma("sp", [(X[:], src[r0:r0 + 128, :])], BX)
                fw.op("act", lambda e: e.activation(out=junk[:], in_=X[:], func=AF.Square, accum_out=sT[:, 0:1]),
                      reads=[BX], writes=[Bjunk, BsT])
                fw.op("act", lambda e: e.activation(out=sT[:, 1:2], in_=sT[:, 0:1], func=AF.Ln, scale=1.0 / D, bias=EPS),
                      reads=[BsT], writes=[BsT])
                fw.op("act", lambda e: e.activation(out=sT[:, 2:3], in_=sT[:, 1:2], func=AF.Exp, scale=-0.5),
                      reads=[BsT], writes=[BsT])
                fw.op("dve", lambda e: e.scalar_tensor_tensor(out=H[:], in0=X[:], scalar=sT[:, 2:3], in1=gm[:],
                                                              op0=ALU.mult, op1=ALU.mult),
                      reads=[BX, BsT, Bgm], writes=[BH])

            def p1_s2(bi):
                tile, blk = blocks[bi]
                own = tile >= NT
                t_loc = tile - NT if own else tile
                h4 = hT4[tile % 2]; Bh4 = BhT4[tile % 2]
                H = hb[bi % 3]; BH = Bhb[bi % 3]
                P = pt[bi % 2]; BP = Bpt[bi % 2]
                fw.op("pe", [(lambda e, dc=dc: e.transpose(out=P[:, dc * 128:(dc + 1) * 128],
                                                            in_=H[:, dc * 128:(dc + 1) * 128], identity=ident[:]))
                             for dc in range(NDC)], reads=[BH, Bident], writes=[BP])
                if bi % 2 == 0:
                    fw.op("act", lambda e: e.copy(out=h4[:, :, blk * 128:(blk + 1) * 128],
                                                  in_=P[:].rearrange("p (c t) -> p c t", c=NDC)),
                          reads=[BP], writes=[Bh4])
                else:
                    fw.op("dve", lambda e: e.tensor_copy(out=h4[:, :, blk * 128:(blk + 1) * 128],
                                                         in_=P[:].rearrange("p (c t) -> p c t", c=NDC)),
                          reads=[BP], writes=[Bh4])
                if blk == 3:
                    dstd = hTo if own else hTs
                    fw.dma("pool", [(dstd[t_loc * 128:(t_loc + 1) * 128, :], h4[:].rearrange("p c t -> p (c t)"))],
                           BhTo if own else BhTs, reads=[Bh4])

            p1_s1(0)
            for bi in range(len(blocks)):
                if bi + 1 < len(blocks):
                    p1_s1(bi + 1)
                p1_s2(bi)
            fw.barrier()

        with contextlib.ExitStack() as st:
            Wu = SB(st, "Wu", [128, NDC, 1024], BF16); BWu = Buf("Wu")
            Wv = SB(st, "Wv", [128, NDC, 1024], BF16); BWv = Buf("Wv")
            w_in_v = w_in.rearrange("(c p) f -> p c f", p=128)
            for half in range(2):
                fw.dma("pool", [(Wu[:, half * 8:(half + 1) * 8, :], w_in_v[:, half * 8:(half + 1) * 8, 0:1024])], BWu)
                fw.dma("pool", [(Wv[:, half * 8:(half + 1) * 8, :], w_in_v[:, half * 8:(half + 1) * 8, 1024:2048])], BWv)
            lg_t = SB(st, "lg_t", [128, 1024], F32); Blg = Buf("lng")
            lb_t = SB(st, "lb_t", [128, 1024], F32); Blb = Buf("lnb")
            fw.dma("sp", [(lg_t[:], ln_g[0:1, :].partition_broadcast(128))], Blg)
            fw.dma("sp", [(lb_t[:], ln_b[0:1, :].partition_broadcast(128))], Blb)
            bsr_f = SB(st, "bsr_f", [1, 1024], F32); Bbsrf = Buf("bsrf")
            bsr = SB(st, "bsr", [1, 1024], BF16); Bbsr = Buf("bsr")
            fw.dma("sp", [(bsr_f[:], b_sp[0:1, :])], Bbsrf)
            fw.op("dve", lambda e: e.tensor_copy(out=bsr[:], in_=bsr_f[:]), reads=[Bbsrf], writes=[Bbsr])
            wsl = SB(st, "wsl", [128, 8, 128], F32); Bwsl = Buf("wsl")
            fw.dma("sp", [(wsl[:], w_sp.rearrange("g t s -> t g s"))], Bwsl)
            wsT = SB(st, "wsT", [128, 8, 128], BF16); BwsT = Buf("wsT")
            wsTf = SB(st, "wsTf", [128, 8, 128], F32); BwsTf = Buf("wsTf")
            pw = PS(st, "pw", [128, 1024], F32); Bpw = Buf("pw")
            fw.op("pe", [(lambda e, g=g: e.transpose(out=pw[:, g * 128:(g + 1) * 128], in_=wsl[:, g, :], identity=identf[:]))
                         for g in range(8)], reads=[Bwsl, Bidentf], writes=[Bpw])
            fw.op("dve", lambda e: e.tensor_copy(out=wsTf[:].rearrange("p g t -> p (g t)"), in_=pw[:]), reads=[Bpw], writes=[BwsTf])
            for g in range(8):
                fw.op("pool", lambda e: e.affine_select(out=wsT[:, g, :], in_=wsTf[:, g, :], pattern=[[1, 128]],
                                                        compare_op=ALU.is_ge, fill=0.0, base=0, channel_multiplier=-1),
                      reads=[BwsTf], writes=[BwsT])
            hto = SB(st, "hto", [128, NDC, 512], BF16); Bhto = Buf("hto")
            guT = SB(st, "guT", [128, 8, 512], BF16); BguT = Buf("guT")
            pu = [PS(st, f"pu{i}", [128, 512], F32) for i in range(2)]
            Bpu = [Buf(f"pu{i}") for i in range(2)]
            pv = PS(st, "pv", [128, 1024], F32); Bpv = Buf("pv")
            pm = PS(st, "pm", [128, 1024], F32); Bpm = Buf("pm")
            t1 = SB(st, "t1", [128, 1024], F32); Bt1 = Buf("t1")
            t2 = SB(st, "t2", [128, 1024], F32); Bt2 = Buf("t2")
            gv = SB(st, "gv", [128, 1024], F32); Bgv = Buf("gv")
            vn = SB(st, "vn", [128, 1024], BF16); Bvn = Buf("vn")
            u1 = SB(st, "u1", [128, 512], F32); Bu1 = Buf("u1")
            u2 = SB(st, "u2", [128, 512], F32); Bu2 = Buf("u2")
            sv = SB(st, "sv", [128, 8], F32); Bsv = Buf("sv")
            GC = 1.5957691216057308

            def gelu_ops(dst_ap, src_ap, a_ap, b_ap, reads, Ba, Bb, Bdst):
                fw.op("act", lambda e: e.copy(out=b_ap, in_=src_ap), reads=reads, writes=[Bb])
                fw.op("dve", lambda e: e.tensor_tensor(out=a_ap, in0=b_ap, in1=b_ap, op=ALU.mult), reads=[Bb], writes=[Ba])
                fw.op("dve", lambda e: e.tensor_scalar(out=a_ap, in0=a_ap, scalar1=0.044715, scalar2=1.0, op0=ALU.mult, op1=ALU.add),
                      reads=[Ba], writes=[Ba])
                fw.op("dve", lambda e: e.tensor_tensor(out=a_ap, in0=a_ap, in1=b_ap, op=ALU.mult), reads=[Ba, Bb], writes=[Ba])
                fw.op("act", lambda e: e.activation(out=a_ap, in_=a_ap, func=AF.Sigmoid, scale=GC), reads=[Ba], writes=[Ba])
                fw.op("dve", lambda e: e.tensor_tensor(out=dst_ap, in0=a_ap, in1=b_ap, op=ALU.mult), reads=[Ba, Bb], writes=[Bdst])

            u1r = [u1, SB(st, "u1b", [128, 512], F32)]; Bu1r = [Bu1, Buf("u1b")]
            u2r = [u2, SB(st, "u2b", [128, 512], F32)]; Bu2r = [Bu2, Buf("u2b")]
            t1r = [t1, SB(st, "t1b", [128, 1024], F32)]; Bt1r = [Bt1, Buf("t1b")]
            t2r = [t2, SB(st, "t2b", [128, 1024], F32)]; Bt2r = [Bt2, Buf("t2b")]
            pvr = [pv, pw]; Bpvr = [Bpv, Bpw]

            def gelu_s1(a_ap, b_ap, src_ap, Ba, Bb, Bsrc):
                fw.op("act", lambda e: e.copy(out=b_ap, in_=src_ap), reads=[Bsrc], writes=[Bb])
                fw.op("dve", lambda e: e.tensor_tensor(out=a_ap, in0=b_ap, in1=b_ap, op=ALU.mult), reads=[Bb], writes=[Ba])
                fw.op("dve", lambda e: e.tensor_scalar(out=a_ap, in0=a_ap, scalar1=0.044715, scalar2=1.0, op0=ALU.mult, op1=ALU.add),
                      reads=[Ba], writes=[Ba])
                fw.op("dve", lambda e: e.tensor_tensor(out=a_ap, in0=a_ap, in1=b_ap, op=ALU.mult), reads=[Ba, Bb], writes=[Ba])

            def gelu_s2(dst_ap, a_ap, b_ap, Ba, Bb, Bdst):
                fw.op("act", lambda e: e.activation(out=a_ap, in_=a_ap, func=AF.Sigmoid, scale=GC), reads=[Ba], writes=[Ba])
                fw.op("dve", lambda e: e.tensor_tensor(out=dst_ap, in0=a_ap, in1=b_ap, op=ALU.mult), reads=[Ba, Bb], writes=[Bdst])

            ucnt = {"u": 0, "v": 0}
            for tl in range(2):
                fw.dma("sp", [(hto[:].rearrange("p c t -> p (c t)"), hTo[tl * 128:(tl + 1) * 128, :])], Bhto, reads=[BhTo])

                def u_s1(cc):
                    k = (ucnt["u"] + cc) % 2
                    P = pu[k]; BP = Bpu[k]
                    fw.op("pe", [(lambda e, dc=dc: e.matmul(P[:], lhsT=Wu[:, dc, cc * 128:(cc + 1) * 128], rhs=hto[:, dc, :],
                                                            start=(dc == 0), stop=(dc == NDC - 1))) for dc in range(NDC)],
                          reads=[BWu, Bhto], writes=[BP])
                    gelu_s1(u1r[k][:], u2r[k][:], P[:], Bu1r[k], Bu2r[k], BP)

                def u_s2(cc):
                    k = (ucnt["u"] + cc) % 2
                    gelu_s2(guT[:, cc, :], u1r[k][:], u2r[k][:], Bu1r[k], Bu2r[k], BguT)

                u_s1(0)
                for cc in range(8):
                    if cc + 1 < 8:
                        u_s1(cc + 1)
                    u_s2(cc)
                ucnt["u"] += 8

                def v_s1(blk):
                    k = (ucnt["v"] + blk) % 2
                    PV = pvr[k]; BPV = Bpvr[k]
                    fw.op("pe", [(lambda e, dc=dc, hf=hf: e.matmul(PV[:, hf * 512:(hf + 1) * 512], lhsT=hto[:, dc, blk * 128:(blk + 1) * 128],
                                                                  rhs=Wv[:, dc, hf * 512:(hf + 1) * 512],
                                                                  start=(dc == 0), stop=(dc == NDC - 1)))
                                 for hf in range(2) for dc in range(NDC)], reads=[BWv, Bhto], writes=[BPV])
                    gelu_s1(t1r[k][:], t2r[k][:], PV[:], Bt1r[k], Bt2r[k], BPV)

                def v_s2(blk):
                    k = (ucnt["v"] + blk) % 2
                    tb = tl * 4 + blk
                    T1 = t1r[k]; T2 = t2r[k]; BT1 = Bt1r[k]; BT2 = Bt2r[k]
                    gelu_s2(gv[:], T1[:], T2[:], BT1, BT2, Bgv)
                    fw.op("act", lambda e: e.activation(out=T2[:], in_=gv[:], func=AF.Copy, accum_out=sv[:, 0:1]), reads=[Bgv], writes=[BT2, Bsv])
                    fw.op("act", lambda e: e.activation(out=T2[:], in_=gv[:], func=AF.Square, accum_out=sv[:, 1:2]), reads=[Bgv], writes=[BT2, Bsv])
                    fw.op("dve", lambda e: e.tensor_scalar(out=sv[:, 2:3], in0=sv[:, 0:1], scalar1=1.0 / 1024, scalar2=None, op0=ALU.mult), reads=[Bsv], writes=[Bsv])
                    fw.op("dve", lambda e: e.tensor_tensor(out=sv[:, 3:4], in0=sv[:, 2:3], in1=sv[:, 2:3], op=ALU.mult), reads=[Bsv], writes=[Bsv])
                    fw.op("dve", lambda e: e.scalar_tensor_tensor(out=sv[:, 4:5], in0=sv[:, 1:2], scalar=1.0 / 1024, in1=sv[:, 3:4],
                                                                  op0=ALU.mult, op1=ALU.subtract), reads=[Bsv], writes=[Bsv])
                    fw.op("act", lambda e: e.activation(out=sv[:, 5:6], in_=sv[:, 4:5], func=AF.Ln, bias=EPS), reads=[Bsv], writes=[Bsv])
                    fw.op("act", lambda e: e.activation(out=sv[:, 6:7], in_=sv[:, 5:6], func=AF.Exp, scale=-0.5), reads=[Bsv], writes=[Bsv])
                    fw.op("dve", lambda e: e.tensor_scalar(out=T1[:], in0=gv[:], scalar1=sv[:, 2:3], scalar2=sv[:, 6:7],
                                                           op0=ALU.subtract, op1=ALU.mult), reads=[Bgv, Bsv], writes=[BT1])
                    fw.op("dve", lambda e: e.tensor_tensor(out=T1[:], in0=T1[:], in1=lg_t[:], op=ALU.mult), reads=[Blg], writes=[BT1])
                    fw.op("dve", lambda e: e.tensor_tensor(out=vn[:], in0=T1[:], in1=lb_t[:], op=ALU.add), reads=[BT1, Blb], writes=[Bvn])
                    fns = []
                    for g in range(8):
                        fns.append(lambda e, g=g: e.matmul(pm[:, g * 128:(g + 1) * 128], lhsT=vn[:, g * 128:(g + 1) * 128],
                                                           rhs=wsT[:, g, :], start=True, stop=False))
                        fns.append(lambda e, g=g: e.matmul(pm[:, g * 128:(g + 1) * 128], lhsT=ones[0:1, :],
                                                           rhs=bsr[0:1, g * 128:(g + 1) * 128], start=False, stop=True))
                    fw.op("pe", fns, reads=[Bvn, BwsT, Bones, Bbsr], writes=[Bpm])
                    fw.op("dve", lambda e: e.tensor_tensor(out=y_aT[:, :, tb * 128:(tb + 1) * 128],
                                                           in0=pm[:].rearrange("p (g t) -> p g t", g=8),
                                                           in1=guT[:, :, blk * 128:(blk + 1) * 128], op=ALU.mult),
                          reads=[Bpm, BguT], writes=[By_a])

                v_s1(0)
                for blk in range(4):
                    if blk + 1 < 4:
                        v_s1(blk + 1)
                    v_s2(blk)
                ucnt["v"] += 4
            fw.barrier()

        with contextlib.ExitStack() as st:
            KT = [SB(st, f"KT{i}", [128, S], BF16) for i in range(2)]; BKT = [Buf(f"KT{i}") for i in range(2)]
            V = [SB(st, f"V{i}", [128, NB, 128], BF16) for i in range(2)]; BV = [Buf(f"V{i}") for i in range(2)]
            QT = [SB(st, f"QT{i}", [128, 1024], BF16) for i in range(2)]; BQT = [Buf(f"QT{i}") for i in range(2)]
            Wq = [SB(st, f"Wq{i}", [128, NDC, 128], BF16) for i in range(2)]; BWq = [Buf(f"Wq{i}") for i in range(2)]
            Wk = [SB(st, f"Wk{i}", [128, NDC, 128], BF16) for i in range(2)]; BWk = [Buf(f"Wk{i}") for i in range(2)]
            Wvh = [SB(st, f"Wvh{i}", [128, NDC, 128], BF16) for i in range(2)]; BWvh = [Buf(f"Wvh{i}") for i in range(2)]
            htl = [SB(st, f"htl{i}", [128, NDC, 512], BF16) for i in range(2)]
            Bhtl = [Buf(f"htl{i}") for i in range(2)]
            Et = [SB(st, f"Et{i}", [128, 512], F32) for i in range(4)]; BEt = [Buf(f"Et{i}") for i in range(4)]
            Xt = [SB(st, f"Xt{i}", [128, 512], F32) for i in range(2)]; BXt = [Buf(f"Xt{i}") for i in range(2)]
            Lt = [SB(st, f"Lt{i}", [128, 512], BF16) for i in range(4)]; BLt = [Buf(f"Lt{i}") for i in range(4)]
            Rt = [SB(st, f"Rt{i}", [128, 512], BF16) for i in range(5)]; BRt = [Buf(f"Rt{i}") for i in range(5)]
            Wt = [SB(st, f"Wt{i}", [128, 512], BF16) for i in range(3)]; BWt = [Buf(f"Wt{i}") for i in range(3)]
            Mt = [SB(st, f"Mt{i}", [128, 512], BF16) for i in range(4)]; BMt = [Buf(f"Mt{i}") for i in range(4)]
            pz = [PS(st, f"pz{i}", [128, 512], F32) for i in range(2)]; Bpz = [Buf(f"pz{i}") for i in range(2)]
            pS = [PS(st, f"pS{i}", [128, 512], F32) for i in range(2)]; BpS = [Buf(f"pS{i}") for i in range(2)]
            pacc = PS(st, "pacc", [128, 512], F32); Bpacc = Buf("pacc")
            pk = [PS(st, f"pk{i}", [128, 512], F32) for i in range(2)]; Bpk = [Buf(f"pk{i}") for i in range(2)]
            ppv = PS(st, "ppv", [128, 512], F32); Bppv = Buf("ppv")
            w_in_v = w_in.rearrange("(c p) f -> p c f", p=128)
            cnt = {"ht": 0, "pk": 0}

            def load_w(hd):
                h2_ = hd % 2
                for (Wt_, BWt_, off) in ((Wq[h2_], BWq[h2_], 2048), (Wk[h2_], BWk[h2_], 3072), (Wvh[h2_], BWvh[h2_], 4096)):
                    fw.dma("pool", [(Wt_[:, q * 4:(q + 1) * 4, :], w_in_v[:, q * 4:(q + 1) * 4, off + hd * 128:off + (hd + 1) * 128])
                                    for q in range(4)], BWt_)

            def proj_tile_pieces(hd, tile):
                h2_ = hd % 2
                stt_ = {}

                def p0():
                    HT = htl[cnt["ht"] % 2]; BHT = Bhtl[cnt["ht"] % 2]; cnt["ht"] += 1
                    P = pk[cnt["pk"] % 2]; BP = Bpk[cnt["pk"] % 2]; cnt["pk"] += 1
                    stt_.update(HT=HT, BHT=BHT, P=P, BP=BP)
                    fw.dma("sp", [(HT[:].rearrange("p c t -> p (c t)"), hTs[tile * 128:(tile + 1) * 128, :])], BHT, reads=[BhTs])
                    fw.op("pe", [(lambda e, dc=dc: e.matmul(P[:], lhsT=Wk[h2_][:, dc, :], rhs=HT[:, dc, :],
                                                            start=(dc == 0), stop=False)) for dc in range(NDC // 2)],
                          reads=[BWk[h2_], BHT], writes=[BP])

                def p1():
                    HT, BHT, P, BP = stt_["HT"], stt_["BHT"], stt_["P"], stt_["BP"]
                    fw.op("pe", [(lambda e, dc=dc: e.matmul(P[:], lhsT=Wk[h2_][:, dc, :], rhs=HT[:, dc, :],
                                                            start=False, stop=(dc == NDC - 1))) for dc in range(NDC // 2, NDC)],
                          reads=[BWk[h2_], BHT], writes=[BP])
                    fw.op("act", lambda e: e.copy(out=KT[h2_][:, tile * 512:(tile + 1) * 512], in_=P[:]), reads=[BP], writes=[BKT[h2_]])

                def pv_(b):
                    def f():
                        HT, BHT = stt_["HT"], stt_["BHT"]
                        fw.op("pe", [(lambda e, dc=dc: e.matmul(ppv[:, b * 128:(b + 1) * 128], lhsT=HT[:, dc, b * 128:(b + 1) * 128],
                                                                rhs=Wvh[h2_][:, dc, :], start=(dc == 0), stop=(dc == NDC - 1)))
                                     for dc in range(NDC)], reads=[BWvh[h2_], BHT], writes=[Bppv])
                        if b == 3:
                            fw.op("dve", lambda e: e.tensor_copy(out=V[h2_][:, tile * 4:(tile + 1) * 4, :].rearrange("p b d -> p (b d)"), in_=ppv[:]),
                                  reads=[Bppv], writes=[BV[h2_]])
                    return f
                return [p0, p1, pv_(0), pv_(1), pv_(2), pv_(3)]

            def proj_q_pieces(hd, tl):
                h2_ = hd % 2
                stt_ = {}

                def p0():
                    HT = htl[cnt["ht"] % 2]; BHT = Bhtl[cnt["ht"] % 2]; cnt["ht"] += 1
                    P = pk[cnt["pk"] % 2]; BP = Bpk[cnt["pk"] % 2]; cnt["pk"] += 1
                    stt_.update(HT=HT, BHT=BHT, P=P, BP=BP)
                    fw.dma("sp", [(HT[:].rearrange("p c t -> p (c t)"), hTo[tl * 128:(tl + 1) * 128, :])], BHT, reads=[BhTo])
                    fw.op("pe", [(lambda e, dc=dc: e.matmul(P[:], lhsT=Wq[h2_][:, dc, :], rhs=HT[:, dc, :],
                                                            start=(dc == 0), stop=False)) for dc in range(NDC // 2)],
                          reads=[BWq[h2_], BHT], writes=[BP])

                def p1():
                    HT, BHT, P, BP = stt_["HT"], stt_["BHT"], stt_["P"], stt_["BP"]
                    fw.op("pe", [(lambda e, dc=dc: e.matmul(P[:], lhsT=Wq[h2_][:, dc, :], rhs=HT[:, dc, :],
                                                            start=False, stop=(dc == NDC - 1))) for dc in range(NDC // 2, NDC)],
                          reads=[BWq[h2_], BHT], writes=[BP])
                    fw.op("act", lambda e: e.mul(out=QT[h2_][:, tl * 512:(tl + 1) * 512], in_=P[:], mul=SCALE), reads=[BP], writes=[BQT[h2_]])
                return [p0, p1]

            def proj_jobs(hd):
                jobs = []
                for t in range(NT):
                    jobs += proj_tile_pieces(hd, t)
                for t in range(2):
                    jobs += proj_q_pieces(hd, t)
                return jobs

            Bwdb = Buf("wdb")
            CROWS = 256
            conv_chunks = [(r0, min(CROWS, 32 * DE - r0)) for r0 in range(0, 32 * DE, CROWS)]
            conv_state = {"done": 0, "step": 0}

            def conv_tick(total_steps):
                conv_state["step"] += 1
                want = min(len(conv_chunks), (conv_state["step"] * len(conv_chunks) + total_steps - 1) // total_steps)
                while conv_state["done"] < want:
                    r0, nr = conv_chunks[conv_state["done"]]
                    fw.dma("pool", [(wdb[r0:r0 + nr, :], w_down[r0:r0 + nr, :])], Bwdb)
                    conv_state["done"] += 1

            units = []
            for tl in range(2):
                nkb = NB // 2 if tl == 0 else NB
                for un in range(nkb):
                    l = nkb - 1 - un
                    units.append((tl, l, (tl == 0) or (l >= NB // 2), un == 0, un == nkb - 1))
            NU = len(units)
            gbase = 0
            load_w(0)
            for j in proj_jobs(0):
                j()
            for hd in range(8):
                h2_ = hd % 2
                jobs = []
                if hd + 1 < 8:
                    load_w(hd + 1)
                    jobs = proj_jobs(hd + 1)
                nj = len(jobs)
                jdone = 0

                def sgM(i):
                    tl, l, masked, first, last = units[i]
                    gi = gbase + i
                    qs = slice(tl * 512, (tl + 1) * 512)
                    M = Mt[gi % 4]; BM = BMt[gi % 4]
                    if masked:
                        fw.op("dve", lambda e: e.tensor_scalar(out=M[:], in0=qpos[:, qs], scalar1=kpos[:, l:l + 1], scalar2=NEG,
                                                               op0=ALU.is_le, op1=ALU.mult), reads=[Bqpos, Bkpos], writes=[BM])

                def sgA(i):
                    tl, l, masked, first, last = units[i]
                    gi = gbase + i
                    qs = slice(tl * 512, (tl + 1) * 512); ks = slice(l * 128, (l + 1) * 128)
                    Z = pz[gi % 2]; BZ = Bpz[gi % 2]; E = Et[gi % 4]; BE = BEt[gi % 4]; L = Lt[gi % 4]; BL = BLt[gi % 4]
                    M = Mt[gi % 4]; BM = BMt[gi % 4]; Rp = Rt[gi % 5]; BRp = BRt[gi % 5]; Rn = Rt[(gi + 1) % 5]; BRn = BRt[(gi + 1) % 5]
                    if first:
                        fw.op("pool", lambda e: e.memset(Rp[:], 0.0), writes=[BRp])
                    fz = [lambda e: e.matmul(Z[:], lhsT=KT[h2_][:, ks], rhs=QT[h2_][:, qs], start=True, stop=not masked)]
                    if masked:
                        fz.append(lambda e: e.matmul(Z[:], lhsT=ident[:], rhs=M[:], start=False, stop=True))
                    fw.op("pe", fz, reads=[BKT[h2_], BQT[h2_], Bident] + ([BM] if masked else []), writes=[BZ])
                    fw.op("act", lambda e: e.activation(out=E[:], in_=Z[:], func=AF.Exp), reads=[BZ], writes=[BE])
                    fw.op("act", lambda e: e.activation(out=L[:], in_=E[:], func=AF.Ln, bias=1.0), reads=[BE], writes=[BL])
                    if not last:
                        fw.op("pool", lambda e: e.tensor_tensor(out=Rn[:], in0=Rp[:], in1=L[:], op=ALU.add), reads=[BRp, BL], writes=[BRn])

                def sgB(i):
                    tl, l, masked, first, last = units[i]
                    gi = gbase + i
                    E = Et[gi % 4]; BE = BEt[gi % 4]; L = Lt[gi % 4]; BL = BLt[gi % 4]; Rp = Rt[gi % 5]; BRp = BRt[gi % 5]
                    Sp = pS[gi % 2]; BSp = BpS[gi % 2]; X = Xt[gi % 2]; BX = BXt[gi % 2]; Wm = Wt[gi % 3]; BWm = BWt[gi % 3]
                    fs = []
                    if not first:
                        fs.append(lambda e: e.matmul(Sp[:], lhsT=negOnes[:], rhs=Rp[:], start=True, stop=False))
                    fs.append(lambda e: e.matmul(Sp[:], lhsT=negU[:], rhs=L[:], start=first, stop=True))
                    fw.op("pe", fs, reads=[BnegOnes, BnegU, BL] + ([] if first else [BRp]), writes=[BSp])
                    fw.op("act", lambda e: e.activation(out=X[:], in_=Sp[:], func=AF.Exp), reads=[BSp], writes=[BX])
                    fw.op("dve", lambda e: e.tensor_tensor(out=Wm[:], in0=E[:], in1=X[:], op=ALU.mult), reads=[BE, BX], writes=[BWm])

                def sgC(i):
                    tl, l, masked, first, last = units[i]
                    gi = gbase + i
                    qs = slice(tl * 512, (tl + 1) * 512)
                    Wm = Wt[gi % 3]; BWm = BWt[gi % 3]
                    fw.op("pe", lambda e: e.matmul(pacc[:], lhsT=V[h2_][:, l, :], rhs=Wm[:], start=first, stop=last),
                          reads=[BV[h2_], BWm], writes=[Bpacc])
                    if last:
                        fw.op("dve", lambda e: e.tensor_copy(out=y_bT[:, hd, qs], in_=pacc[:]), reads=[Bpacc], writes=[By_b])

                sgM(0)
                sgM(1)
                for step in range(NU + 3):
                    if step + 2 < NU:
                        sgM(step + 2)
                    if step < NU:
                        sgA(step)
                    if 0 <= step - 2 < NU:
                        sgB(step - 2)
                    if 0 <= step - 3 < NU:
                        sgC(step - 3)
                    conv_tick(8 * (NU + 3))
                    want = min(nj, ((step + 1) * nj + NU - 1) // NU) if nj else 0
                    while jdone < want:
                        jobs[jdone]()
                        jdone += 1
                while jdone < nj:
                    jobs[jdone]()
                    jdone += 1
                gbase += NU
            conv_state["step"] = 10 ** 9
            conv_tick(1)
            fw.barrier()

        with contextlib.ExitStack() as st:
            mT = SB(st, "mT", [128, NDC, 1024], BF16); BmT = Buf("mT")
            hto2 = SB(st, "hto2", [128, 2, NDC, 512], BF16); Bhto2 = Buf("hto2")
            for tl in range(2):
                fw.dma("sp", [(hto2[:, tl, :, :].rearrange("p c t -> p (c t)"), hTo[tl * 128:(tl + 1) * 128, :])], Bhto2, reads=[BhTo])
            wga = [SB(st, f"wga{i}", [128, NDC, 128], BF16) for i in range(2)]; Bwga = [Buf(f"wga{i}") for i in range(2)]
            wgb = [SB(st, f"wgb{i}", [128, NDC, 128], BF16) for i in range(2)]; Bwgb = [Buf(f"wgb{i}") for i in range(2)]
            wa = [SB(st, f"wa{i}", [128, 8, 128], BF16) for i in range(2)]; Bwa = [Buf(f"wa{i}") for i in range(2)]
            wb = [SB(st, f"wb{i}", [128, 8, 128], BF16) for i in range(2)]; Bwb = [Buf(f"wb{i}") for i in range(2)]
            sga = SB(st, "sga", [128, 512], F32); Bsga = Buf("sga")
            sgb = SB(st, "sgb", [128, 512], F32); Bsgb = Buf("sgb")
            ta = SB(st, "ta", [128, 512], F32); Bta = Buf("ta")
            tb_ = SB(st, "tb_", [128, 512], F32); Btb = Buf("tb_")
            pga = PS(st, "pga", [128, 512], F32); Bpga = Buf("pga")
            pgb = PS(st, "pgb", [128, 512], F32); Bpgb = Buf("pgb")
            pA = PS(st, "pA", [128, 512], F32); BpA = Buf("pA")
            pB = PS(st, "pB", [128, 512], F32); BpB = Buf("pB")
            w_in_v = w_in.rearrange("(c p) f -> p c f", p=128)
            w_ba_v = w_ba.rearrange("(c p) f -> p c f", p=128)
            w_bb_v = w_bb.rearrange("(c p) f -> p c f", p=128)
            for j in range(NDC):
                i2 = j % 2
                js = slice(j * 128, (j + 1) * 128)
                fw.dma("pool", [(wga[i2][:, q * 4:(q + 1) * 4, :], w_in_v[:, q * 4:(q + 1) * 4, 5120 + j * 128:5120 + (j + 1) * 128]) for q in range(4)], Bwga[i2])
                fw.dma("pool", [(wgb[i2][:, q * 4:(q + 1) * 4, :], w_in_v[:, q * 4:(q + 1) * 4, 7168 + j * 128:7168 + (j + 1) * 128]) for q in range(4)], Bwgb[i2])
                fw.dma("pool", [(wa[i2][:, q * 4:(q + 1) * 4, :], w_ba_v[:, q * 4:(q + 1) * 4, js]) for q in range(2)], Bwa[i2])
                fw.dma("pool", [(wb[i2][:, q * 4:(q + 1) * 4, :], w_bb_v[:, q * 4:(q + 1) * 4, js]) for q in range(2)], Bwb[i2])
                for tl in range(2):
                    qs = slice(tl * 512, (tl + 1) * 512)
                    fw.op("pe", [(lambda e, dc=dc: e.matmul(pga[:], lhsT=wga[i2][:, dc, :], rhs=hto2[:, tl, dc, :],
                                                            start=(dc == 0), stop=(dc == NDC - 1))) for dc in range(NDC)],
                          reads=[Bwga[i2], Bhto2], writes=[Bpga])
                    fw.op("act", lambda e: e.activation(out=sga[:], in_=pga[:], func=AF.Sigmoid), reads=[Bpga], writes=[Bsga])
                    fw.op("pe", [(lambda e, dc=dc: e.matmul(pgb[:], lhsT=wgb[i2][:, dc, :], rhs=hto2[:, tl, dc, :],
                                                            start=(dc == 0), stop=(dc == NDC - 1))) for dc in range(NDC)],
                          reads=[Bwgb[i2], Bhto2], writes=[Bpgb])
                    fw.op("act", lambda e: e.activation(out=sgb[:], in_=pgb[:], func=AF.Sigmoid), reads=[Bpgb], writes=[Bsgb])
                    fw.op("pe", [(lambda e, cc=cc: e.matmul(pA[:], lhsT=wa[i2][:, cc, :], rhs=y_aT[:, cc, qs],
                                                            start=(cc == 0), stop=(cc == 7))) for cc in range(8)],
                          reads=[Bwa[i2], By_a], writes=[BpA])
                    fw.op("dve", lambda e: e.tensor_tensor(out=ta[:], in0=pA[:], in1=sga[:], op=ALU.mult), reads=[BpA, Bsga], writes=[Bta])
                    fw.op("pe", [(lambda e, cc=cc: e.matmul(pB[:], lhsT=wb[i2][:, cc, :], rhs=y_bT[:, cc, qs],
                                                            start=(cc == 0), stop=(cc == 7))) for cc in range(8)],
                          reads=[Bwb[i2], By_b], writes=[BpB])
                    fw.op("dve", lambda e: e.tensor_tensor(out=tb_[:], in0=pB[:], in1=sgb[:], op=ALU.mult), reads=[BpB, Bsgb], writes=[Btb])
                    fw.op("dve", lambda e: e.tensor_tensor(out=mT[:, j, qs], in0=ta[:], in1=tb_[:], op=ALU.add), reads=[Bta, Btb], writes=[BmT])
            fw.dma("sp", [(mTs[:, :], mT[:].rearrange("p c t -> p (c t)"))], BmTs, reads=[BmT])
            if debug:
                fw.dma("sp", [(dbg_ya[:, :], y_aT[:].rearrange("p c t -> p (c t)")), (dbg_yb[:, :], y_bT[:].rearrange("p c t -> p (c t)"))],
                       Bdbg, reads=[By_a, By_b])
            fw.barrier()
        stA.close()

        with contextlib.ExitStack() as st6:
            x1 = SB(st6, "x1", [128, 8, D], F32)
            Bx1 = [Buf(f"x1_{b}") for b in range(8)]
            h2 = SB(st6, "h2", [128, 8, D], BF16)
            Bh2 = [Buf(f"h2_{b}") for b in range(8)]
            LG = SB(st6, "LG", [128, 8, 36], F32); BLG = Buf("LG")
            for b in range(8):
                fw.dma("sp", [(x1[:, b, :], x_own[b * 128:(b + 1) * 128, :])], Bx1[b])

            with contextlib.ExitStack() as st:
                mT = SB(st, "mT2", [128, NDC, 1024], BF16); BmT = Buf("mT2")
                fw.dma("sp", [(mT[:].rearrange("p c t -> p (c t)"), mTs[:, :])], BmT, reads=[BmTs])
                wo = [SB(st, f"wo{i}", [128, NDC, 256], BF16) for i in range(2)]; Bwo = [Buf(f"wo{i}") for i in range(2)]
                po = [PS(st, f"po{i}", [128, 256], F32) for i in range(2)]; Bpo = [Buf(f"po{i}") for i in range(2)]
                w_out_v = w_out.rearrange("(c p) f -> p c f", p=128)
                k = 0
                for et in range(8):
                    es = slice(et * 256, (et + 1) * 256)
                    fw.dma("pool", [(wo[et % 2][:, q * 4:(q + 1) * 4, :], w_out_v[:, q * 4:(q + 1) * 4, es]) for q in range(4)], Bwo[et % 2])
                    for b in range(8):
                        P = po[k % 2]; BP = Bpo[k % 2]
                        fw.op("pe", [(lambda e, dc=dc: e.matmul(P[:], lhsT=mT[:, dc, b * 128:(b + 1) * 128], rhs=wo[et % 2][:, dc, :],
                                                                start=(dc == 0), stop=(dc == NDC - 1))) for dc in range(NDC)],
                              reads=[BmT, Bwo[et % 2]], writes=[BP])
                        fw.op("dve", lambda e: e.tensor_tensor(out=x1[:, b, es], in0=x1[:, b, es], in1=P[:], op=ALU.add),
                              reads=[BP], writes=[Bx1[b]])
                        k += 1
                fw.barrier()

            with contextlib.ExitStack() as st:
                gf = SB(st, "gf", [128, D], F32); Bgf = Buf("gf")
                fw.dma("sp", [(gf[:], g_ffn[0:1, :].partition_broadcast(128))], Bgf)
                wr = SB(st, "wr", [128, NDC, 36], F32); Bwr = Buf("wr")
                fw.dma("sp", [(wr[:, :, 0:4], w_rg.rearrange("(c p) f -> p c f", p=128)),
                              (wr[:, :, 4:36], w_re.rearrange("(c p) f -> p c f", p=128))], Bwr)
                brt = SB(st, "brt", [128, 36], F32); Bbrt = Buf("brt")
                fw.dma("sp", [(brt[:, 0:4], b_rg[0:1, :].partition_broadcast(128)),
                              (brt[:, 4:36], b_re[0:1, :].partition_broadcast(128))], Bbrt)
                junk2 = SB(st, "junk2", [128, D], BF16); Bj2 = Buf("junk2")
                h2f = [SB(st, f"h2f{i}", [128, D], F32) for i in range(2)]; Bh2f = [Buf(f"h2f{i}") for i in range(2)]
                h2T = [SB(st, f"h2T{i}", [128, NDC, 128], F32) for i in range(2)]; Bh2T = [Buf(f"h2T{i}") for i in range(2)]
                st2 = SB(st, "st2", [128, 8, 4], F32); Bst2 = Buf("st2")
                ptf = [PS(st, f"ptf{i}", [128, 512], F32) for i in range(4)]; Bptf = [Buf(f"ptf{i}") for i in range(4)]
                plg = PS(st, "plg", [128, 64], F32); Bplg = Buf("plg")
                for b in range(8):
                    fw.op("act", lambda e: e.activation(out=junk2[:], in_=x1[:, b, :], func=AF.Square, accum_out=st2[:, b, 0:1]),
                          reads=[Bx1[b]], writes=[Bj2, Bst2])
                    fw.op("act", lambda e: e.activation(out=st2[:, b, 1:2], in_=st2[:, b, 0:1], func=AF.Ln, scale=1.0 / D, bias=EPS), reads=[Bst2], writes=[Bst2])
                    fw.op("act", lambda e: e.activation(out=st2[:, b, 2:3], in_=st2[:, b, 1:2], func=AF.Exp, scale=-0.5), reads=[Bst2], writes=[Bst2])
                    HF = h2f[b % 2]; BHF = Bh2f[b % 2]
                    fw.op("dve", lambda e: e.scalar_tensor_tensor(out=HF[:], in0=x1[:, b, :], scalar=st2[:, b, 2:3], in1=gf[:],
                                                                  op0=ALU.mult, op1=ALU.mult), reads=[Bx1[b], Bst2, Bgf], writes=[BHF])
                    fw.op("pool", lambda e: e.tensor_copy(out=h2[:, b, :], in_=HF[:]), reads=[BHF], writes=[Bh2[b]])
                    HT2 = h2T[b % 2]; BHT2 = Bh2T[b % 2]
                    for q4 in range(4):
                        PT = ptf[q4]; BPT = Bptf[q4]
                        fw.op("pe", [(lambda e, i=i: e.transpose(out=PT[:, i * 128:(i + 1) * 128],
                                                                  in_=HF[:, (q4 * 4 + i) * 128:(q4 * 4 + i + 1) * 128], identity=identf[:]))
                                     for i in range(4)], reads=[BHF, Bidentf], writes=[BPT])
                        fw.op("act" if q4 % 2 == 0 else "dve",
                              (lambda e: e.copy(out=HT2[:, q4 * 4:(q4 + 1) * 4, :].rearrange("p c t -> p (c t)"), in_=PT[:])) if q4 % 2 == 0 else
                              (lambda e: e.tensor_copy(out=HT2[:, q4 * 4:(q4 + 1) * 4, :].rearrange("p c t -> p (c t)"), in_=PT[:])),
                              reads=[BPT], writes=[BHT2])
                    fw.op("pe", [(lambda e, dc=dc: e.matmul(plg[:, 0:36], lhsT=HT2[:, dc, :], rhs=wr[:, dc, :],
                                                            start=(dc == 0), stop=(dc == NDC - 1))) for dc in range(NDC)],
                          reads=[BHT2, Bwr], writes=[Bplg])
                    fw.op("dve", lambda e: e.tensor_tensor(out=LG[:, b, :], in0=plg[:, 0:36], in1=brt[:], op=ALU.add),
                          reads=[Bplg, Bbrt], writes=[BLG])
                if debug:
                    fw.dma("sp", [(dbg_x1[:, :], x1[:].rearrange("p b f -> p (b f)")), (dbg_lg[:, :], LG[:].rearrange("p b f -> p (b f)"))],
                           Bdbg, reads=Bx1 + [BLG])
                fw.barrier()

            with contextlib.ExitStack() as st:
                G = SB(st, "G", [128, 8, 32], F32); BG = Buf("G")
                MK = SB(st, "MK", [128, 8, 32], BF16); BMK = Buf("MK")
                MKf = SB(st, "MKf", [128, 8, 32], F32); BMKf = Buf("MKf")
                POS = SB(st, "POS", [128, 8, 32], F32); BPOS = Buf("POS")
                rs = SB(st, "rs", [128, 8, 64], F32); Brs = Buf("rs")
                pb = [PS(st, f"pb{i}", [128, 512], F32) for i in range(8)]; Bpb = [Buf(f"pb{i}") for i in range(8)]
                ppos = pb[0]; Bppos = Bpb[0]
                def route_ops(b):
                    seq = []
                    R_ = rs[:, b, :]
                    lgp = LG[:, b, 0:4]
                    lep = LG[:, b, 4:36]
                    def A(f, w=()):
                        seq.append(("dve", f, list(w)))
                    A(lambda e: e.reduce_max(out=R_[:, 0:1], in_=lgp, axis=AX.X))
                    A(lambda e: e.tensor_scalar(out=R_[:, 1:2], in0=R_[:, 0:1], scalar1=-1.0, scalar2=None, op0=ALU.mult))
                    A(lambda e: e.tensor_scalar(out=R_[:, 4:8], in0=lgp, scalar1=R_[:, 0:1], scalar2=None, op0=ALU.is_equal))
                    seq.append(("act", lambda e: e.activation(out=R_[:, 24:28], in_=lgp, func=AF.Exp, bias=R_[:, 1:2], accum_out=R_[:, 2:3]), []))
                    A(lambda e: e.reciprocal(out=R_[:, 3:4], in_=R_[:, 2:3]))
                    A(lambda e: e.tensor_scalar(out=R_[:, 8:16], in0=lep[:, 0:8], scalar1=R_[:, 4:5], scalar2=None, op0=ALU.mult))
                    for g in range(1, 4):
                        A(lambda e, g=g: e.scalar_tensor_tensor(out=R_[:, 8:16], in0=lep[:, g * 8:(g + 1) * 8], scalar=R_[:, 4 + g:5 + g],
                                                                in1=R_[:, 8:16], op0=ALU.mult, op1=ALU.add))
                    A(lambda e: e.reduce_max(out=R_[:, 16:17], in_=R_[:, 8:16], axis=AX.X))
                    A(lambda e: e.tensor_scalar(out=R_[:, 32:40], in0=R_[:, 8:16], scalar1=R_[:, 16:17], scalar2=None, op0=ALU.is_equal))
                    A(lambda e: e.scalar_tensor_tensor(out=R_[:, 40:48], in0=R_[:, 32:40], scalar=-1.0e30, in1=R_[:, 8:16], op0=ALU.mult, op1=ALU.add))
                    A(lambda e: e.reduce_max(out=R_[:, 17:18], in_=R_[:, 40:48], axis=AX.X))
                    A(lambda e: e.tensor_scalar(out=R_[:, 48:56], in0=R_[:, 40:48], scalar1=R_[:, 17:18], scalar2=None, op0=ALU.is_equal))
                    A(lambda e: e.tensor_tensor(out=R_[:, 18:19], in0=R_[:, 17:18], in1=R_[:, 16:17], op=ALU.subtract))
                    seq.append(("act", lambda e: e.activation(out=R_[:, 19:20], in_=R_[:, 18:19], func=AF.Exp), []))
                    A(lambda e: e.tensor_scalar(out=R_[:, 20:21], in0=R_[:, 19:20], scalar1=1.0, scalar2=None, op0=ALU.add))
                    A(lambda e: e.reciprocal(out=R_[:, 21:22], in_=R_[:, 20:21]))
                    A(lambda e: e.tensor_tensor(out=R_[:, 22:23], in0=R_[:, 19:20], in1=R_[:, 21:22], op=ALU.mult))
                    A(lambda e: e.tensor_tensor(out=R_[:, 23:24], in0=R_[:, 21:22], in1=R_[:, 3:4], op=ALU.mult))
                    A(lambda e: e.tensor_tensor(out=R_[:, 24:25], in0=R_[:, 22:23], in1=R_[:, 3:4], op=ALU.mult))
                    A(lambda e: e.tensor_scalar(out=R_[:, 56:64], in0=R_[:, 32:40], scalar1=R_[:, 23:24], scalar2=None, op0=ALU.mult))
                    A(lambda e: e.scalar_tensor_tensor(out=R_[:, 56:64], in0=R_[:, 48:56], scalar=R_[:, 24:25], in1=R_[:, 56:64], op0=ALU.mult, op1=ALU.add))
                    for g in range(4):
                        A(lambda e, g=g: e.tensor_scalar(out=G[:, b, g * 8:(g + 1) * 8], in0=R_[:, 56:64], scalar1=R_[:, 4 + g:5 + g], scalar2=None, op0=ALU.mult), w=[BGb[b]])
                    A(lambda e: e.tensor_scalar(out=MKf[:, b, :], in0=G[:, b, :], scalar1=0.0, scalar2=None, op0=ALU.is_gt), w=[BMKfb[b]])
                    A(lambda e: e.tensor_copy(out=MK[:, b, :], in_=MKf[:, b, :]), w=[BMKb[b]])
                    return seq

                Brsb = [Buf(f"rs{b}") for b in range(8)]
                BGb = [Buf(f"G{b}") for b in range(8)]
                BMKfb = [Buf(f"MKf{b}") for b in range(8)]
                BMKb = [Buf(f"MK{b}") for b in range(8)]
                seqs = [route_ops(b) for b in range(8)]
                for k in range(len(seqs[0])):
                    for b in range(8):
                        eng_, f_, w_ = seqs[b][k]
                        fw.op(eng_, f_, reads=[BLG, Brsb[b], BGb[b], BMKfb[b]], writes=[Brsb[b]] + w_)
                for b in range(8):
                    fns = [lambda e: e.matmul(ppos[:, 0:32], lhsT=stri[:], rhs=MK[:, b, :], start=True, stop=(b == 0))]
                    for b2 in range(b):
                        fns.append(lambda e, b2=b2: e.matmul(ppos[:, 0:32], lhsT=ones[:], rhs=MK[:, b2, :], start=False, stop=(b2 == b - 1)))
                    fw.op("pe", fns, reads=[Bstri, Bones] + BMKb, writes=[Bppos])
                    fw.op("dve", lambda e: e.tensor_copy(out=POS[:, b, :], in_=ppos[:, 0:32]), reads=[Bppos], writes=[BPOS])

                if debug:
                    fw.dma("sp", [(dbg_g[:, :], G[:].rearrange("p b f -> p (b f)")), (dbg_pos[:, :], POS[:].rearrange("p b f -> p (b f)"))],
                           Bdbg, reads=BGb + [BPOS])
                P01 = [SB(st, f"P01_{i}", [128, 8, 128], BF16) for i in range(2)]; BP01 = [Buf(f"P01_{i}") for i in range(2)]
                Pw = [SB(st, f"Pw_{i}", [128, 8, 128], BF16) for i in range(2)]; BPw = [Buf(f"Pw_{i}") for i in range(2)]
                PwT = [SB(st, f"PwT_{i}", [128, 8, 128], BF16) for i in range(3)]; BPwT = [Buf(f"PwT_{i}") for i in range(3)]
                XgT = SB(st, "XgT", [128, NDC, 128], BF16); BXgT = Buf("XgT")
                NSL = 10
                gu = [SB(st, f"gu{i}", [128, 2, DE], BF16) for i in range(NSL)]; Bgu = [Buf(f"gu{i}") for i in range(NSL)]
                NDL = 6
                dn = [SB(st, f"dn{i}", [128, D], BF16) for i in range(NDL)]; Bdn = [Buf(f"dn{i}") for i in range(NDL)]
                sg = SB(st, "sg", [128, NFC * 128], F32); Bsg = Buf("sg")
                hdT = SB(st, "hdT", [128, NFC, 128], BF16); BhdT = Buf("hdT")
                ybr = [SB(st, f"yb{i}", [128, D], BF16) for i in range(2)]; Bybr = [Buf(f"yb{i}") for i in range(2)]
                w_gate_v = w_gate.rearrange("(e c p) f -> e c p f", p=128, c=NDC)
                w_up_v = w_up.rearrange("(e c p) f -> e c p f", p=128, c=NDC)
                wdb_v = wdb.rearrange("(e c p) f -> e c p f", p=128, c=NFC)
                si = 0
                di = 0
                cstate = {"ci": 0}
                pending = []
                for ex in range(n_moe_experts):
                    i2 = ex % 2
                    for b in range(8):
                        fw.op("dve", [lambda e: e.tensor_scalar(out=P01[i2][:, b, :], in0=iota_s[:], scalar1=POS[:, b, ex:ex + 1],
                                                                scalar2=MKf[:, b, ex:ex + 1], op0=ALU.is_equal, op1=ALU.mult),
                                      lambda e: e.tensor_scalar(out=Pw[i2][:, b, :], in0=iota_s[:], scalar1=POS[:, b, ex:ex + 1],
                                                                scalar2=G[:, b, ex:ex + 1], op0=ALU.is_equal, op1=ALU.mult)],
                              reads=[Biota, BPOS] + BMKfb + BGb, writes=[BP01[i2], BPw[i2]])
                    fw.op("pe", [(lambda e, b=b: e.matmul(pb[b // 4][:, (b % 4) * 128:(b % 4 + 1) * 128], lhsT=Pw[i2][:, b, :], rhs=ident[:],
                                                          start=True, stop=True)) for b in range(8)],
                          reads=[BPw[i2], Bident], writes=[Bpb[0], Bpb[1]])
                    for hh in range(2):
                        fw.op("act", lambda e: e.copy(out=PwT[ex % 3][:, hh * 4:(hh + 1) * 4, :].rearrange("p b t -> p (b t)"), in_=pb[hh][:]),
                              reads=[Bpb[hh]], writes=[BPwT[ex % 3]])
                    for q4 in range(4):
                        PB = pb[4 + q4]; BPB = Bpb[4 + q4]
                        fw.op("pe", [(lambda e, i=i, b=b: e.matmul(PB[:, i * 128:(i + 1) * 128],
                                                                   lhsT=h2[:, b, (q4 * 4 + i) * 128:(q4 * 4 + i + 1) * 128],
                                                                   rhs=P01[i2][:, b, :], start=(b == 0), stop=(b == 7)))
                                     for i in range(4) for b in range(8)], reads=Bh2 + [BP01[i2]], writes=[BPB])
                        fw.op("act" if q4 % 2 == 0 else "dve",
                              (lambda e: e.copy(out=XgT[:, q4 * 4:(q4 + 1) * 4, :].rearrange("p c t -> p (c t)"), in_=PB[:])) if q4 % 2 == 0 else
                              (lambda e: e.tensor_copy(out=XgT[:, q4 * 4:(q4 + 1) * 4, :].rearrange("p c t -> p (c t)"), in_=PB[:])),
                              reads=[BPB], writes=[BXgT])
                    for dc in range(NDC):
                        GU = gu[si % NSL]; BGU = Bgu[si % NSL]
                        fw.dma("pool", [(GU[:, 0, :], w_gate_v[ex, dc]), (GU[:, 1, :], w_up_v[ex, dc])], BGU)
                        fns = []
                        for fc in range(NFC):
                            fns.append(lambda e, fc=fc: e.matmul(pb[fc // 4][:, (fc % 4) * 128:(fc % 4 + 1) * 128], lhsT=GU[:, 0, fc * 128:(fc + 1) * 128],
                                                                 rhs=XgT[:, dc, :], start=(dc == 0 and fc % 4 == 0), stop=(dc == NDC - 1)))
                            fns.append(lambda e, fc=fc: e.matmul(pb[2 + fc // 4][:, (fc % 4) * 128:(fc % 4 + 1) * 128], lhsT=GU[:, 1, fc * 128:(fc + 1) * 128],
                                                                 rhs=XgT[:, dc, :], start=(dc == 0 and fc % 4 == 0), stop=(dc == NDC - 1)))
                        fw.op("pe", fns, reads=[BGU, BXgT], writes=[Bpb[0], Bpb[1], Bpb[2], Bpb[3]])
                        si += 1
                        for _ in range(2):
                            if pending:
                                pending.pop(0)()
                    while pending:
                        pending.pop(0)()
                    for hh in range((NFC + 3) // 4):
                        w_ = min(4, NFC - hh * 4) * 128
                        fw.op("act", lambda e: e.activation(out=sg[:, hh * 512:hh * 512 + w_], in_=pb[hh][:, 0:w_], func=AF.Silu), reads=[Bpb[hh]], writes=[Bsg])
                        fw.op("dve", lambda e: e.tensor_tensor(out=hdT[:, hh * 4:hh * 4 + w_ // 128, :].rearrange("p c t -> p (c t)"),
                                                               in0=sg[:, hh * 512:hh * 512 + w_], in1=pb[2 + hh][:, 0:w_], op=ALU.mult),
                              reads=[Bsg, Bpb[2 + hh]], writes=[BhdT])
                    for fc in range(NFC):
                        DN = dn[di % NDL]; BDN = Bdn[di % NDL]
                        fw.dma("sp", [(DN[:], wdb_v[ex, fc])], BDN, reads=[Bwdb])
                        fw.op("pe", [(lambda e, dm=dm: e.matmul(pb[4 + dm][:], lhsT=hdT[:, fc, :], rhs=DN[:, dm * 512:(dm + 1) * 512],
                                                                start=(fc == 0), stop=(fc == NFC - 1))) for dm in range(4)],
                              reads=[BDN, BhdT], writes=[Bpb[4 + dm] for dm in range(4)])
                        di += 1
                    for dm in range(4):
                        fw.op("act", lambda e: e.copy(out=ybr[i2][:, dm * 512:(dm + 1) * 512], in_=pb[4 + dm][:]), reads=[Bpb[4 + dm]], writes=[Bybr[i2]])
                    if ex % 2 == 0 and ex + 1 < n_moe_experts:
                        continue
                    grp_ = [ex] if ex % 2 == 0 else [ex - 1, ex]

                    def mk_item(b, dm, grp_=grp_):
                        def item():
                            PC = pb[4 + cstate["ci"] % 4]; BPC = Bpb[4 + cstate["ci"] % 4]
                            cstate["ci"] += 1
                            fw.op("pe", [(lambda e, k=k, ee=ee: e.matmul(PC[:], lhsT=PwT[ee % 3][:, b, :], rhs=ybr[ee % 2][:, dm * 512:(dm + 1) * 512],
                                                                         start=(k == 0), stop=(k == len(grp_) - 1))) for k, ee in enumerate(grp_)],
                                  reads=[BPwT[ee % 3] for ee in grp_] + [Bybr[ee % 2] for ee in grp_], writes=[BPC])
                            fw.op("dve", lambda e: e.tensor_tensor(out=x1[:, b, dm * 512:(dm + 1) * 512], in0=x1[:, b, dm * 512:(dm + 1) * 512],
                                                                   in1=PC[:], op=ALU.add), reads=[BPC], writes=[Bx1[b]])
                        return item
                    pending.extend(mk_item(b, dm) for b in range(8) for dm in range(4))
                while pending:
                    pending.pop(0)()
                if debug:
                    fw.dma("sp", [(dbg_x2[:, :], x1[:].rearrange("p b f -> p (b f)"))], Bdbg, reads=Bx1)
                fw.barrier()

            with contextlib.ExitStack() as st:
                gfin = SB(st, "gfin", [128, D], F32); Bgfin = Buf("gfin")
                fw.dma("sp", [(gfin[:], g_fin[0:1, :].partition_broadcast(128))], Bgfin)
                junk3 = SB(st, "junk3", [128, D], BF16); Bj3 = Buf("junk3")
                st3 = SB(st, "st3", [128, 8, 4], F32); Bst3 = Buf("st3")
                ot = [SB(st, f"ot{i}", [128, D], F32) for i in range(2)]; Bot = [Buf(f"ot{i}") for i in range(2)]
                Bout = Buf("out")
                for b in range(8):
                    fw.op("act", lambda e: e.activation(out=junk3[:], in_=x1[:, b, :], func=AF.Square, accum_out=st3[:, b, 0:1]),
                          reads=[Bx1[b]], writes=[Bj3, Bst3])
                    fw.op("act", lambda e: e.activation(out=st3[:, b, 1:2], in_=st3[:, b, 0:1], func=AF.Ln, scale=1.0 / D, bias=EPS), reads=[Bst3], writes=[Bst3])
                    fw.op("act", lambda e: e.activation(out=st3[:, b, 2:3], in_=st3[:, b, 1:2], func=AF.Exp, scale=-0.5), reads=[Bst3], writes=[Bst3])
                    O = ot[b % 2]; BO = Bot[b % 2]
                    fw.op("dve", lambda e: e.scalar_tensor_tensor(out=O[:], in0=x1[:, b, :], scalar=st3[:, b, 2:3], in1=gfin[:],
                                                                  op0=ALU.mult, op1=ALU.mult), reads=[Bx1[b], Bst3, Bgfin], writes=[BO])
                    fw.dma("sp", [(out_d[b * 128:(b + 1) * 128, :], O[:])], Bout, reads=[BO])
                fw.barrier()
    return nc


NT_FULL = 16
NCORES = 8


def own_rows(c, NT):
    a = np.arange(c * 512, (c + 1) * 512)
    b = np.arange((NT - 1 - c) * 512, (NT - c) * 512)
    return np.concatenate([a, b])


def make_in_maps(inputs, NT, ncores):
    f = lambda a: np.ascontiguousarray(np.asarray(a, dtype=np.float32))
    x = f(inputs["x"]).reshape(-1, D)
    DE = inputs["w_gate"].shape[-1]
    shared = {
        "x_all": x,
        "g_mix": f(inputs["g_mix"]).reshape(1, D),
        "w_in": f(inputs["w_in"]).reshape(D, 9216),
        "ln_v_g": f(inputs["ln_v_g"]).reshape(1, 1024),
        "ln_v_b": f(inputs["ln_v_b"]).reshape(1, 1024),
        "w_spatial": f(inputs["w_spatial"]).reshape(8, 128, 128),
        "b_spatial": f(inputs["b_spatial"]).reshape(1, 1024),
        "w_branch_a": f(inputs["w_branch_a"]).reshape(1024, D),
        "w_branch_b": f(inputs["w_branch_b"]).reshape(1024, D),
        "w_out": f(inputs["w_out"]).reshape(D, D),
        "g_ffn": f(inputs["g_ffn"]).reshape(1, D),
        "w_router_group": f(inputs["w_router_group"]).reshape(D, 4),
        "b_router_group": f(inputs["b_router_group"]).reshape(1, 4),
        "w_router_expert": f(inputs["w_router_expert"]).reshape(D, 32),
        "b_router_expert": f(inputs["b_router_expert"]).reshape(1, 32),
        "w_gate": f(inputs["w_gate"]).reshape(32 * D, DE),
        "w_up": f(inputs["w_up"]).reshape(32 * D, DE),
        "w_down": f(inputs["w_down"]).reshape(32 * DE, D),
        "g_final": f(inputs["g_final"]).reshape(1, D),
    }
    maps = []
    for c in range(ncores):
        rows = own_rows(c, NT)
        m = dict(shared)
        m["x_own"] = np.ascontiguousarray(x[rows])
        m["qpos"] = rows.astype(np.float32).reshape(1, 1024)
        maps.append(m)
    return maps


def kernel(**inputs):
    NT = NT_FULL
    DE = inputs["w_gate"].shape[-1]
    nc = build_nc(NT, DE)
    in_maps = make_in_maps(inputs, NT, NCORES)
    res = run_bass_kernel_spmd(nc, in_maps, core_ids=list(range(NCORES)))
    out = np.zeros((NT * 512, D), np.float32)
    for c in range(NCORES):
        out[own_rows(c, NT)] = res.results[c]["out"]
    return out.reshape(1, NT * 512, D)
```

```python
import contextlib
import numpy as np
import concourse.bass as bass
import concourse.mybir as mybir
from concourse.bass_utils import run_bass_kernel_spmd

F32 = mybir.dt.float32
BF16 = mybir.dt.bfloat16
AF = mybir.ActivationFunctionType
ALU = mybir.AluOpType
AX = mybir.AxisListType

D = 2048
NDC = 16
EPS = 1e-6
NEG = -30000.0


class Buf:
    __slots__ = ("name", "w", "r", "dsem", "dcnt")

    def __init__(self, name):
        self.name = name
        self.w = None
        self.r = []
        self.dsem = None
        self.dcnt = 0


class FW:
    def __init__(self, nc, stack):
        self.nc = nc
        self.stack = stack
        self.eng = {"pe": nc.tensor, "act": nc.scalar, "dve": nc.vector, "pool": nc.gpsimd, "sp": nc.sync}
        self.sems = {}
        self.cnt = {}
        self.cur = {}
        self.waited = {k: {} for k in self.eng}
        self.nsem = 0
        self.dbufs = []
        self.nins = 0
        for k in ("pe", "act", "dve", "pool"):
            self._new_sem(k)

    def _mk(self, name):
        h = self.stack.enter_context(self.nc.semaphore(name))
        self.nsem += 1
        return h

    def _new_sem(self, k):
        name = f"s_{k}_{self.nsem}"
        self.sems[name] = self._mk(name)
        self.cnt[name] = 0
        self.cur[k] = name

    def _wait(self, k, tok):
        if tok is None:
            return
        s, v = tok
        if self.waited[k].get(s, 0) >= v:
            return
        self.eng[k].wait_ge(self.sems[s], v)
        self.waited[k][s] = v

    def _deps(self, k, reads, writes, extra):
        for b in reads:
            self._wait(k, b.w)
        for b in writes:
            self._wait(k, b.w)
            for t in b.r:
                self._wait(k, t)
        for t in extra:
            self._wait(k, t)

    @staticmethod
    def _commit(tok, reads, writes):
        for b in reads:
            b.r.append(tok)
        for b in writes:
            b.w = tok
            b.r = []

    def op(self, k, fns, reads=(), writes=(), extra=()):
        if callable(fns):
            fns = [fns]
        self._deps(k, reads, writes, extra)
        e = self.eng[k]
        ins = None
        for f in fns:
            ins = f(e)
            self.nins += 1
        s = self.cur[k]
        ins.then_inc(self.sems[s], 1)
        self.cnt[s] += 1
        tok = (s, self.cnt[s])
        self._commit(tok, reads, writes)
        return tok

    def dma(self, q, pairs, dst, reads=(), extra=()):
        self._deps(q, reads, [dst], extra)
        e = self.eng[q]
        if dst.dsem is None:
            dst.dsem = f"d_{dst.name}_{self.nsem}"
            self.sems[dst.dsem] = self._mk(dst.dsem)
            self.dbufs.append(dst)
        for (o, i) in pairs:
            e.dma_start(out=o, in_=i).then_inc(self.sems[dst.dsem], 16)
            dst.dcnt += 16
            self.nins += 1
        tok = (dst.dsem, dst.dcnt)
        self._commit(tok, reads, [dst])
        return tok

    def barrier(self, engines=("pe", "act", "dve", "pool", "sp")):
        toks = [(s, self.cnt[s]) for s in self.cnt if self.cnt[s] > 0]
        toks += [(b.dsem, b.dcnt) for b in self.dbufs if b.dcnt > 0]
        for k in engines:
            for t in toks:
                self._wait(k, t)


def build_nc(NT, DE, n_moe_experts=32, debug=False):
    S = NT * 512
    NB = S // 128
    NFC = DE // 128
    SCALE = 128 ** -0.5
    nc = bass.Bass("TRN2", target_bir_lowering=False)

    def din(name, shape):
        return nc.dram_tensor(name, shape, F32, kind="ExternalInput").ap()

    x_all = din("x_all", [S, D])
    x_own = din("x_own", [1024, D])
    qpos_d = din("qpos", [1, 1024])
    g_mix = din("g_mix", [1, D])
    w_in = din("w_in", [D, 9216])
    ln_g = din("ln_v_g", [1, 1024])
    ln_b = din("ln_v_b", [1, 1024])
    w_sp = din("w_spatial", [8, 128, 128])
    b_sp = din("b_spatial", [1, 1024])
    w_ba = din("w_branch_a", [1024, D])
    w_bb = din("w_branch_b", [1024, D])
    w_out = din("w_out", [D, D])
    g_ffn = din("g_ffn", [1, D])
    w_rg = din("w_router_group", [D, 4])
    b_rg = din("b_router_group", [1, 4])
    w_re = din("w_router_expert", [D, 32])
    b_re = din("b_router_expert", [1, 32])
    w_gate = din("w_gate", [32 * D, DE])
    w_up = din("w_up", [32 * D, DE])
    w_down = din("w_down", [32 * DE, D])
    g_fin = din("g_final", [1, D])
    out_d = nc.dram_tensor("out", [1024, D], F32, kind="ExternalOutput").ap()
    hTs = nc.dram_tensor("hTs", [NT * 128, NDC * 512], BF16).ap()
    hTo = nc.dram_tensor("hTo", [2 * 128, NDC * 512], BF16).ap()
    mTs = nc.dram_tensor("mTs", [128, NDC * 1024], BF16).ap()
    wdb = nc.dram_tensor("wdb", [32 * DE, D], BF16).ap()
    NUP_P2, NUP_AT = 6, 8
    NUP = NUP_P2 + NUP_AT
    wub = nc.dram_tensor("wub", [NUP * D, DE], BF16).ap()
    Bcv = [Buf(f"cv{i}") for i in range(4)]
    cvn = {"k": 0}

    def conv_up(fw_, r0, nr):
        b_ = Bcv[cvn["k"] % 4]; cvn["k"] += 1
        fw_.dma("pool", [(wub[r0:r0 + nr, :], w_up[r0:r0 + nr, :])], b_)
    if debug:
        dbg_ya = nc.dram_tensor("dbg_ya", [128, 8 * 1024], BF16, kind="ExternalOutput").ap()
        dbg_yb = nc.dram_tensor("dbg_yb", [128, 8 * 1024], BF16, kind="ExternalOutput").ap()
        dbg_x1 = nc.dram_tensor("dbg_x1", [128, 8 * D], F32, kind="ExternalOutput").ap()
        dbg_lg = nc.dram_tensor("dbg_lg", [128, 8 * 36], F32, kind="ExternalOutput").ap()
        dbg_g = nc.dram_tensor("dbg_g", [128, 8 * 32], F32, kind="ExternalOutput").ap()
        dbg_pos = nc.dram_tensor("dbg_pos", [128, 8 * 32], F32, kind="ExternalOutput").ap()
        dbg_x2 = nc.dram_tensor("dbg_x2", [128, 8 * D], F32, kind="ExternalOutput").ap()
    Bdbg = Buf("dbg")

    with contextlib.ExitStack() as top:
        fw = FW(nc, top)

        def SB(st, name, shape, dt):
            return st.enter_context(nc.sbuf_tensor("sb_" + name, shape, dt))

        def PS(st, name, shape, dt):
            return st.enter_context(nc.psum_tensor("ps_" + name, shape, dt))

        ident = SB(top, "ident", [128, 128], BF16); Bident = Buf("ident")
        identf = SB(top, "identf", [128, 128], F32); Bidentf = Buf("identf")
        negU = SB(top, "negU", [128, 128], BF16); BnegU = Buf("negU")
        negOnes = SB(top, "negOnes", [128, 128], BF16); BnegOnes = Buf("negOnes")
        ones = SB(top, "ones", [128, 128], BF16); Bones = Buf("ones")
        onesf = SB(top, "onesf", [128, 128], F32); Bonesf = Buf("onesf")
        stri = SB(top, "stri", [128, 128], BF16); Bstri = Buf("stri")
        kpos = SB(top, "kpos", [128, NB], F32); Bkpos = Buf("kpos")
        iota_s = SB(top, "iota_s", [128, 128], F32); Biota = Buf("iota")
        stA = contextlib.ExitStack()
        qpos = SB(stA, "qpos", [128, 1024], F32); Bqpos = Buf("qpos")
        y_aT = SB(stA, "y_aT", [128, 8, 1024], BF16); By_a = Buf("y_aT")
        y_bT = SB(stA, "y_bT", [128, 8, 1024], BF16); By_b = Buf("y_bT")
        BmTs = Buf("mTs")

        fw.op("pool", lambda e: e.memset(ones[:], 1.0), writes=[Bones])
        fw.op("pool", lambda e: e.memset(onesf[:], 1.0), writes=[Bonesf])
        fw.op("pool", lambda e: e.memset(negOnes[:], -1.0), writes=[BnegOnes])
        fw.op("pool", lambda e: e.affine_select(out=ident[:], in_=ones[:], pattern=[[-1, 128]],
                                                compare_op=ALU.is_equal, fill=0.0, base=0, channel_multiplier=1),
              reads=[Bones], writes=[Bident])
        fw.op("pool", lambda e: e.affine_select(out=identf[:], in_=onesf[:], pattern=[[-1, 128]],
                                                compare_op=ALU.is_equal, fill=0.0, base=0, channel_multiplier=1),
              reads=[Bonesf], writes=[Bidentf])
        fw.op("pool", lambda e: e.affine_select(out=negU[:], in_=negOnes[:], pattern=[[-1, 128]],
                                                compare_op=ALU.is_ge, fill=0.0, base=0, channel_multiplier=1),
              reads=[BnegOnes], writes=[BnegU])
        fw.op("pool", lambda e: e.affine_select(out=stri[:], in_=ones[:], pattern=[[1, 128]],
                                                compare_op=ALU.is_gt, fill=0.0, base=0, channel_multiplier=-1),
              reads=[Bones], writes=[Bstri])
        fw.op("pool", lambda e: e.iota(kpos[:], pattern=[[128, NB]], base=0, channel_multiplier=1,
                                       allow_small_or_imprecise_dtypes=True), writes=[Bkpos])
        fw.op("pool", lambda e: e.iota(iota_s[:], pattern=[[1, 128]], base=0, channel_multiplier=0,
                                       allow_small_or_imprecise_dtypes=True), writes=[Biota])
        fw.dma("sp", [(qpos[:], qpos_d[0:1, :].partition_broadcast(128))], Bqpos)

        BhTs = Buf("hTs")
        BhTo = Buf("hTo")
        with contextlib.ExitStack() as st:
            gm = SB(st, "gm", [128, D], F32); Bgm = Buf("gm")
            fw.dma("sp", [(gm[:], g_mix[0:1, :].partition_broadcast(128))], Bgm)
            xt = [SB(st, f"xt{i}", [128, D], F32) for i in range(4)]
            Bxt = [Buf(f"xt{i}") for i in range(4)]
            junk = SB(st, "junk", [128, D], BF16); Bjunk = Buf("junk")
            stt = [SB(st, f"stt{i}", [128, 4], F32) for i in range(4)]
            Bstt = [Buf(f"stt{i}") for i in range(4)]
            hb = [SB(st, f"hb{i}", [128, D], BF16) for i in range(3)]
            Bhb = [Buf(f"hb{i}") for i in range(3)]
            hT4 = [SB(st, f"hT4{i}", [128, NDC, 512], BF16) for i in range(2)]
            BhT4 = [Buf(f"hT4{i}") for i in range(2)]
            pt = [PS(st, f"pt{i}", [128, D], BF16) for i in range(2)]
            Bpt = [Buf(f"pt{i}") for i in range(2)]
            blocks = [(tile, blk) for tile in range(NT + 2) for blk in range(4)]

            def p1_s1(bi):
                tile, blk = blocks[bi]
                own = tile >= NT
                src = x_own if own else x_all
                t_loc = tile - NT if own else tile
                r0 = (t_loc * 4 + blk) * 128
                X = xt[bi % 4]; BX = Bxt[bi % 4]
                sT = stt[bi % 4]; BsT = Bstt[bi % 4]
                H = hb[bi % 3]; BH = Bhb[bi % 3]
                fw.dma("sp", [(X[:], src[r0:r0 + 128, :])], BX)
                fw.op("act", lambda e: e.activation(out=junk[:], in_=X[:], func=AF.Square, accum_out=sT[:, 0:1]),
                      reads=[BX], writes=[Bjunk, BsT])
                fw.op("act", lambda e: e.activation(out=sT[:, 1:2], in_=sT[:, 0:1], func=AF.Ln, scale=1.0 / D, bias=EPS),
                      reads=[BsT], writes=[BsT])
                fw.op("act", lambda e: e.activation(out=sT[:, 2:3], in_=sT[:, 1:2], func=AF.Exp, scale=-0.5),
                      reads=[BsT], writes=[BsT])
                fw.op("dve", lambda e: e.scalar_tensor_tensor(out=H[:], in0=X[:], scalar=sT[:, 2:3], in1=gm[:],
                                                              op0=ALU.mult, op1=ALU.mult),
                      reads=[BX, BsT, Bgm], writes=[BH])

            def p1_s2(bi):
                tile, blk = blocks[bi]
                own = tile >= NT
                t_loc = tile - NT if own else tile
                h4 = hT4[tile % 2]; Bh4 = BhT4[tile % 2]
                H = hb[bi % 3]; BH = Bhb[bi % 3]
                P = pt[bi % 2]; BP = Bpt[bi % 2]
                fw.op("pe", [(lambda e, dc=dc: e.transpose(out=P[:, dc * 128:(dc + 1) * 128],
                                                            in_=H[:, dc * 128:(dc + 1) * 128], identity=ident[:]))
                             for dc in range(NDC)], reads=[BH, Bident], writes=[BP])
                if bi % 2 == 0:
                    fw.op("act", lambda e: e.copy(out=h4[:, :, blk * 128:(blk + 1) * 128],
                                                  in_=P[:].rearrange("p (c t) -> p c t", c=NDC)),
                          reads=[BP], writes=[Bh4])
                else:
                    fw.op("dve", lambda e: e.tensor_copy(out=h4[:, :, blk * 128:(blk + 1) * 128],
                                                         in_=P[:].rearrange("p (c t) -> p c t", c=NDC)),
                          reads=[BP], writes=[Bh4])
                if blk == 3:
                    dstd = hTo if own else hTs
                    fw.dma("pool", [(dstd[t_loc * 128:(t_loc + 1) * 128, :], h4[:].rearrange("p c t -> p (c t)"))],
                           BhTo if own else BhTs, reads=[Bh4])

            p1_s1(0)
            for bi in range(len(blocks)):
                if bi + 1 < len(blocks):
                    p1_s1(bi + 1)
                p1_s2(bi)
            fw.barrier()

        with contextlib.ExitStack() as st:
            Wu = SB(st, "Wu", [128, NDC, 1024], BF16); BWu = Buf("Wu")
            Wv = SB(st, "Wv", [128, NDC, 1024], BF16); BWv = Buf("Wv")
            w_in_v = w_in.rearrange("(c p) f -> p c f", p=128)
            for half in range(2):
                fw.dma("pool", [(Wu[:, half * 8:(half + 1) * 8, :], w_in_v[:, half * 8:(half + 1) * 8, 0:1024])], BWu)
                fw.dma("pool", [(Wv[:, half * 8:(half + 1) * 8, :], w_in_v[:, half * 8:(half + 1) * 8, 1024:2048])], BWv)
            lg_t = SB(st, "lg_t", [128, 1024], F32); Blg = Buf("lng")
            lb_t = SB(st, "lb_t", [128, 1024], F32); Blb = Buf("lnb")
            fw.dma("sp", [(lg_t[:], ln_g[0:1, :].partition_broadcast(128))], Blg)
            fw.dma("sp", [(lb_t[:], ln_b[0:1, :].partition_broadcast(128))], Blb)
            bsr_f = SB(st, "bsr_f", [1, 1024], F32); Bbsrf = Buf("bsrf")
            bsr = SB(st, "bsr", [1, 1024], BF16); Bbsr = Buf("bsr")
            fw.dma("sp", [(bsr_f[:], b_sp[0:1, :])], Bbsrf)
            fw.op("dve", lambda e: e.tensor_copy(out=bsr[:], in_=bsr_f[:]), reads=[Bbsrf], writes=[Bbsr])
            wsl = SB(st, "wsl", [128, 8, 128], F32); Bwsl = Buf("wsl")
            fw.dma("sp", [(wsl[:], w_sp.rearrange("g t s -> t g s"))], Bwsl)
            wsT = SB(st, "wsT", [128, 8, 128], BF16); BwsT = Buf("wsT")
            wsTf = SB(st, "wsTf", [128, 8, 128], F32); BwsTf = Buf("wsTf")
            pw = PS(st, "pw", [128, 1024], F32); Bpw = Buf("pw")
            fw.op("pe", [(lambda e, g=g: e.transpose(out=pw[:, g * 128:(g + 1) * 128], in_=wsl[:, g, :], identity=identf[:]))
                         for g in range(8)], reads=[Bwsl, Bidentf], writes=[Bpw])
            fw.op("dve", lambda e: e.tensor_copy(out=wsTf[:].rearrange("p g t -> p (g t)"), in_=pw[:]), reads=[Bpw], writes=[BwsTf])
            for g in range(8):
                fw.op("pool", lambda e: e.affine_select(out=wsT[:, g, :], in_=wsTf[:, g, :], pattern=[[1, 128]],
                                                        compare_op=ALU.is_ge, fill=0.0, base=0, channel_multiplier=-1),
                      reads=[BwsTf], writes=[BwsT])
            hto = SB(st, "hto", [128, NDC, 512], BF16); Bhto = Buf("hto")
            guT = SB(st, "guT", [128, 8, 512], BF16); BguT = Buf("guT")
            pu = [PS(st, f"pu{i}", [128, 512], F32) for i in range(2)]
            Bpu = [Buf(f"pu{i}") for i in range(2)]
            pv = PS(st, "pv", [128, 1024], F32); Bpv = Buf("pv")
            pm = PS(st, "pm", [128, 1024], F32); Bpm = Buf("pm")
            t1 = SB(st, "t1", [128, 1024], F32); Bt1 = Buf("t1")
            t2 = SB(st, "t2", [128, 1024], F32); Bt2 = Buf("t2")
            gv = SB(st, "gv", [128, 1024], F32); Bgv = Buf("gv")
            vn = SB(st, "vn", [128, 1024], BF16); Bvn = Buf("vn")
            u1 = SB(st, "u1", [128, 512], F32); Bu1 = Buf("u1")
            u2 = SB(st, "u2", [128, 512], F32); Bu2 = Buf("u2")
            sv = SB(st, "sv", [128, 8], F32); Bsv = Buf("sv")
            GC = 1.5957691216057308

            def gelu_ops(dst_ap, src_ap, a_ap, b_ap, reads, Ba, Bb, Bdst):
                fw.op("act", lambda e: e.copy(out=b_ap, in_=src_ap), reads=reads, writes=[Bb])
                fw.op("dve", lambda e: e.tensor_tensor(out=a_ap, in0=b_ap, in1=b_ap, op=ALU.mult), reads=[Bb], writes=[Ba])
                fw.op("dve", lambda e: e.tensor_scalar(out=a_ap, in0=a_ap, scalar1=0.044715, scalar2=1.0, op0=ALU.mult, op1=ALU.add),
                      reads=[Ba], writes=[Ba])
                fw.op("dve", lambda e: e.tensor_tensor(out=a_ap, in0=a_ap, in1=b_ap, op=ALU.mult), reads=[Ba, Bb], writes=[Ba])
                fw.op("act", lambda e: e.activation(out=a_ap, in_=a_ap, func=AF.Sigmoid, scale=GC), reads=[Ba], writes=[Ba])
                fw.op("dve", lambda e: e.tensor_tensor(out=dst_ap, in0=a_ap, in1=b_ap, op=ALU.mult), reads=[Ba, Bb], writes=[Bdst])

            u1r = [u1, SB(st, "u1b", [128, 512], F32)]; Bu1r = [Bu1, Buf("u1b")]
            u2r = [u2, SB(st, "u2b", [128, 512], F32)]; Bu2r = [Bu2, Buf("u2b")]
            t1r = [t1, SB(st, "t1b", [128, 1024], F32)]; Bt1r = [Bt1, Buf("t1b")]
            t2r = [t2, SB(st, "t2b", [128, 1024], F32)]; Bt2r = [Bt2, Buf("t2b")]
            pvr = [pv, pw]; Bpvr = [Bpv, Bpw]

            def gelu_s1(a_ap, b_ap, src_ap, Ba, Bb, Bsrc):
                fw.op("act", lambda e: e.copy(out=b_ap, in_=src_ap), reads=[Bsrc], writes=[Bb])
                fw.op("dve", lambda e: e.tensor_tensor(out=a_ap, in0=b_ap, in1=b_ap, op=ALU.mult), reads=[Bb], writes=[Ba])
                fw.op("dve", lambda e: e.tensor_scalar(out=a_ap, in0=a_ap, scalar1=0.044715, scalar2=1.0, op0=ALU.mult, op1=ALU.add),
                      reads=[Ba], writes=[Ba])
                fw.op("dve", lambda e: e.tensor_tensor(out=a_ap, in0=a_ap, in1=b_ap, op=ALU.mult), reads=[Ba, Bb], writes=[Ba])

            def gelu_s2(dst_ap, a_ap, b_ap, Ba, Bb, Bdst):
                fw.op("act", lambda e: e.activation(out=a_ap, in_=a_ap, func=AF.Sigmoid, scale=GC), reads=[Ba], writes=[Ba])
                fw.op("dve", lambda e: e.tensor_tensor(out=dst_ap, in0=a_ap, in1=b_ap, op=ALU.mult), reads=[Ba, Bb], writes=[Bdst])

            for r0 in range(0, NUP_P2 * D, 512):
                conv_up(fw, r0, 512)
            ucnt = {"u": 0, "v": 0}
            for tl in range(2):
                fw.dma("sp", [(hto[:].rearrange("p c t -> p (c t)"), hTo[tl * 128:(tl + 1) * 128, :])], Bhto, reads=[BhTo])

                def u_s1(cc):
                    k = (ucnt["u"] + cc) % 2
                    P = pu[k]; BP = Bpu[k]
                    fw.op("pe", [(lambda e, dc=dc: e.matmul(P[:], lhsT=Wu[:, dc, cc * 128:(cc + 1) * 128], rhs=hto[:, dc, :],
                                                            start=(dc == 0), stop=(dc == NDC - 1))) for dc in range(NDC)],
                          reads=[BWu, Bhto], writes=[BP])
                    gelu_s1(u1r[k][:], u2r[k][:], P[:], Bu1r[k], Bu2r[k], BP)

                def u_s2(cc):
                    k = (ucnt["u"] + cc) % 2
                    gelu_s2(guT[:, cc, :], u1r[k][:], u2r[k][:], Bu1r[k], Bu2r[k], BguT)

                u_s1(0)
                for cc in range(8):
                    if cc + 1 < 8:
                        u_s1(cc + 1)
                    u_s2(cc)
                ucnt["u"] += 8

                def v_s1(blk):
                    k = (ucnt["v"] + blk) % 2
                    PV = pvr[k]; BPV = Bpvr[k]
                    fw.op("pe", [(lambda e, dc=dc, hf=hf: e.matmul(PV[:, hf * 512:(hf + 1) * 512], lhsT=hto[:, dc, blk * 128:(blk + 1) * 128],
                                                                  rhs=Wv[:, dc, hf * 512:(hf + 1) * 512],
                                                                  start=(dc == 0), stop=(dc == NDC - 1)))
                                 for hf in range(2) for dc in range(NDC)], reads=[BWv, Bhto], writes=[BPV])
                    gelu_s1(t1r[k][:], t2r[k][:], PV[:], Bt1r[k], Bt2r[k], BPV)

                def v_s2(blk):
                    k = (ucnt["v"] + blk) % 2
                    tb = tl * 4 + blk
                    T1 = t1r[k]; T2 = t2r[k]; BT1 = Bt1r[k]; BT2 = Bt2r[k]
                    gelu_s2(gv[:], T1[:], T2[:], BT1, BT2, Bgv)
                    fw.op("act", lambda e: e.activation(out=T2[:], in_=gv[:], func=AF.Copy, accum_out=sv[:, 0:1]), reads=[Bgv], writes=[BT2, Bsv])
                    fw.op("act", lambda e: e.activation(out=T2[:], in_=gv[:], func=AF.Square, accum_out=sv[:, 1:2]), reads=[Bgv], writes=[BT2, Bsv])
                    fw.op("dve", lambda e: e.tensor_scalar(out=sv[:, 2:3], in0=sv[:, 0:1], scalar1=1.0 / 1024, scalar2=None, op0=ALU.mult), reads=[Bsv], writes=[Bsv])
                    fw.op("dve", lambda e: e.tensor_tensor(out=sv[:, 3:4], in0=sv[:, 2:3], in1=sv[:, 2:3], op=ALU.mult), reads=[Bsv], writes=[Bsv])
                    fw.op("dve", lambda e: e.scalar_tensor_tensor(out=sv[:, 4:5], in0=sv[:, 1:2], scalar=1.0 / 1024, in1=sv[:, 3:4],
                                                                  op0=ALU.mult, op1=ALU.subtract), reads=[Bsv], writes=[Bsv])
                    fw.op("act", lambda e: e.activation(out=sv[:, 5:6], in_=sv[:, 4:5], func=AF.Ln, bias=EPS), reads=[Bsv], writes=[Bsv])
                    fw.op("act", lambda e: e.activation(out=sv[:, 6:7], in_=sv[:, 5:6], func=AF.Exp, scale=-0.5), reads=[Bsv], writes=[Bsv])
                    fw.op("dve", lambda e: e.tensor_scalar(out=T1[:], in0=gv[:], scalar1=sv[:, 2:3], scalar2=sv[:, 6:7],
                                                           op0=ALU.subtract, op1=ALU.mult), reads=[Bgv, Bsv], writes=[BT1])
                    fw.op("dve", lambda e: e.tensor_tensor(out=T1[:], in0=T1[:], in1=lg_t[:], op=ALU.mult), reads=[Blg], writes=[BT1])
                    fw.op("dve", lambda e: e.tensor_tensor(out=vn[:], in0=T1[:], in1=lb_t[:], op=ALU.add), reads=[BT1, Blb], writes=[Bvn])
                    fns = []
                    for g in range(8):
                        fns.append(lambda e, g=g: e.matmul(pm[:, g * 128:(g + 1) * 128], lhsT=vn[:, g * 128:(g + 1) * 128],
                                                           rhs=wsT[:, g, :], start=True, stop=False))
                        fns.append(lambda e, g=g: e.matmul(pm[:, g * 128:(g + 1) * 128], lhsT=ones[0:1, :],
                                                           rhs=bsr[0:1, g * 128:(g + 1) * 128], start=False, stop=True))
                    fw.op("pe", fns, reads=[Bvn, BwsT, Bones, Bbsr], writes=[Bpm])
                    fw.op("dve", lambda e: e.tensor_tensor(out=y_aT[:, :, tb * 128:(tb + 1) * 128],
                                                           in0=pm[:].rearrange("p (g t) -> p g t", g=8),
                                                           in1=guT[:, :, blk * 128:(blk + 1) * 128], op=ALU.mult),
                          reads=[Bpm, BguT], writes=[By_a])

                v_s1(0)
                for blk in range(4):
                    if blk + 1 < 4:
                        v_s1(blk + 1)
                    v_s2(blk)
                ucnt["v"] += 4
            fw.barrier()

        with contextlib.ExitStack() as st:
            KT = [SB(st, f"KT{i}", [128, S], BF16) for i in range(2)]; BKT = [Buf(f"KT{i}") for i in range(2)]
            V = [SB(st, f"V{i}", [128, NB, 128], BF16) for i in range(2)]; BV = [Buf(f"V{i}") for i in range(2)]
            QT = [SB(st, f"QT{i}", [128, 1024], BF16) for i in range(2)]; BQT = [Buf(f"QT{i}") for i in range(2)]
            Wq = [SB(st, f"Wq{i}", [128, NDC, 128], BF16) for i in range(2)]; BWq = [Buf(f"Wq{i}") for i in range(2)]
            Wk = [SB(st, f"Wk{i}", [128, NDC, 128], BF16) for i in range(2)]; BWk = [Buf(f"Wk{i}") for i in range(2)]
            Wvh = [SB(st, f"Wvh{i}", [128, NDC, 128], BF16) for i in range(2)]; BWvh = [Buf(f"Wvh{i}") for i in range(2)]
            htl = [SB(st, f"htl{i}", [128, NDC, 512], BF16) for i in range(2)]
            Bhtl = [Buf(f"htl{i}") for i in range(2)]
            Et = [SB(st, f"Et{i}", [128, 512], F32) for i in range(4)]; BEt = [Buf(f"Et{i}") for i in range(4)]
            Xt = [SB(st, f"Xt{i}", [128, 512], F32) for i in range(2)]; BXt = [Buf(f"Xt{i}") for i in range(2)]
            Lt = [SB(st, f"Lt{i}", [128, 512], BF16) for i in range(4)]; BLt = [Buf(f"Lt{i}") for i in range(4)]
            Rt = [SB(st, f"Rt{i}", [128, 512], BF16) for i in range(5)]; BRt = [Buf(f"Rt{i}") for i in range(5)]
            Wt = [SB(st, f"Wt{i}", [128, 512], BF16) for i in range(3)]; BWt = [Buf(f"Wt{i}") for i in range(3)]
            Mt = [SB(st, f"Mt{i}", [128, 512], BF16) for i in range(4)]; BMt = [Buf(f"Mt{i}") for i in range(4)]
            pz = [PS(st, f"pz{i}", [128, 512], F32) for i in range(2)]; Bpz = [Buf(f"pz{i}") for i in range(2)]
            pS = [PS(st, f"pS{i}", [128, 512], F32) for i in range(2)]; BpS = [Buf(f"pS{i}") for i in range(2)]
            pacc = PS(st, "pacc", [128, 512], F32); Bpacc = Buf("pacc")
            pk = [PS(st, f"pk{i}", [128, 512], F32) for i in range(2)]; Bpk = [Buf(f"pk{i}") for i in range(2)]
            ppv = PS(st, "ppv", [128, 512], F32); Bppv = Buf("ppv")
            w_in_v = w_in.rearrange("(c p) f -> p c f", p=128)
            cnt = {"ht": 0, "pk": 0}

            def load_w(hd):
                h2_ = hd % 2
                for (Wt_, BWt_, off) in ((Wq[h2_], BWq[h2_], 2048), (Wk[h2_], BWk[h2_], 3072), (Wvh[h2_], BWvh[h2_], 4096)):
                    fw.dma("pool", [(Wt_[:, q * 4:(q + 1) * 4, :], w_in_v[:, q * 4:(q + 1) * 4, off + hd * 128:off + (hd + 1) * 128])
                                    for q in range(4)], BWt_)

            def proj_tile_pieces(hd, tile):
                h2_ = hd % 2
                stt_ = {}

                def p0():
                    HT = htl[cnt["ht"] % 2]; BHT = Bhtl[cnt["ht"] % 2]; cnt["ht"] += 1
                    P = pk[cnt["pk"] % 2]; BP = Bpk[cnt["pk"] % 2]; cnt["pk"] += 1
                    stt_.update(HT=HT, BHT=BHT, P=P, BP=BP)
                    fw.dma("sp", [(HT[:].rearrange("p c t -> p (c t)"), hTs[tile * 128:(tile + 1) * 128, :])], BHT, reads=[BhTs])
                    fw.op("pe", [(lambda e, dc=dc: e.matmul(P[:], lhsT=Wk[h2_][:, dc, :], rhs=HT[:, dc, :],
                                                            start=(dc == 0), stop=False)) for dc in range(NDC // 2)],
                          reads=[BWk[h2_], BHT], writes=[BP])

                def p1():
                    HT, BHT, P, BP = stt_["HT"], stt_["BHT"], stt_["P"], stt_["BP"]
                    fw.op("pe", [(lambda e, dc=dc: e.matmul(P[:], lhsT=Wk[h2_][:, dc, :], rhs=HT[:, dc, :],
                                                            start=False, stop=(dc == NDC - 1))) for dc in range(NDC // 2, NDC)],
                          reads=[BWk[h2_], BHT], writes=[BP])
                    fw.op("act", lambda e: e.copy(out=KT[h2_][:, tile * 512:(tile + 1) * 512], in_=P[:]), reads=[BP], writes=[BKT[h2_]])

                def pv_(b):
                    def f():
                        HT, BHT = stt_["HT"], stt_["BHT"]
                        fw.op("pe", [(lambda e, dc=dc: e.matmul(ppv[:, b * 128:(b + 1) * 128], lhsT=HT[:, dc, b * 128:(b + 1) * 128],
                                                                rhs=Wvh[h2_][:, dc, :], start=(dc == 0), stop=(dc == NDC - 1)))
                                     for dc in range(NDC)], reads=[BWvh[h2_], BHT], writes=[Bppv])
                        if b == 3:
                            fw.op("dve", lambda e: e.tensor_copy(out=V[h2_][:, tile * 4:(tile + 1) * 4, :].rearrange("p b d -> p (b d)"), in_=ppv[:]),
                                  reads=[Bppv], writes=[BV[h2_]])
                    return f
                return [p0, p1, pv_(0), pv_(1), pv_(2), pv_(3)]

            def proj_q_pieces(hd, tl):
                h2_ = hd % 2
                stt_ = {}

                def p0():
                    HT = htl[cnt["ht"] % 2]; BHT = Bhtl[cnt["ht"] % 2]; cnt["ht"] += 1
                    P = pk[cnt["pk"] % 2]; BP = Bpk[cnt["pk"] % 2]; cnt["pk"] += 1
                    stt_.update(HT=HT, BHT=BHT, P=P, BP=BP)
                    fw.dma("sp", [(HT[:].rearrange("p c t -> p (c t)"), hTo[tl * 128:(tl + 1) * 128, :])], BHT, reads=[BhTo])
                    fw.op("pe", [(lambda e, dc=dc: e.matmul(P[:], lhsT=Wq[h2_][:, dc, :], rhs=HT[:, dc, :],
                                                            start=(dc == 0), stop=False)) for dc in range(NDC // 2)],
                          reads=[BWq[h2_], BHT], writes=[BP])

                def p1():
                    HT, BHT, P, BP = stt_["HT"], stt_["BHT"], stt_["P"], stt_["BP"]
                    fw.op("pe", [(lambda e, dc=dc: e.matmul(P[:], lhsT=Wq[h2_][:, dc, :], rhs=HT[:, dc, :],
                                                            start=False, stop=(dc == NDC - 1))) for dc in range(NDC // 2, NDC)],
                          reads=[BWq[h2_], BHT], writes=[BP])
                    fw.op("act", lambda e: e.mul(out=QT[h2_][:, tl * 512:(tl + 1) * 512], in_=P[:], mul=SCALE), reads=[BP], writes=[BQT[h2_]])
                return [p0, p1]

            def proj_jobs(hd):
                jobs = []
                for t in range(NT):
                    jobs += proj_tile_pieces(hd, t)
                for t in range(2):
                    jobs += proj_q_pieces(hd, t)
                return jobs

            Bwdb = Buf("wdb")
            CROWS = 256
            conv_chunks = [("dn", r0, min(CROWS, 32 * DE - r0)) for r0 in range(0, 32 * DE, CROWS)]
            up_chunks = [("up", r0, 512) for r0 in range(NUP_P2 * D, NUP * D, 512)]
            merged_, ui_ = [], 0
            for i_, c_ in enumerate(conv_chunks):
                merged_.append(c_)
                while ui_ < len(up_chunks) and (ui_ + 1) * len(conv_chunks) <= (i_ + 1) * len(up_chunks):
                    merged_.append(up_chunks[ui_]); ui_ += 1
            merged_ += up_chunks[ui_:]
            conv_chunks = merged_
            conv_state = {"done": 0, "step": 0}

            def conv_tick(total_steps):
                conv_state["step"] += 1
                want = min(len(conv_chunks), (conv_state["step"] * len(conv_chunks) + total_steps - 1) // total_steps)
                while conv_state["done"] < want:
                    kind_, r0, nr = conv_chunks[conv_state["done"]]
                    if kind_ == "dn":
                        fw.dma("pool", [(wdb[r0:r0 + nr, :], w_down[r0:r0 + nr, :])], Bwdb)
                    else:
                        conv_up(fw, r0, nr)
                    conv_state["done"] += 1

            units = []
            for tl in range(2):
                nkb = NB // 2 if tl == 0 else NB
                for un in range(nkb):
                    l = nkb - 1 - un
                    units.append((tl, l, (tl == 0) or (l >= NB // 2), un == 0, un == nkb - 1))
            NU = len(units)
            gbase = 0
            load_w(0)
            for j in proj_jobs(0):
                j()
            for hd in range(8):
                h2_ = hd % 2
                jobs = []
                if hd + 1 < 8:
                    load_w(hd + 1)
                    jobs = proj_jobs(hd + 1)
                nj = len(jobs)
                jdone = 0

                def sgM(i):
                    tl, l, masked, first, last = units[i]
                    gi = gbase + i
                    qs = slice(tl * 512, (tl + 1) * 512)
                    M = Mt[gi % 4]; BM = BMt[gi % 4]
                    if masked:
                        fw.op("dve", lambda e: e.tensor_scalar(out=M[:], in0=qpos[:, qs], scalar1=kpos[:, l:l + 1], scalar2=NEG,
                                                               op0=ALU.is_le, op1=ALU.mult), reads=[Bqpos, Bkpos], writes=[BM])

                def sgA(i):
                    tl, l, masked, first, last = units[i]
                    gi = gbase + i
                    qs = slice(tl * 512, (tl + 1) * 512); ks = slice(l * 128, (l + 1) * 128)
                    Z = pz[gi % 2]; BZ = Bpz[gi % 2]; E = Et[gi % 4]; BE = BEt[gi % 4]; L = Lt[gi % 4]; BL = BLt[gi % 4]
                    M = Mt[gi % 4]; BM = BMt[gi % 4]; Rp = Rt[gi % 5]; BRp = BRt[gi % 5]; Rn = Rt[(gi + 1) % 5]; BRn = BRt[(gi + 1) % 5]
                    if first:
                        fw.op("pool", lambda e: e.memset(Rp[:], 0.0), writes=[BRp])
                    fz = [lambda e: e.matmul(Z[:], lhsT=KT[h2_][:, ks], rhs=QT[h2_][:, qs], start=True, stop=not masked)]
                    if masked:
                        fz.append(lambda e: e.matmul(Z[:], lhsT=ident[:], rhs=M[:], start=False, stop=True))
                    fw.op("pe", fz, reads=[BKT[h2_], BQT[h2_], Bident] + ([BM] if masked else []), writes=[BZ])
                    fw.op("act", lambda e: e.activation(out=E[:], in_=Z[:], func=AF.Exp), reads=[BZ], writes=[BE])
                    fw.op("act", lambda e: e.activation(out=L[:], in_=E[:], func=AF.Ln, bias=1.0), reads=[BE], writes=[BL])
                    if not last:
                        fw.op("pool", lambda e: e.tensor_tensor(out=Rn[:], in0=Rp[:], in1=L[:], op=ALU.add), reads=[BRp, BL], writes=[BRn])

                def sgB(i):
                    tl, l, masked, first, last = units[i]
                    gi = gbase + i
                    E = Et[gi % 4]; BE = BEt[gi % 4]; L = Lt[gi % 4]; BL = BLt[gi % 4]; Rp = Rt[gi % 5]; BRp = BRt[gi % 5]
                    Sp = pS[gi % 2]; BSp = BpS[gi % 2]; X = Xt[gi % 2]; BX = BXt[gi % 2]; Wm = Wt[gi % 3]; BWm = BWt[gi % 3]
                    fs = []
                    if not first:
                        fs.append(lambda e: e.matmul(Sp[:], lhsT=negOnes[:], rhs=Rp[:], start=True, stop=False))
                    fs.append(lambda e: e.matmul(Sp[:], lhsT=negU[:], rhs=L[:], start=first, stop=True))
                    fw.op("pe", fs, reads=[BnegOnes, BnegU, BL] + ([] if first else [BRp]), writes=[BSp])
                    fw.op("act", lambda e: e.activation(out=X[:], in_=Sp[:], func=AF.Exp), reads=[BSp], writes=[BX])
                    fw.op("dve", lambda e: e.tensor_tensor(out=Wm[:], in0=E[:], in1=X[:], op=ALU.mult), reads=[BE, BX], writes=[BWm])

                def sgC(i):
                    tl, l, masked, first, last = units[i]
                    gi = gbase + i
                    qs = slice(tl * 512, (tl + 1) * 512)
                    Wm = Wt[gi % 3]; BWm = BWt[gi % 3]
                    fw.op("pe", lambda e: e.matmul(pacc[:], lhsT=V[h2_][:, l, :], rhs=Wm[:], start=first, stop=last),
                          reads=[BV[h2_], BWm], writes=[Bpacc])
                    if last:
                        fw.op("dve", lambda e: e.tensor_copy(out=y_bT[:, hd, qs], in_=pacc[:]), reads=[Bpacc], writes=[By_b])

                sgM(0)
                sgM(1)
                for step in range(NU + 3):
                    if step + 2 < NU:
                        sgM(step + 2)
                    if step < NU:
                        sgA(step)
                    if 0 <= step - 2 < NU:
                        sgB(step - 2)
                    if 0 <= step - 3 < NU:
                        sgC(step - 3)
                    conv_tick(8 * (NU + 3))
                    want = min(nj, ((step + 1) * nj + NU - 1) // NU) if nj else 0
                    while jdone < want:
                        jobs[jdone]()
                        jdone += 1
                while jdone < nj:
                    jobs[jdone]()
                    jdone += 1
                gbase += NU
            conv_state["step"] = 10 ** 9
            conv_tick(1)
            fw.barrier()

        with contextlib.ExitStack() as st:
            mT = SB(st, "mT", [128, NDC, 1024], BF16); BmT = Buf("mT")
            hto2 = SB(st, "hto2", [128, 2, NDC, 512], BF16); Bhto2 = Buf("hto2")
            for tl in range(2):
                fw.dma("sp", [(hto2[:, tl, :, :].rearrange("p c t -> p (c t)"), hTo[tl * 128:(tl + 1) * 128, :])], Bhto2, reads=[BhTo])
            wga = [SB(st, f"wga{i}", [128, NDC, 128], BF16) for i in range(2)]; Bwga = [Buf(f"wga{i}") for i in range(2)]
            wgb = [SB(st, f"wgb{i}", [128, NDC, 128], BF16) for i in range(2)]; Bwgb = [Buf(f"wgb{i}") for i in range(2)]
            wa = [SB(st, f"wa{i}", [128, 8, 128], BF16) for i in range(2)]; Bwa = [Buf(f"wa{i}") for i in range(2)]
            wb = [SB(st, f"wb{i}", [128, 8, 128], BF16) for i in range(2)]; Bwb = [Buf(f"wb{i}") for i in range(2)]
            sga = SB(st, "sga", [128, 512], F32); Bsga = Buf("sga")
            sgb = SB(st, "sgb", [128, 512], F32); Bsgb = Buf("sgb")
            ta = SB(st, "ta", [128, 512], F32); Bta = Buf("ta")
            tb_ = SB(st, "tb_", [128, 512], F32); Btb = Buf("tb_")
            pga = PS(st, "pga", [128, 512], F32); Bpga = Buf("pga")
            pgb = PS(st, "pgb", [128, 512], F32); Bpgb = Buf("pgb")
            pA = PS(st, "pA", [128, 512], F32); BpA = Buf("pA")
            pB = PS(st, "pB", [128, 512], F32); BpB = Buf("pB")
            w_in_v = w_in.rearrange("(c p) f -> p c f", p=128)
            w_ba_v = w_ba.rearrange("(c p) f -> p c f", p=128)
            w_bb_v = w_bb.rearrange("(c p) f -> p c f", p=128)
            for j in range(NDC):
                i2 = j % 2
                js = slice(j * 128, (j + 1) * 128)
                fw.dma("pool", [(wga[i2][:, q * 4:(q + 1) * 4, :], w_in_v[:, q * 4:(q + 1) * 4, 5120 + j * 128:5120 + (j + 1) * 128]) for q in range(4)], Bwga[i2])
                fw.dma("pool", [(wgb[i2][:, q * 4:(q + 1) * 4, :], w_in_v[:, q * 4:(q + 1) * 4, 7168 + j * 128:7168 + (j + 1) * 128]) for q in range(4)], Bwgb[i2])
                fw.dma("pool", [(wa[i2][:, q * 4:(q + 1) * 4, :], w_ba_v[:, q * 4:(q + 1) * 4, js]) for q in range(2)], Bwa[i2])
                fw.dma("pool", [(wb[i2][:, q * 4:(q + 1) * 4, :], w_bb_v[:, q * 4:(q + 1) * 4, js]) for q in range(2)], Bwb[i2])
                for tl in range(2):
                    qs = slice(tl * 512, (tl + 1) * 512)
                    fw.op("pe", [(lambda e, dc=dc: e.matmul(pga[:], lhsT=wga[i2][:, dc, :], rhs=hto2[:, tl, dc, :],
                                                            start=(dc == 0), stop=(dc == NDC - 1))) for dc in range(NDC)],
                          reads=[Bwga[i2], Bhto2], writes=[Bpga])
                    fw.op("act", lambda e: e.activation(out=sga[:], in_=pga[:], func=AF.Sigmoid), reads=[Bpga], writes=[Bsga])
                    fw.op("pe", [(lambda e, dc=dc: e.matmul(pgb[:], lhsT=wgb[i2][:, dc, :], rhs=hto2[:, tl, dc, :],
                                                            start=(dc == 0), stop=(dc == NDC - 1))) for dc in range(NDC)],
                          reads=[Bwgb[i2], Bhto2], writes=[Bpgb])
                    fw.op("act", lambda e: e.activation(out=sgb[:], in_=pgb[:], func=AF.Sigmoid), reads=[Bpgb], writes=[Bsgb])
                    fw.op("pe", [(lambda e, cc=cc: e.matmul(pA[:], lhsT=wa[i2][:, cc, :], rhs=y_aT[:, cc, qs],
                                                            start=(cc == 0), stop=(cc == 7))) for cc in range(8)],
                          reads=[Bwa[i2], By_a], writes=[BpA])
                    fw.op("dve", lambda e: e.tensor_tensor(out=ta[:], in0=pA[:], in1=sga[:], op=ALU.mult), reads=[BpA, Bsga], writes=[Bta])
                    fw.op("pe", [(lambda e, cc=cc: e.matmul(pB[:], lhsT=wb[i2][:, cc, :], rhs=y_bT[:, cc, qs],
                                                            start=(cc == 0), stop=(cc == 7))) for cc in range(8)],
                          reads=[Bwb[i2], By_b], writes=[BpB])
                    fw.op("dve", lambda e: e.tensor_tensor(out=tb_[:], in0=pB[:], in1=sgb[:], op=ALU.mult), reads=[BpB, Bsgb], writes=[Btb])
                    fw.op("dve", lambda e: e.tensor_tensor(out=mT[:, j, qs], in0=ta[:], in1=tb_[:], op=ALU.add), reads=[Bta, Btb], writes=[BmT])
            fw.dma("sp", [(mTs[:, :], mT[:].rearrange("p c t -> p (c t)"))], BmTs, reads=[BmT])
            if debug:
                fw.dma("sp", [(dbg_ya[:, :], y_aT[:].rearrange("p c t -> p (c t)")), (dbg_yb[:, :], y_bT[:].rearrange("p c t -> p (c t)"))],
                       Bdbg, reads=[By_a, By_b])
            fw.barrier()
        stA.close()

        with contextlib.ExitStack() as st6:
            x1 = SB(st6, "x1", [128, 8, D], F32)
            Bx1 = [Buf(f"x1_{b}") for b in range(8)]
            h2 = SB(st6, "h2", [128, 8, D], BF16)
            Bh2 = [Buf(f"h2_{b}") for b in range(8)]
            LG = SB(st6, "LG", [128, 8, 36], F32); BLG = Buf("LG")
            for b in range(8):
                fw.dma("sp", [(x1[:, b, :], x_own[b * 128:(b + 1) * 128, :])], Bx1[b])

            with contextlib.ExitStack() as st:
                mT = SB(st, "mT2", [128, NDC, 1024], BF16); BmT = Buf("mT2")
                fw.dma("sp", [(mT[:].rearrange("p c t -> p (c t)"), mTs[:, :])], BmT, reads=[BmTs])
                wo = [SB(st, f"wo{i}", [128, NDC, 256], BF16) for i in range(2)]; Bwo = [Buf(f"wo{i}") for i in range(2)]
                po = [PS(st, f"po{i}", [128, 256], F32) for i in range(2)]; Bpo = [Buf(f"po{i}") for i in range(2)]
                w_out_v = w_out.rearrange("(c p) f -> p c f", p=128)
                k = 0
                for et in range(8):
                    es = slice(et * 256, (et + 1) * 256)
                    fw.dma("pool", [(wo[et % 2][:, q * 4:(q + 1) * 4, :], w_out_v[:, q * 4:(q + 1) * 4, es]) for q in range(4)], Bwo[et % 2])
                    for b in range(8):
                        P = po[k % 2]; BP = Bpo[k % 2]
                        fw.op("pe", [(lambda e, dc=dc: e.matmul(P[:], lhsT=mT[:, dc, b * 128:(b + 1) * 128], rhs=wo[et % 2][:, dc, :],
                                                                start=(dc == 0), stop=(dc == NDC - 1))) for dc in range(NDC)],
                              reads=[BmT, Bwo[et % 2]], writes=[BP])
                        fw.op("dve", lambda e: e.tensor_tensor(out=x1[:, b, es], in0=x1[:, b, es], in1=P[:], op=ALU.add),
                              reads=[BP], writes=[Bx1[b]])
                        k += 1
                fw.barrier()

            with contextlib.ExitStack() as st:
                gf = SB(st, "gf", [128, D], F32); Bgf = Buf("gf")
                fw.dma("sp", [(gf[:], g_ffn[0:1, :].partition_broadcast(128))], Bgf)
                wr = SB(st, "wr", [128, NDC, 36], F32); Bwr = Buf("wr")
                fw.dma("sp", [(wr[:, :, 0:4], w_rg.rearrange("(c p) f -> p c f", p=128)),
                              (wr[:, :, 4:36], w_re.rearrange("(c p) f -> p c f", p=128))], Bwr)
                brt = SB(st, "brt", [128, 36], F32); Bbrt = Buf("brt")
                fw.dma("sp", [(brt[:, 0:4], b_rg[0:1, :].partition_broadcast(128)),
                              (brt[:, 4:36], b_re[0:1, :].partition_broadcast(128))], Bbrt)
                junk2 = SB(st, "junk2", [128, D], BF16); Bj2 = Buf("junk2")
                h2f = [SB(st, f"h2f{i}", [128, D], F32) for i in range(2)]; Bh2f = [Buf(f"h2f{i}") for i in range(2)]
                h2T = [SB(st, f"h2T{i}", [128, NDC, 128], F32) for i in range(2)]; Bh2T = [Buf(f"h2T{i}") for i in range(2)]
                st2 = SB(st, "st2", [128, 8, 4], F32); Bst2 = Buf("st2")
                ptf = [PS(st, f"ptf{i}", [128, 512], F32) for i in range(4)]; Bptf = [Buf(f"ptf{i}") for i in range(4)]
                plg = PS(st, "plg", [128, 64], F32); Bplg = Buf("plg")
                for b in range(8):
                    fw.op("act", lambda e: e.activation(out=junk2[:], in_=x1[:, b, :], func=AF.Square, accum_out=st2[:, b, 0:1]),
                          reads=[Bx1[b]], writes=[Bj2, Bst2])
                    fw.op("act", lambda e: e.activation(out=st2[:, b, 1:2], in_=st2[:, b, 0:1], func=AF.Ln, scale=1.0 / D, bias=EPS), reads=[Bst2], writes=[Bst2])
                    fw.op("act", lambda e: e.activation(out=st2[:, b, 2:3], in_=st2[:, b, 1:2], func=AF.Exp, scale=-0.5), reads=[Bst2], writes=[Bst2])
                    HF = h2f[b % 2]; BHF = Bh2f[b % 2]
                    fw.op("dve", lambda e: e.scalar_tensor_tensor(out=HF[:], in0=x1[:, b, :], scalar=st2[:, b, 2:3], in1=gf[:],
                                                                  op0=ALU.mult, op1=ALU.mult), reads=[Bx1[b], Bst2, Bgf], writes=[BHF])
                    fw.op("pool", lambda e: e.tensor_copy(out=h2[:, b, :], in_=HF[:]), reads=[BHF], writes=[Bh2[b]])
                    HT2 = h2T[b % 2]; BHT2 = Bh2T[b % 2]
                    for q4 in range(4):
                        PT = ptf[q4]; BPT = Bptf[q4]
                        fw.op("pe", [(lambda e, i=i: e.transpose(out=PT[:, i * 128:(i + 1) * 128],
                                                                  in_=HF[:, (q4 * 4 + i) * 128:(q4 * 4 + i + 1) * 128], identity=identf[:]))
                                     for i in range(4)], reads=[BHF, Bidentf], writes=[BPT])
                        fw.op("act" if q4 % 2 == 0 else "dve",
                              (lambda e: e.copy(out=HT2[:, q4 * 4:(q4 + 1) * 4, :].rearrange("p c t -> p (c t)"), in_=PT[:])) if q4 % 2 == 0 else
                              (lambda e: e.tensor_copy(out=HT2[:, q4 * 4:(q4 + 1) * 4, :].rearrange("p c t -> p (c t)"), in_=PT[:])),
                              reads=[BPT], writes=[BHT2])
                    fw.op("pe", [(lambda e, dc=dc: e.matmul(plg[:, 0:36], lhsT=HT2[:, dc, :], rhs=wr[:, dc, :],
                                                            start=(dc == 0), stop=(dc == NDC - 1))) for dc in range(NDC)],
                          reads=[BHT2, Bwr], writes=[Bplg])
                    fw.op("dve", lambda e: e.tensor_tensor(out=LG[:, b, :], in0=plg[:, 0:36], in1=brt[:], op=ALU.add),
                          reads=[Bplg, Bbrt], writes=[BLG])
                if debug:
                    fw.dma("sp", [(dbg_x1[:, :], x1[:].rearrange("p b f -> p (b f)")), (dbg_lg[:, :], LG[:].rearrange("p b f -> p (b f)"))],
                           Bdbg, reads=Bx1 + [BLG])
                fw.barrier()

            with contextlib.ExitStack() as st:
                G = SB(st, "G", [128, 8, 32], F32); BG = Buf("G")
                MK = SB(st, "MK", [128, 8, 32], BF16); BMK = Buf("MK")
                MKf = SB(st, "MKf", [128, 8, 32], F32); BMKf = Buf("MKf")
                POS = SB(st, "POS", [128, 8, 32], F32); BPOS = Buf("POS")
                rs = SB(st, "rs", [128, 8, 64], F32); Brs = Buf("rs")
                pb = [PS(st, f"pb{i}", [128, 512], F32) for i in range(8)]; Bpb = [Buf(f"pb{i}") for i in range(8)]
                ppos = pb[0]; Bppos = Bpb[0]
                def route_ops(b):
                    seq = []
                    R_ = rs[:, b, :]
                    lgp = LG[:, b, 0:4]
                    lep = LG[:, b, 4:36]
                    def A(f, w=()):
                        seq.append(("dve", f, list(w)))
                    A(lambda e: e.reduce_max(out=R_[:, 0:1], in_=lgp, axis=AX.X))
                    A(lambda e: e.tensor_scalar(out=R_[:, 1:2], in0=R_[:, 0:1], scalar1=-1.0, scalar2=None, op0=ALU.mult))
                    A(lambda e: e.tensor_scalar(out=R_[:, 4:8], in0=lgp, scalar1=R_[:, 0:1], scalar2=None, op0=ALU.is_equal))
                    seq.append(("act", lambda e: e.activation(out=R_[:, 24:28], in_=lgp, func=AF.Exp, bias=R_[:, 1:2], accum_out=R_[:, 2:3]), []))
                    A(lambda e: e.reciprocal(out=R_[:, 3:4], in_=R_[:, 2:3]))
                    A(lambda e: e.tensor_scalar(out=R_[:, 8:16], in0=lep[:, 0:8], scalar1=R_[:, 4:5], scalar2=None, op0=ALU.mult))
                    for g in range(1, 4):
                        A(lambda e, g=g: e.scalar_tensor_tensor(out=R_[:, 8:16], in0=lep[:, g * 8:(g + 1) * 8], scalar=R_[:, 4 + g:5 + g],
                                                                in1=R_[:, 8:16], op0=ALU.mult, op1=ALU.add))
                    A(lambda e: e.reduce_max(out=R_[:, 16:17], in_=R_[:, 8:16], axis=AX.X))
                    A(lambda e: e.tensor_scalar(out=R_[:, 32:40], in0=R_[:, 8:16], scalar1=R_[:, 16:17], scalar2=None, op0=ALU.is_equal))
                    A(lambda e: e.scalar_tensor_tensor(out=R_[:, 40:48], in0=R_[:, 32:40], scalar=-1.0e30, in1=R_[:, 8:16], op0=ALU.mult, op1=ALU.add))
                    A(lambda e: e.reduce_max(out=R_[:, 17:18], in_=R_[:, 40:48], axis=AX.X))
                    A(lambda e: e.tensor_scalar(out=R_[:, 48:56], in0=R_[:, 40:48], scalar1=R_[:, 17:18], scalar2=None, op0=ALU.is_equal))
                    A(lambda e: e.tensor_tensor(out=R_[:, 18:19], in0=R_[:, 17:18], in1=R_[:, 16:17], op=ALU.subtract))
                    seq.append(("act", lambda e: e.activation(out=R_[:, 19:20], in_=R_[:, 18:19], func=AF.Exp), []))
                    A(lambda e: e.tensor_scalar(out=R_[:, 20:21], in0=R_[:, 19:20], scalar1=1.0, scalar2=None, op0=ALU.add))
                    A(lambda e: e.reciprocal(out=R_[:, 21:22], in_=R_[:, 20:21]))
                    A(lambda e: e.tensor_tensor(out=R_[:, 22:23], in0=R_[:, 19:20], in1=R_[:, 21:22], op=ALU.mult))
                    A(lambda e: e.tensor_tensor(out=R_[:, 23:24], in0=R_[:, 21:22], in1=R_[:, 3:4], op=ALU.mult))
                    A(lambda e: e.tensor_tensor(out=R_[:, 24:25], in0=R_[:, 22:23], in1=R_[:, 3:4], op=ALU.mult))
                    A(lambda e: e.tensor_scalar(out=R_[:, 56:64], in0=R_[:, 32:40], scalar1=R_[:, 23:24], scalar2=None, op0=ALU.mult))
                    A(lambda e: e.scalar_tensor_tensor(out=R_[:, 56:64], in0=R_[:, 48:56], scalar=R_[:, 24:25], in1=R_[:, 56:64], op0=ALU.mult, op1=ALU.add))
                    for g in range(4):
                        A(lambda e, g=g: e.tensor_scalar(out=G[:, b, g * 8:(g + 1) * 8], in0=R_[:, 56:64], scalar1=R_[:, 4 + g:5 + g], scalar2=None, op0=ALU.mult), w=[BGb[b]])
                    A(lambda e: e.tensor_scalar(out=MKf[:, b, :], in0=G[:, b, :], scalar1=0.0, scalar2=None, op0=ALU.is_gt), w=[BMKfb[b]])
                    A(lambda e: e.tensor_copy(out=MK[:, b, :], in_=MKf[:, b, :]), w=[BMKb[b]])
                    return seq

                Brsb = [Buf(f"rs{b}") for b in range(8)]
                BGb = [Buf(f"G{b}") for b in range(8)]
                BMKfb = [Buf(f"MKf{b}") for b in range(8)]
                BMKb = [Buf(f"MK{b}") for b in range(8)]
                seqs = [route_ops(b) for b in range(8)]
                for k in range(len(seqs[0])):
                    for b in range(8):
                        eng_, f_, w_ = seqs[b][k]
                        fw.op(eng_, f_, reads=[BLG, Brsb[b], BGb[b], BMKfb[b]], writes=[Brsb[b]] + w_)
                for b in range(8):
                    fns = [lambda e: e.matmul(ppos[:, 0:32], lhsT=stri[:], rhs=MK[:, b, :], start=True, stop=(b == 0))]
                    for b2 in range(b):
                        fns.append(lambda e, b2=b2: e.matmul(ppos[:, 0:32], lhsT=ones[:], rhs=MK[:, b2, :], start=False, stop=(b2 == b - 1)))
                    fw.op("pe", fns, reads=[Bstri, Bones] + BMKb, writes=[Bppos])
                    fw.op("dve", lambda e: e.tensor_copy(out=POS[:, b, :], in_=ppos[:, 0:32]), reads=[Bppos], writes=[BPOS])

                if debug:
                    fw.dma("sp", [(dbg_g[:, :], G[:].rearrange("p b f -> p (b f)")), (dbg_pos[:, :], POS[:].rearrange("p b f -> p (b f)"))],
                           Bdbg, reads=BGb + [BPOS])
                P01 = [SB(st, f"P01_{i}", [128, 8, 128], BF16) for i in range(2)]; BP01 = [Buf(f"P01_{i}") for i in range(2)]
                Pw = [SB(st, f"Pw_{i}", [128, 8, 128], BF16) for i in range(2)]; BPw = [Buf(f"Pw_{i}") for i in range(2)]
                PwT = [SB(st, f"PwT_{i}", [128, 8, 128], BF16) for i in range(3)]; BPwT = [Buf(f"PwT_{i}") for i in range(3)]
                XgT = SB(st, "XgT", [128, NDC, 128], BF16); BXgT = Buf("XgT")
                NSL = 10
                gu = [SB(st, f"gu{i}", [128, 2, DE], BF16) for i in range(NSL)]; Bgu = [Buf(f"gu{i}") for i in range(NSL)]
                Bguu = [Buf(f"guu{i}") for i in range(NSL)]
                wub_v = wub.rearrange("(e c p) f -> e c p f", p=128, c=NDC)
                NDL = 6
                dn = [SB(st, f"dn{i}", [128, D], BF16) for i in range(NDL)]; Bdn = [Buf(f"dn{i}") for i in range(NDL)]
                sg = SB(st, "sg", [128, NFC * 128], F32); Bsg = Buf("sg")
                hdT = SB(st, "hdT", [128, NFC, 128], BF16); BhdT = Buf("hdT")
                ybr = [SB(st, f"yb{i}", [128, D], BF16) for i in range(2)]; Bybr = [Buf(f"yb{i}") for i in range(2)]
                w_gate_v = w_gate.rearrange("(e c p) f -> e c p f", p=128, c=NDC)
                w_up_v = w_up.rearrange("(e c p) f -> e c p f", p=128, c=NDC)
                wdb_v = wdb.rearrange("(e c p) f -> e c p f", p=128, c=NFC)
                si = 0
                di = 0
                cstate = {"ci": 0}
                pending = []
                for ex in range(n_moe_experts):
                    i2 = ex % 2
                    for b in range(8):
                        fw.op("dve", [lambda e: e.tensor_scalar(out=P01[i2][:, b, :], in0=iota_s[:], scalar1=POS[:, b, ex:ex + 1],
                                                                scalar2=MKf[:, b, ex:ex + 1], op0=ALU.is_equal, op1=ALU.mult),
                                      lambda e: e.tensor_scalar(out=Pw[i2][:, b, :], in0=iota_s[:], scalar1=POS[:, b, ex:ex + 1],
                                                                scalar2=G[:, b, ex:ex + 1], op0=ALU.is_equal, op1=ALU.mult)],
                              reads=[Biota, BPOS] + BMKfb + BGb, writes=[BP01[i2], BPw[i2]])
                    fw.op("pe", [(lambda e, b=b: e.matmul(pb[b // 4][:, (b % 4) * 128:(b % 4 + 1) * 128], lhsT=Pw[i2][:, b, :], rhs=ident[:],
                                                          start=True, stop=True)) for b in range(8)],
                          reads=[BPw[i2], Bident], writes=[Bpb[0], Bpb[1]])
                    for hh in range(2):
                        fw.op("act", lambda e: e.copy(out=PwT[ex % 3][:, hh * 4:(hh + 1) * 4, :].rearrange("p b t -> p (b t)"), in_=pb[hh][:]),
                              reads=[Bpb[hh]], writes=[BPwT[ex % 3]])
                    for q4 in range(4):
                        PB = pb[4 + q4]; BPB = Bpb[4 + q4]
                        fw.op("pe", [(lambda e, i=i, b=b: e.matmul(PB[:, i * 128:(i + 1) * 128],
                                                                   lhsT=h2[:, b, (q4 * 4 + i) * 128:(q4 * 4 + i + 1) * 128],
                                                                   rhs=P01[i2][:, b, :], start=(b == 0), stop=(b == 7)))
                                     for i in range(4) for b in range(8)], reads=Bh2 + [BP01[i2]], writes=[BPB])
                        fw.op("act" if q4 % 2 == 0 else "dve",
                              (lambda e: e.copy(out=XgT[:, q4 * 4:(q4 + 1) * 4, :].rearrange("p c t -> p (c t)"), in_=PB[:])) if q4 % 2 == 0 else
                              (lambda e: e.tensor_copy(out=XgT[:, q4 * 4:(q4 + 1) * 4, :].rearrange("p c t -> p (c t)"), in_=PB[:])),
                              reads=[BPB], writes=[BXgT])
                    for dc in range(NDC):
                        GU = gu[si % NSL]; BGU = Bgu[si % NSL]
                        BGUu = Bguu[si % NSL]
                        fw.dma("pool", [(GU[:, 0, :], w_gate_v[ex, dc])], BGU)
                        if ex < NUP:
                            fw.dma("sp", [(GU[:, 1, :], wub_v[ex, dc])], BGUu, reads=Bcv)
                        else:
                            fw.dma("pool", [(GU[:, 1, :], w_up_v[ex, dc])], BGUu)
                        fns = []
                        for fc in range(NFC):
                            fns.append(lambda e, fc=fc: e.matmul(pb[fc // 4][:, (fc % 4) * 128:(fc % 4 + 1) * 128], lhsT=GU[:, 0, fc * 128:(fc + 1) * 128],
                                                                 rhs=XgT[:, dc, :], start=(dc == 0 and fc % 4 == 0), stop=(dc == NDC - 1)))
                            fns.append(lambda e, fc=fc: e.matmul(pb[2 + fc // 4][:, (fc % 4) * 128:(fc % 4 + 1) * 128], lhsT=GU[:, 1, fc * 128:(fc + 1) * 128],
                                                                 rhs=XgT[:, dc, :], start=(dc == 0 and fc % 4 == 0), stop=(dc == NDC - 1)))
                        fw.op("pe", fns, reads=[BGU, BGUu, BXgT], writes=[Bpb[0], Bpb[1], Bpb[2], Bpb[3]])
                        si += 1
                        for _ in range(2):
                            if pending:
                                pending.pop(0)()
                    while pending:
                        pending.pop(0)()
                    for hh in range((NFC + 3) // 4):
                        w_ = min(4, NFC - hh * 4) * 128
                        fw.op("act", lambda e: e.activation(out=sg[:, hh * 512:hh * 512 + w_], in_=pb[hh][:, 0:w_], func=AF.Silu), reads=[Bpb[hh]], writes=[Bsg])
                        fw.op("dve", lambda e: e.tensor_tensor(out=hdT[:, hh * 4:hh * 4 + w_ // 128, :].rearrange("p c t -> p (c t)"),
                                                               in0=sg[:, hh * 512:hh * 512 + w_], in1=pb[2 + hh][:, 0:w_], op=ALU.mult),
                              reads=[Bsg, Bpb[2 + hh]], writes=[BhdT])
                    for fc in range(NFC):
                        DN = dn[di % NDL]; BDN = Bdn[di % NDL]
                        fw.dma("sp", [(DN[:], wdb_v[ex, fc])], BDN, reads=[Bwdb])
                        fw.op("pe", [(lambda e, dm=dm: e.matmul(pb[4 + dm][:], lhsT=hdT[:, fc, :], rhs=DN[:, dm * 512:(dm + 1) * 512],
                                                                start=(fc == 0), stop=(fc == NFC - 1))) for dm in range(4)],
                              reads=[BDN, BhdT], writes=[Bpb[4 + dm] for dm in range(4)])
                        di += 1
                    for dm in range(4):
                        fw.op("act", lambda e: e.copy(out=ybr[i2][:, dm * 512:(dm + 1) * 512], in_=pb[4 + dm][:]), reads=[Bpb[4 + dm]], writes=[Bybr[i2]])
                    if ex % 2 == 0 and ex + 1 < n_moe_experts:
                        continue
                    grp_ = [ex] if ex % 2 == 0 else [ex - 1, ex]

                    def mk_item(b, dm, grp_=grp_):
                        def item():
                            PC = pb[4 + cstate["ci"] % 4]; BPC = Bpb[4 + cstate["ci"] % 4]
                            cstate["ci"] += 1
                            fw.op("pe", [(lambda e, k=k, ee=ee: e.matmul(PC[:], lhsT=PwT[ee % 3][:, b, :], rhs=ybr[ee % 2][:, dm * 512:(dm + 1) * 512],
                                                                         start=(k == 0), stop=(k == len(grp_) - 1))) for k, ee in enumerate(grp_)],
                                  reads=[BPwT[ee % 3] for ee in grp_] + [Bybr[ee % 2] for ee in grp_], writes=[BPC])
                            fw.op("dve", lambda e: e.tensor_tensor(out=x1[:, b, dm * 512:(dm + 1) * 512], in0=x1[:, b, dm * 512:(dm + 1) * 512],
                                                                   in1=PC[:], op=ALU.add), reads=[BPC], writes=[Bx1[b]])
                        return item
                    pending.extend(mk_item(b, dm) for b in range(8) for dm in range(4))
                while pending:
                    pending.pop(0)()
                if debug:
                    fw.dma("sp", [(dbg_x2[:, :], x1[:].rearrange("p b f -> p (b f)"))], Bdbg, reads=Bx1)
                fw.barrier()

            with contextlib.ExitStack() as st:
                gfin = SB(st, "gfin", [128, D], F32); Bgfin = Buf("gfin")
                fw.dma("sp", [(gfin[:], g_fin[0:1, :].partition_broadcast(128))], Bgfin)
                junk3 = SB(st, "junk3", [128, D], BF16); Bj3 = Buf("junk3")
                st3 = SB(st, "st3", [128, 8, 4], F32); Bst3 = Buf("st3")
                ot = [SB(st, f"ot{i}", [128, D], F32) for i in range(2)]; Bot = [Buf(f"ot{i}") for i in range(2)]
                Bout = Buf("out")
                for b in range(8):
                    fw.op("act", lambda e: e.activation(out=junk3[:], in_=x1[:, b, :], func=AF.Square, accum_out=st3[:, b, 0:1]),
                          reads=[Bx1[b]], writes=[Bj3, Bst3])
                    fw.op("act", lambda e: e.activation(out=st3[:, b, 1:2], in_=st3[:, b, 0:1], func=AF.Ln, scale=1.0 / D, bias=EPS), reads=[Bst3], writes=[Bst3])
                    fw.op("act", lambda e: e.activation(out=st3[:, b, 2:3], in_=st3[:, b, 1:2], func=AF.Exp, scale=-0.5), reads=[Bst3], writes=[Bst3])
                    O = ot[b % 2]; BO = Bot[b % 2]
                    fw.op("dve", lambda e: e.scalar_tensor_tensor(out=O[:], in0=x1[:, b, :], scalar=st3[:, b, 2:3], in1=gfin[:],
                                                                  op0=ALU.mult, op1=ALU.mult), reads=[Bx1[b], Bst3, Bgfin], writes=[BO])
                    fw.dma("sp", [(out_d[b * 128:(b + 1) * 128, :], O[:])], Bout, reads=[BO])
                fw.barrier()
    return nc


NT_FULL = 16
NCORES = 8


def own_rows(c, NT):
    a = np.arange(c * 512, (c + 1) * 512)
    b = np.arange((NT - 1 - c) * 512, (NT - c) * 512)
    return np.concatenate([a, b])


def make_in_maps(inputs, NT, ncores):
    f = lambda a: np.ascontiguousarray(np.asarray(a, dtype=np.float32))
    x = f(inputs["x"]).reshape(-1, D)
    DE = inputs["w_gate"].shape[-1]
    shared = {
        "x_all": x,
        "g_mix": f(inputs["g_mix"]).reshape(1, D),
        "w_in": f(inputs["w_in"]).reshape(D, 9216),
        "ln_v_g": f(inputs["ln_v_g"]).reshape(1, 1024),
        "ln_v_b": f(inputs["ln_v_b"]).reshape(1, 1024),
        "w_spatial": f(inputs["w_spatial"]).reshape(8, 128, 128),
        "b_spatial": f(inputs["b_spatial"]).reshape(1, 1024),
        "w_branch_a": f(inputs["w_branch_a"]).reshape(1024, D),
        "w_branch_b": f(inputs["w_branch_b"]).reshape(1024, D),
        "w_out": f(inputs["w_out"]).reshape(D, D),
        "g_ffn": f(inputs["g_ffn"]).reshape(1, D),
        "w_router_group": f(inputs["w_router_group"]).reshape(D, 4),
        "b_router_group": f(inputs["b_router_group"]).reshape(1, 4),
        "w_router_expert": f(inputs["w_router_expert"]).reshape(D, 32),
        "b_router_expert": f(inputs["b_router_expert"]).reshape(1, 32),
        "w_gate": f(inputs["w_gate"]).reshape(32 * D, DE),
        "w_up": f(inputs["w_up"]).reshape(32 * D, DE),
        "w_down": f(inputs["w_down"]).reshape(32 * DE, D),
        "g_final": f(inputs["g_final"]).reshape(1, D),
    }
    maps = []
    for c in range(ncores):
        rows = own_rows(c, NT)
        m = dict(shared)
        m["x_own"] = np.ascontiguousarray(x[rows])
        m["qpos"] = rows.astype(np.float32).reshape(1, 1024)
        maps.append(m)
    return maps


def kernel(**inputs):
    NT = NT_FULL
    DE = inputs["w_gate"].shape[-1]
    nc = build_nc(NT, DE)
    in_maps = make_in_maps(inputs, NT, NCORES)
    res = run_bass_kernel_spmd(nc, in_maps, core_ids=list(range(NCORES)))
    out = np.zeros((NT * 512, D), np.float32)
    for c in range(NCORES):
        out[own_rows(c, NT)] = res.results[c]["out"]
    return out.reshape(1, NT * 512, D)
```
